# Optimizing a Trainium2 kernel written in Bass

```python
import math
import jax, jax.numpy as jnp
from jax import lax
import numpy as np

D_MODEL = 1024
BATCH = 4
SEQ = 8192
DEPTH = 2

HEAD_DIM = 64
N_MIXERS = 4
GROUP_WIDTH = D_MODEL // N_MIXERS
N_GROUP_HEADS = GROUP_WIDTH // HEAD_DIM
DIFF_HALF = HEAD_DIM // 2
ROPE_THETA = 10000.0
MAX_POS_OFFSET = 4096
Q_BLOCK = 128
MOBA_BLOCK = 256
MOBA_TOPK = 3
MOBA_Q_BLOCK = 32
NSA_CMP_LEN = 32
NSA_CMP_STRIDE = 16
NSA_CMP_HIDDEN = 256
NSA_SEL_BLOCK = 64
NSA_SEL_TOPK = 16
NSA_WINDOW = 512
NSA_N_BRANCH = 3
D_FF = 3584
N_EXPERTS = 8
TOP_K = 2
MOE_ROW_BLOCK = 256
PLE_DIM = 256
LN_EPS = 1e-5
RMS_EPS = 1e-5
DEEPNORM_ALPHA = (2 * DEPTH) ** 0.25
DEEPNORM_BETA = (8 * DEPTH) ** -0.25
IN_SPLITS = (GROUP_WIDTH,) * 10 + (HEAD_DIM,) * 6 + (N_GROUP_HEADS * NSA_N_BRANCH,)
N_IN = sum(IN_SPLITS)

kernel_name = 'hymba_style_sb_diff_moba_nsa_deepnorm_moe'


def layer_norm(x, g, b):
    xf = x.astype(jnp.float32)
    mu = jnp.mean(xf, -1, keepdims=True)
    var = jnp.mean(jnp.square(xf - mu), -1, keepdims=True)
    return ((xf - mu) * lax.rsqrt(var + LN_EPS) * g + b).astype(x.dtype)


def rope(x, positions):
    half = x.shape[-1] // 2
    inv_freq = ROPE_THETA ** (-jnp.arange(half, dtype=jnp.float32) / half)
    ang = positions.astype(jnp.float32)[:, :, None, None] * inv_freq
    cos, sin = jnp.cos(ang), jnp.sin(ang)
    x1 = x[..., :half].astype(jnp.float32)
    x2 = x[..., half:].astype(jnp.float32)
    return jnp.concatenate([x1 * cos - x2 * sin, x2 * cos + x1 * sin], -1).astype(x.dtype)


def split_heads(t, n_heads):
    B, S, _ = t.shape
    return t.reshape(B, S, n_heads, -1).transpose(0, 2, 1, 3)


def merge_heads(t):
    B, H, S, d = t.shape
    return t.transpose(0, 2, 1, 3).reshape(B, S, H * d)


def to_qblocks(t, blk):
    S, d = t.shape[-2], t.shape[-1]
    t = t.reshape(t.shape[:-2] + (S // blk, blk, d))
    return jnp.moveaxis(t, -3, 0)


def from_qblocks(o):
    o = jnp.moveaxis(o, 0, -3)
    return o.reshape(o.shape[:-3] + (o.shape[-3] * o.shape[-2], o.shape[-1]))


def masked_softmax(s, mask):
    s = jnp.where(mask, s, -jnp.inf)
    m = jnp.max(s, axis=-1, keepdims=True)
    m = jnp.where(jnp.isfinite(m), m, 0.0)
    e = jnp.exp(s - m)
    den = jnp.sum(e, axis=-1, keepdims=True)
    return e / jnp.where(den > 0, den, 1.0)


def stick_breaking_attention(q, k, v):
    B, H, S, d = q.shape
    scale = d ** -0.5
    kpos = jnp.arange(S)

    def block(args):
        qb, i = args
        qpos = i * Q_BLOCK + jnp.arange(Q_BLOCK)
        z = jnp.einsum('bhqd,bhkd->bhqk', qb, k).astype(jnp.float32) * scale
        past = kpos[None, :] < qpos[:, None]
        log_1m = jnp.where(past, jax.nn.log_sigmoid(-z), 0.0)
        later = lax.cumsum(log_1m, axis=3, reverse=True) - log_1m
        w = jnp.where(past, jnp.exp(jax.nn.log_sigmoid(z) + later), 0.0)
        return jnp.einsum('bhqk,bhkd->bhqd', w.astype(v.dtype), v)

    return from_qblocks(lax.map(block, (to_qblocks(q, Q_BLOCK), jnp.arange(S // Q_BLOCK))))


def diff_attention(q, k, v, lam, gain, lam_init):
    S, dh = q.shape[-2], q.shape[-1]
    scale = dh ** -0.5
    kpos = jnp.arange(S)

    def block(args):
        qb, i = args
        qpos = i * Q_BLOCK + jnp.arange(Q_BLOCK)
        s = jnp.einsum('bhcqd,bhckd->bhcqk', qb, k).astype(jnp.float32) * scale
        p = jax.nn.softmax(jnp.where(kpos[None, :] <= qpos[:, None], s, -jnp.inf), axis=-1)
        a = p[:, :, 0] - lam * p[:, :, 1]
        return jnp.einsum('bhqk,bhkd->bhqd', a.astype(v.dtype), v)

    o = from_qblocks(lax.map(block, (to_qblocks(q, Q_BLOCK), jnp.arange(S // Q_BLOCK))))
    of = o.astype(jnp.float32)
    of = of * lax.rsqrt(jnp.mean(of * of, -1, keepdims=True) + RMS_EPS) * gain
    return (of * (1.0 - lam_init)).astype(v.dtype)


def moba_attention(q, k, v):
    B, H, S, d = q.shape
    scale = d ** -0.5
    nb = -(-S // MOBA_BLOCK)
    pad = nb * MOBA_BLOCK - S
    kp = jnp.pad(k, ((0, 0), (0, 0), (0, pad), (0, 0)))
    vp = jnp.pad(v, ((0, 0), (0, 0), (0, pad), (0, 0)))
    kb = kp.reshape(B, H, nb, MOBA_BLOCK, d)
    vb = vp.reshape(B, H, nb, MOBA_BLOCK, d)
    k_mean = jnp.mean(kb.astype(jnp.float32), axis=3).astype(k.dtype)
    n_top = min(MOBA_TOPK, nb)
    n_g = n_top * MOBA_BLOCK
    blk_ids = jnp.arange(nb)
    in_blk = jnp.arange(MOBA_BLOCK)
    bi = jnp.arange(B)[:, None, None, None]
    hi = jnp.arange(H)[None, :, None, None]

    def block(args):
        qb, i = args
        q0 = i * MOBA_Q_BLOCK
        qpos = q0 + jnp.arange(MOBA_Q_BLOCK)
        own = q0 // MOBA_BLOCK
        gate = jnp.einsum('bhqd,bhnd->bhqn', qb, k_mean).astype(jnp.float32)
        gate = jnp.where(blk_ids < own, gate, -jnp.inf)
        _, sel = lax.top_k(gate, n_top)
        k_g = kb[bi, hi, sel]
        v_g = vb[bi, hi, sel]
        s_g = jnp.einsum('bhqd,bhqnld->bhqnl', qb, k_g).astype(jnp.float32) * scale
        m_g = jnp.broadcast_to((sel < own)[..., None], s_g.shape)
        k_o = lax.dynamic_slice_in_dim(kp, own * MOBA_BLOCK, MOBA_BLOCK, axis=2)
        v_o = lax.dynamic_slice_in_dim(vp, own * MOBA_BLOCK, MOBA_BLOCK, axis=2)
        s_o = jnp.einsum('bhqd,bhld->bhql', qb, k_o).astype(jnp.float32) * scale
        m_o = jnp.broadcast_to((own * MOBA_BLOCK + in_blk)[None, :] <= qpos[:, None], s_o.shape)
        p = masked_softmax(jnp.concatenate([s_g.reshape(B, H, MOBA_Q_BLOCK, n_g), s_o], -1),
                           jnp.concatenate([m_g.reshape(B, H, MOBA_Q_BLOCK, n_g), m_o], -1)).astype(v.dtype)
        return (jnp.einsum('bhqnl,bhqnld->bhqd', p[..., :n_g].reshape(s_g.shape), v_g)
                + jnp.einsum('bhql,bhld->bhqd', p[..., n_g:], v_o))

    return from_qblocks(lax.map(block, (to_qblocks(q, MOBA_Q_BLOCK), jnp.arange(S // MOBA_Q_BLOCK))))


def compress_blocks(t, pos_enc, w1, w2):
    B, S, d = t.shape
    n_span = NSA_CMP_LEN // NSA_CMP_STRIDE
    c = t.reshape(B, S // NSA_CMP_STRIDE, NSA_CMP_STRIDE, d)
    n_chunks = c.shape[1]
    blocks = jnp.concatenate([c[:, j:n_chunks - n_span + 1 + j] for j in range(n_span)], axis=2)
    blocks = (blocks + pos_enc).reshape(B, n_chunks - n_span + 1, NSA_CMP_LEN * d)
    return jax.nn.silu(blocks @ w1) @ w2


def nsa_attention(q, qr, kc_tok, vc_tok, ks, vs, kw, vw, gates, cmp_pos, cmp_w1, cmp_w2):
    B, S, H, d = q.shape
    scale = d ** -0.5
    kc = compress_blocks(kc_tok, cmp_pos[0], cmp_w1[0], cmp_w2[0])
    vc = compress_blocks(vc_tok, cmp_pos[1], cmp_w1[1], cmp_w2[1])
    n_cmp = kc.shape[1]
    cmp_start = jnp.arange(n_cmp) * NSA_CMP_STRIDE
    cmp_end = cmp_start + NSA_CMP_LEN - 1
    n_sel = S // NSA_SEL_BLOCK
    k_top = min(NSA_SEL_TOPK, n_sel)
    sel_start = jnp.arange(n_sel) * NSA_SEL_BLOCK
    overlap = ((cmp_start[:, None] < sel_start[None, :] + NSA_SEL_BLOCK)
               & (cmp_start[:, None] + NSA_CMP_LEN > sel_start[None, :])).astype(jnp.float32)
    ksb = ks.reshape(B, n_sel, NSA_SEL_BLOCK, d)
    vsb = vs.reshape(B, n_sel, NSA_SEL_BLOCK, d)
    kw_pad = jnp.pad(kw, ((0, 0), (NSA_WINDOW, 0), (0, 0)))
    vw_pad = jnp.pad(vw, ((0, 0), (NSA_WINDOW, 0), (0, 0)))
    bi = jnp.arange(B)[:, None, None]
    sel_ids = jnp.arange(n_sel)
    in_blk = jnp.arange(NSA_SEL_BLOCK)
    win_off = jnp.arange(NSA_WINDOW + Q_BLOCK)

    def block(args):
        qb, qrb, gb, i = args
        q0 = i * Q_BLOCK
        qpos = q0 + jnp.arange(Q_BLOCK)
        s_c = jnp.einsum('bhqd,bnd->bhqn', qb, kc).astype(jnp.float32) * scale
        p_c = masked_softmax(s_c, cmp_end[None, :] <= qpos[:, None])
        o_c = jnp.einsum('bhqn,bnd->bhqd', p_c.astype(vc.dtype), vc)
        imp = jnp.einsum('bhqn,nm->bqm', p_c, overlap)
        qblk = qpos // NSA_SEL_BLOCK
        causal_blk = sel_ids[None, :] <= qblk[:, None]
        forced = (sel_ids[None, :] == 0) | (sel_ids[None, :] == qblk[:, None]) | (sel_ids[None, :] == qblk[:, None] - 1)
        imp = jnp.where(forced, jnp.inf, jnp.where(causal_blk, imp, -jnp.inf))
        _, sel = lax.top_k(imp, k_top)
        k_g = ksb[bi, sel]
        v_g = vsb[bi, sel]
        kpos = sel[..., None] * NSA_SEL_BLOCK + in_blk
        mask_s = (sel <= qblk[None, :, None])[..., None] & (kpos <= qpos[None, :, None, None])
        s_s = jnp.einsum('bhqd,bqnld->bhqnl', qrb, k_g).astype(jnp.float32) * scale
        p_s = masked_softmax(s_s.reshape(B, H, Q_BLOCK, -1), mask_s.reshape(B, 1, Q_BLOCK, -1))
        o_s = jnp.einsum('bhqnl,bqnld->bhqd', p_s.reshape(s_s.shape).astype(v_g.dtype), v_g)
        k_w = lax.dynamic_slice_in_dim(kw_pad, q0, NSA_WINDOW + Q_BLOCK, axis=1)
        v_w = lax.dynamic_slice_in_dim(vw_pad, q0, NSA_WINDOW + Q_BLOCK, axis=1)
        kpos_w = q0 - NSA_WINDOW + win_off
        mask_w = ((kpos_w[None, :] >= 0) & (kpos_w[None, :] <= qpos[:, None])
                  & (kpos_w[None, :] > qpos[:, None] - NSA_WINDOW))
        s_w = jnp.einsum('bhqd,bkd->bhqk', qrb, k_w).astype(jnp.float32) * scale
        p_w = masked_softmax(s_w, mask_w)
        o_w = jnp.einsum('bhqk,bkd->bhqd', p_w.astype(v_w.dtype), v_w)
        return gb[..., 0:1] * o_c + gb[..., 1:2] * o_s + gb[..., 2:3] * o_w

    qt = q.transpose(0, 2, 1, 3)
    qrt = qr.transpose(0, 2, 1, 3)
    gt = gates.transpose(0, 2, 1, 3)
    out = lax.map(block, (to_qblocks(qt, Q_BLOCK), to_qblocks(qrt, Q_BLOCK), to_qblocks(gt, Q_BLOCK),
                          jnp.arange(S // Q_BLOCK)))
    return from_qblocks(out)


def token_mixer(h, positions, w_in, w_out, diff_lambda, diff_gain, cmp_pos, cmp_w1, cmp_w2, layer_idx):
    B, S, _ = h.shape
    H = N_GROUP_HEADS
    proj = jnp.einsum('bsd,dn->bsn', h, w_in)
    points = [int(v) for v in np.cumsum(IN_SPLITS)[:-1]]
    (a_q, a_k, a_v, b_q, b_k, b_v, c_q, c_k, c_v, d_q,
     d_kc, d_vc, d_ks, d_vs, d_kw, d_vw, d_g) = jnp.split(proj, points, axis=-1)
    o_a = stick_breaking_attention(split_heads(a_q, H), split_heads(a_k, H), split_heads(a_v, H))
    bq = rope(b_q.reshape(B, S, 2 * H, DIFF_HALF), positions).reshape(B, S, H, 2, DIFF_HALF).transpose(0, 2, 3, 1, 4)
    bk = rope(b_k.reshape(B, S, 2 * H, DIFF_HALF), positions).reshape(B, S, H, 2, DIFF_HALF).transpose(0, 2, 3, 1, 4)
    lam_init = 0.8 - 0.6 * math.exp(-0.3 * layer_idx)
    lf = diff_lambda.astype(jnp.float32)
    lam = jnp.exp(jnp.sum(lf[0] * lf[1])) - jnp.exp(jnp.sum(lf[2] * lf[3])) + lam_init
    o_b = diff_attention(bq, bk, split_heads(b_v, H), lam, diff_gain, lam_init)
    cq = rope(c_q.reshape(B, S, H, HEAD_DIM), positions).transpose(0, 2, 1, 3)
    ck = rope(c_k.reshape(B, S, H, HEAD_DIM), positions).transpose(0, 2, 1, 3)
    o_c = moba_attention(cq, ck, split_heads(c_v, H))
    dq = d_q.reshape(B, S, H, HEAD_DIM)
    ks_r = rope(d_ks.reshape(B, S, 1, HEAD_DIM), positions).reshape(B, S, HEAD_DIM)
    kw_r = rope(d_kw.reshape(B, S, 1, HEAD_DIM), positions).reshape(B, S, HEAD_DIM)
    gates = jax.nn.sigmoid(d_g.astype(jnp.float32)).astype(h.dtype).reshape(B, S, H, NSA_N_BRANCH)
    o_d = nsa_attention(dq, rope(dq, positions), d_kc, d_vc, ks_r, d_vs, kw_r, d_vw, gates, cmp_pos, cmp_w1, cmp_w2)
    mixed = jnp.concatenate([merge_heads(o_a), merge_heads(o_b), merge_heads(o_c), merge_heads(o_d)], -1)
    return jnp.einsum('bsn,nd->bsd', mixed, w_out)


def swiglu(x, w1, w3, w2):
    return (jax.nn.silu(x @ w1) * (x @ w3)) @ w2


def moe_swiglu(x, w_router, w1, w3, w2):
    B, S, D = x.shape
    T = B * S
    xf = x.reshape(T, D)
    logits = (xf @ w_router).astype(jnp.float32)
    top_logits, top_e = lax.top_k(logits, TOP_K)
    gates = jax.nn.softmax(top_logits, axis=-1).astype(x.dtype)
    flat_e = top_e.reshape(-1)
    order = jnp.argsort(flat_e)
    e_sorted = flat_e[order]
    tok_sorted = order // TOP_K
    gate_sorted = gates.reshape(-1)[order]
    counts = jnp.bincount(flat_e, length=N_EXPERTS)
    padded = (counts + MOE_ROW_BLOCK - 1) // MOE_ROW_BLOCK * MOE_ROW_BLOCK
    start = jnp.cumsum(counts) - counts
    pad_end = jnp.cumsum(padded)
    pad_start = pad_end - padded
    dest = pad_start[e_sorted] + jnp.arange(T * TOP_K) - start[e_sorted]
    n_rows = T * TOP_K + N_EXPERTS * MOE_ROW_BLOCK
    n_blk = n_rows // MOE_ROW_BLOCK
    row_tok = jnp.zeros((n_rows,), jnp.int32).at[dest].set(tok_sorted)
    blk_e = jnp.minimum(jnp.searchsorted(pad_end, jnp.arange(n_blk) * MOE_ROW_BLOCK, side='right'), N_EXPERTS - 1)
    xb = xf[row_tok].reshape(n_blk, MOE_ROW_BLOCK, D)

    def expert_block(args):
        xblk, e = args
        return swiglu(xblk, w1[e], w3[e], w2[e])

    y = lax.map(expert_block, (xb, blk_e)).reshape(n_rows, D)
    out = jnp.zeros((T, D), x.dtype).at[tok_sorted].add(y[dest] * gate_sorted[:, None])
    return out.reshape(B, S, D)


def setup_inputs(seed: int = 0) -> dict:
    key = jax.random.key(seed)
    ks = jax.random.split(key, 24)
    f32 = jnp.float32
    n_dense = (DEPTH + 1) // 2
    n_moe = DEPTH // 2

    def nrm(k, shape, scale):
        return jax.random.normal(k, shape, f32) * scale

    offsets = jax.random.randint(ks[2], (BATCH, 1), 0, MAX_POS_OFFSET, dtype=jnp.int32)
    return {
        'x': nrm(ks[0], (BATCH, SEQ, D_MODEL), 1.0),
        'p': nrm(ks[1], (DEPTH, BATCH, SEQ, PLE_DIM), 1.0),
        'positions': offsets + jnp.arange(SEQ, dtype=jnp.int32)[None, :],
        'w_in': nrm(ks[3], (DEPTH, D_MODEL, N_IN), D_MODEL ** -0.5),
        'w_out': nrm(ks[4], (DEPTH, D_MODEL, D_MODEL), D_MODEL ** -0.5 * DEEPNORM_BETA),
        'ln_mix_g': 1.0 + nrm(ks[5], (DEPTH, D_MODEL), 0.02),
        'ln_mix_b': nrm(ks[6], (DEPTH, D_MODEL), 0.02),
        'diff_lambda': nrm(ks[7], (DEPTH, 4, DIFF_HALF), 0.1),
        'diff_gain': 1.0 + nrm(ks[8], (DEPTH, HEAD_DIM), 0.02),
        'nsa_cmp_pos': nrm(ks[9], (DEPTH, 2, NSA_CMP_LEN, HEAD_DIM), 0.1),
        'nsa_cmp_w1': nrm(ks[10], (DEPTH, 2, NSA_CMP_LEN * HEAD_DIM, NSA_CMP_HIDDEN), (NSA_CMP_LEN * HEAD_DIM) ** -0.5),
        'nsa_cmp_w2': nrm(ks[11], (DEPTH, 2, NSA_CMP_HIDDEN, HEAD_DIM), NSA_CMP_HIDDEN ** -0.5),
        'ffn_w1': nrm(ks[12], (n_dense, D_MODEL, D_FF), D_MODEL ** -0.5),
        'ffn_w3': nrm(ks[13], (n_dense, D_MODEL, D_FF), D_MODEL ** -0.5),
        'ffn_w2': nrm(ks[14], (n_dense, D_FF, D_MODEL), D_FF ** -0.5 * DEEPNORM_BETA),
        'moe_router': nrm(ks[15], (n_moe, D_MODEL, N_EXPERTS), D_MODEL ** -0.5),
        'moe_w1': nrm(ks[16], (n_moe, N_EXPERTS, D_MODEL, D_FF), D_MODEL ** -0.5),
        'moe_w3': nrm(ks[17], (n_moe, N_EXPERTS, D_MODEL, D_FF), D_MODEL ** -0.5),
        'moe_w2': nrm(ks[18], (n_moe, N_EXPERTS, D_FF, D_MODEL), D_FF ** -0.5 * DEEPNORM_BETA),
        'ln_ffn_g': 1.0 + nrm(ks[19], (DEPTH, D_MODEL), 0.02),
        'ln_ffn_b': nrm(ks[20], (DEPTH, D_MODEL), 0.02),
        'ple_proj': nrm(ks[21], (DEPTH, PLE_DIM, D_MODEL), PLE_DIM ** -0.5),
        'ple_gate': nrm(ks[22], (DEPTH, D_MODEL, D_MODEL), D_MODEL ** -0.5),
    }


def reference(x, p, positions, w_in, w_out, ln_mix_g, ln_mix_b, diff_lambda, diff_gain,
              nsa_cmp_pos, nsa_cmp_w1, nsa_cmp_w2, ffn_w1, ffn_w3, ffn_w2,
              moe_router, moe_w1, moe_w3, moe_w2, ln_ffn_g, ln_ffn_b, ple_proj, ple_gate):
    h = x
    for i in range(DEPTH):
        mix = token_mixer(h, positions, w_in[i], w_out[i], diff_lambda[i], diff_gain[i],
                          nsa_cmp_pos[i], nsa_cmp_w1[i], nsa_cmp_w2[i], i)
        h = layer_norm(DEEPNORM_ALPHA * h + mix, ln_mix_g[i], ln_mix_b[i])
        if i % 2 == 0:
            f = swiglu(h, ffn_w1[i // 2], ffn_w3[i // 2], ffn_w2[i // 2])
        else:
            f = moe_swiglu(h, moe_router[i // 2], moe_w1[i // 2], moe_w3[i // 2], moe_w2[i // 2])
        h = layer_norm(DEEPNORM_ALPHA * h + f, ln_ffn_g[i], ln_ffn_b[i])
        h = h + jax.nn.sigmoid(h @ ple_gate[i]) * (p[i] @ ple_proj[i])
    return h
```

```python
import math
import numpy as np
import ml_dtypes
import concourse.bass as bass
import concourse.mybir as mybir
from contextlib import ExitStack
from concourse.bass_utils import run_bass_kernel_spmd

F32 = mybir.dt.float32
BF16 = mybir.dt.bfloat16
I32 = mybir.dt.int32
AF = mybir.ActivationFunctionType
ALU = mybir.AluOpType
AX = mybir.AxisListType

ENGS = ['pe', 'act', 'dve', 'pool', 'sp']
N_DMA_SEMS = 8
SEM_ROLL = 30000


class Tile:
    __slots__ = ('t', 'lastw', 'readers', 'name')

    def __init__(self, t, name=''):
        self.t = t
        self.lastw = None
        self.readers = {}
        self.name = name

    def __getitem__(self, idx):
        return self.t[idx]


class TileView(Tile):
    __slots__ = ('base',)

    def __init__(self, base_ap, name=''):
        Tile.__init__(self, None, name)
        self.base = base_ap

    def __getitem__(self, idx):
        return self.base[idx]


class Sched:
    def __init__(self, nc):
        self.nc = nc
        self.es = ExitStack()
        self.scopes = []
        self.q = {e: [] for e in ENGS}
        self.cnt = {}
        self.sems = {}
        self.epoch = {}
        for e in ['pe', 'act', 'dve', 'pool']:
            self.epoch[e] = 0
            self._newsem((e, 0))
        self.dma_rr = {}
        for e in ['sp', 'act', 'pool']:
            for i in range(N_DMA_SEMS):
                self.epoch[('d', e, i)] = 0
                self._newsem(('d', e, i, 0))
            self.dma_rr[e] = 0
        self.seen = {e: {} for e in ENGS}
        self.ntiles = 0
        self.ninstr = 0

    def _newsem(self, key):
        self.sems[key] = self.es.enter_context(self.nc.semaphore('s_' + '_'.join(str(k) for k in key)))
        self.cnt[key] = 0

    def _alloc_stack(self):
        return self.scopes[-1] if self.scopes else self.es

    def sbuf(self, shape, dt, name=None):
        self.ntiles += 1
        name = ('sb%d_' % self.ntiles) + (name or '')
        t = self._alloc_stack().enter_context(self.nc.sbuf_tensor(name, list(shape), dt))
        return Tile(t, name)

    def psum(self, shape, dt=F32, name=None):
        self.ntiles += 1
        name = ('ps%d_' % self.ntiles) + (name or '')
        t = self._alloc_stack().enter_context(self.nc.psum_tensor(name, list(shape), dt))
        return Tile(t, name)

    def dram(self, name, shape, dt, kind='Internal'):
        t = self.nc.dram_tensor(name, list(shape), dt, kind=kind)
        return Tile(t, name)

    def sub(self, tile, name=''):
        return Tile(tile.t, name or tile.name)

    def push_scope(self):
        self.scopes.append(ExitStack())

    def pop_scope(self):
        self.barrier()
        self.scopes.pop().close()

    def barrier(self):
        deps = [(k, v) for k, v in self.cnt.items() if v > 0]
        for e in ENGS:
            waits = self._need(e, deps)
            if waits:
                self.q[e].append(([(self.sems[k], v) for (k, v) in waits], None, None, 0))

    def _need(self, eng, deps):
        out = []
        seen = self.seen[eng]
        for (k, v) in deps:
            if seen.get(k, 0) < v:
                seen[k] = v
                out.append((k, v))
        return out

    def op(self, eng, fn, reads=(), writes=()):
        deps = []
        for t in reads:
            if t.lastw is not None:
                deps.append(t.lastw)
        for t in writes:
            if t.lastw is not None:
                deps.append(t.lastw)
            for rk, rv in t.readers.items():
                deps.append((rk, rv))
        if eng == 'pe':
            deps = [d for d in deps if d[0][0] != 'pe']
        waits = self._need(eng, deps)
        key = (eng, self.epoch[eng])
        if self.cnt[key] >= SEM_ROLL:
            self.epoch[eng] += 1
            key = (eng, self.epoch[eng])
            self._newsem(key)
        self.cnt[key] += 1
        v = self.cnt[key]
        self.q[eng].append(([(self.sems[k], val) for (k, val) in waits], fn, self.sems[key], 1))
        self.ninstr += 1
        for t in reads:
            t.readers[key] = v
        for t in writes:
            t.lastw = (key, v)
            t.readers = {}
        return v

    def dma(self, eng, out_ap, in_ap, reads=(), writes=(), **kw):
        i = self.dma_rr[eng]
        self.dma_rr[eng] = (i + 1) % N_DMA_SEMS
        slot = ('d', eng, i)
        k = ('d', eng, i, self.epoch[slot])
        deps = []
        if self.cnt[k] > 0:
            deps.append((k, self.cnt[k]))
        if self.cnt[k] >= SEM_ROLL:
            self.epoch[slot] += 1
            k = ('d', eng, i, self.epoch[slot])
            self._newsem(k)
        for t in reads:
            if t.lastw is not None:
                deps.append(t.lastw)
        for t in writes:
            if t.lastw is not None:
                deps.append(t.lastw)
            deps.extend(t.readers.items())
        waits = self._need(eng, deps)
        self.cnt[k] += 16
        v = self.cnt[k]
        fn = (lambda e, o=out_ap, i_=in_ap, kw=kw: e.dma_start(out=o, in_=i_, **kw))
        self.q[eng].append(([(self.sems[kk], val) for (kk, val) in waits], fn, self.sems[k], 16))
        self.ninstr += 1
        for t in reads:
            t.readers[k] = v
        for t in writes:
            t.lastw = (k, v)
            t.readers = {}
        return (k, v)

    def collective(self, kind, groups, in_t, in_ap, out_t, out_ap):
        eng = 'pool'
        i = self.dma_rr[eng]
        self.dma_rr[eng] = (i + 1) % N_DMA_SEMS
        slot = ('d', eng, i)
        k = ('d', eng, i, self.epoch[slot])
        deps = []
        if self.cnt[k] > 0:
            deps.append((k, self.cnt[k]))
        if in_t.lastw is not None:
            deps.append(in_t.lastw)
        if out_t.lastw is not None:
            deps.append(out_t.lastw)
        deps.extend(out_t.readers.items())
        waits = self._need(eng, deps)
        self.cnt[k] += 16
        v = self.cnt[k]
        fn = (lambda e: e.collective_compute(kind, ALU.bypass, replica_groups=groups, ins=[in_ap], outs=[out_ap]))
        self.q[eng].append(([(self.sems[kk], val) for (kk, val) in waits], fn, self.sems[k], 16))
        self.ninstr += 1
        in_t.readers[k] = v
        out_t.lastw = (k, v)
        out_t.readers = {}

    def finish(self):
        self.barrier()
        q = self.q

        def replay(e, lst):
            for (wl, fn, sem, inc) in lst:
                for (s, v) in wl:
                    e.wait_ge(s, v)
                if fn is not None:
                    fn(e).then_inc(sem, inc)

        with self.nc.Block() as block:
            @block.tensor
            def _(e):
                replay(e, q['pe'])

            @block.scalar
            def _(e):
                replay(e, q['act'])

            @block.vector
            def _(e):
                replay(e, q['dve'])

            @block.gpsimd
            def _(e):
                replay(e, q['pool'])

            @block.sync
            def _(e):
                replay(e, q['sp'])
        self.es.close()

    def mm(self, ot, oap, lt, lap, rt, rap, start=True, stop=True, skip=False):
        if skip:
            return self.op('pe', lambda e: e.matmul(oap, lap, rap, start=start, stop=stop, skip_group_check=True),
                           reads=[lt, rt], writes=[ot])
        return self.op('pe', lambda e: e.matmul(oap, lap, rap, start=start, stop=stop), reads=[lt, rt], writes=[ot])

    def tr(self, ot, oap, it, iap, idt, idap):
        return self.op('pe', lambda e: e.transpose(oap, iap, idap), reads=[it, idt], writes=[ot])

    def act(self, ot, oap, it, iap, func, reads=(), writes=(), **kw):
        return self.op('act', lambda e: e.activation(out=oap, in_=iap, func=func, **kw),
                       reads=[it] + list(reads), writes=[ot] + list(writes))

    def tt(self, eng, ot, oap, at, aap, bt, bap, op):
        return self.op(eng, lambda e: e.tensor_tensor(out=oap, in0=aap, in1=bap, op=op), reads=[at, bt], writes=[ot])

    def ts(self, eng, ot, oap, it, iap, s1, s2, op0, op1=None, reads=()):
        if op1 is None:
            return self.op(eng, lambda e: e.tensor_scalar(out=oap, in0=iap, scalar1=s1, scalar2=None, op0=op0),
                           reads=[it] + list(reads), writes=[ot])
        return self.op(eng, lambda e: e.tensor_scalar(out=oap, in0=iap, scalar1=s1, scalar2=s2, op0=op0, op1=op1),
                       reads=[it] + list(reads), writes=[ot])

    def stt(self, ot, oap, at, aap, scalar, bt, bap, op0, op1, reads=()):
        return self.op('dve', lambda e: e.scalar_tensor_tensor(out=oap, in0=aap, scalar=scalar, in1=bap, op0=op0, op1=op1),
                       reads=[at, bt] + list(reads), writes=[ot])

    def cp(self, eng, ot, oap, it, iap):
        if eng == 'act':
            return self.op('act', lambda e: e.activation(out=oap, in_=iap, func=AF.Copy), reads=[it], writes=[ot])
        return self.op(eng, lambda e: e.tensor_copy(out=oap, in_=iap), reads=[it], writes=[ot])

    def raw(self, eng, meth, reads=(), writes=(), **kw):
        return self.op(eng, lambda e, meth=meth, kw=kw: getattr(e, meth)(**kw), reads=reads, writes=writes)

    def memset(self, eng, t, ap, val):
        return self.op(eng, lambda e, ap=ap, val=val: e.memset(ap, val), writes=[t])


SEQ = 8192
DM = 1024
TO = 4096
NSLOT = 32
DFF = 3584
NFFC = 28
NEXP = 8
ALPHA = 4 ** 0.25
NCMP = 511
NEG = -30000.0
BIG = 1.0e30
TWO_PI = 2.0 * math.pi
TWO_PI_HI = float(np.float32(TWO_PI))
TWO_PI_LO = float(TWO_PI - np.float64(np.float32(TWO_PI)))
MAGIC = 12582912.0

OFF = dict(a_q=0, a_k=256, a_v=512, b_q=768, b_k=1024, b_v=1280, c_q=1536, c_k=1792, c_v=2048, d_q=2304,
           d_kc=2560, d_vc=2624, d_ks=2688, d_vs=2752, d_kw=2816, d_vw=2880, d_g=2944)


def _swap(idx, w):
    idx = np.asarray(idx)
    out = idx.copy().reshape(-1, w)
    h = w // 2
    out = np.concatenate([out[:, h:], out[:, :h]], axis=1)
    return out.reshape(-1)


def _colplan():
    ar = np.arange
    kA = OFF['a_k'] + ar(256)
    bk = OFF['b_k'] + ar(256)
    kB1 = bk.copy().reshape(4, 64)
    kB1[:, 32:] = -1
    kB2 = bk.copy().reshape(4, 64)
    kB2[:, :32] = -1
    kB1s = kB1.copy()
    kB1s[:, :32] = _swap(kB1[:, :32].reshape(-1), 32).reshape(4, 32)
    kB2s = kB2.copy()
    kB2s[:, 32:] = _swap(kB2[:, 32:].reshape(-1), 32).reshape(4, 32)
    kC = OFF['c_k'] + ar(256)
    ks = OFF['d_ks'] + ar(64)
    kw = OFF['d_kw'] + ar(64)
    ksks = np.concatenate([ks, ks])
    kwkw = np.concatenate([kw, kw])
    kcvc = np.concatenate([OFF['d_kc'] + ar(64), OFF['d_vc'] + ar(64)])
    kf = np.concatenate([kA, kB1.reshape(-1), kB1s.reshape(-1), kB2.reshape(-1), kB2s.reshape(-1),
                         kC, _swap(kC, 64), ksks, _swap(ksks, 64), kwkw, _swap(kwkw, 64), kcvc])
    qA = OFF['a_q'] + ar(256)
    qB = OFF['b_q'] + ar(256)
    qC = OFF['c_q'] + ar(256)
    qD = OFF['d_q'] + ar(256)
    qf = np.concatenate([qA, qB, _swap(qB, 32), qC, _swap(qC, 64), qD, _swap(qD, 64)])
    kt = np.concatenate([OFF['a_v'] + ar(256), OFF['b_v'] + ar(256), OFF['c_v'] + ar(256),
                         OFF['d_vs'] + ar(64), OFF['d_vw'] + ar(64)])
    qt = OFF['d_g'] + ar(12)
    return kf, qf, kt, qt


KF_IDX, QF_IDX, KT_IDX, QT_IDX = _colplan()
NKF = len(KF_IDX) // 128
NQF = len(QF_IDX) // 128
KF_SRC = dict(kA=(0, 1), kB1=(2, 3), kB1s=(4, 5), kB2=(6, 7), kB2s=(8, 9), kC=(10, 11), kCs=(12, 13),
              ks=(14,), kss=(15,), kw=(16,), kws=(17,), kcvc=(18,))
KF_DST = dict(kA=(0, 1), kB1=(2, 3), kB2=(4, 5), kC=(6, 7), ks=(8,), kw=(9,), kcvc=(10,))
NKFD = 11
QF_SRC = dict(qA=(0, 1), qB=(2, 3), qBs=(4, 5), qC=(6, 7), qCs=(8, 9), qD=(10, 11), qDs=(12, 13))
QF_DST = dict(qA=(0, 1), qB=(2, 3), qC=(4, 5), qD=(6, 7), qDr=(8, 9))
NQFD = 10
NVA = 14 * 65


def _gather_cols(w, idx):
    out = np.zeros((w.shape[0], len(idx)), dtype=w.dtype)
    m = idx >= 0
    out[:, m] = w[:, idx[m]]
    return out


def _host_consts(parity):
    c = {}
    c['ident'] = np.eye(128, dtype=np.float32)
    j = np.arange(128)[:, None]
    i = np.arange(128)[None, :]
    le = (j <= i).astype(np.float32)
    lt = (j < i).astype(np.float32)
    gt = (j > i).astype(np.float32)
    one = np.ones((128, 128), np.float32)
    zero = np.zeros((128, 128), np.float32)
    if parity == 0:
        ms = [zero, le, zero, lt, gt, one]
    else:
        ms = [le, one, lt, one, zero, gt]
    c['masks'] = np.stack([np.tile(m, (1, 4)) for m in ms], 0).astype(np.float32)
    jj = np.arange(128)[:, None]
    ss = np.arange(128)[None, :]
    c['negtri'] = -(jj >= ss).astype(np.float32)
    c['negones'] = -np.ones((128, 128), np.float32)
    r = np.arange(128)
    tab = np.zeros((128, 16), np.float32)
    tab[:, 0] = 10000.0 ** (-(r % 16) / 16.0)
    tab[:, 1] = 10000.0 ** (-(r % 32) / 32.0)
    tab[:, 2] = np.where((r % 32) < 16, -1.0, 1.0)
    tab[:, 3] = np.where((r % 64) < 32, -1.0, 1.0)
    for ci in range(4):
        tab[:, 4 + ci] = 16.0 * (128 * ci + r) + 31.0
    c['ptab'] = tab
    em = np.zeros((32, 64, 128), np.float32)
    for kb in range(64):
        em[kb // 2, kb, :] = 1.0
    c['emoba'] = em.reshape(32, 64 * 128)
    es = np.zeros((128, 64, 128), np.float32)
    for kb in range(64):
        es[2 * kb, kb, :64] = 1.0
        es[2 * kb + 1, kb, 64:] = 1.0
    c['esel'] = es.reshape(128, 64 * 128)
    n = np.arange(512)[:, None]
    m = np.arange(128)[None, :]
    ov = ((16 * n < 64 * m + 64) & (16 * n + 32 > 64 * m)).astype(np.float32)
    ov[511, :] = 0.0
    c['overlap'] = ov.reshape(4, 128, 128).transpose(1, 0, 2).reshape(128, 512).copy()
    tq = np.zeros((NSLOT, 128), np.float32)
    selm = np.zeros((128, NSLOT, 128), np.float32)
    for s in range(NSLOT):
        qb = 2 * s + parity
        t = qb * 128 + np.arange(128)
        tq[s] = t
        qblk = t // 64
        mm_ = np.arange(128)[None, :]
        forced = (mm_ == 0) | (mm_ == qblk[:, None]) | (mm_ == qblk[:, None] - 1)
        causal = mm_ <= qblk[:, None]
        selm[:, s, :] = np.where(forced, BIG, np.where(causal, 0.0, -BIG))
    c['tq'] = tq.reshape(1, NSLOT * 128)
    c['selm'] = selm.reshape(128, NSLOT * 128)
    return c


def emit_pass(G, P, cfg=None):
    cfg = cfg or {}
    slots = cfg.get('slots', list(range(NSLOT)))
    mixers = cfg.get('mixers', 'ABCD')
    do_tail = cfg.get('tail', True)
    dbg = False
    layer = P['layer']
    moe = P['moe']
    sfx = P['sfx']
    lam_init = 0.8 - 0.6 * math.exp(-0.3 * layer)
    nc, S = G['nc'], G['S']
    hk_rows, hq_rows, posk, posq, p_rows, out_rows = P['hk'], P['hq'], P['posk'], P['posq'], P['prow'], P['out']
    wkf, wqf, wkt, wqt, wout, vecs = P['wkf'], P['wqf'], P['wkt'], P['wqt'], P['wout'], P['vecs']
    dlam, dgain, cpos, cw1, cw2 = P['dlam'], P['dgain'], P['cpos'], P['cw1'], P['cw2']
    fw1, fw3, fw2, wrt, plep, pleg = P['fw1'], P['fw3'], P['fw2'], P['wrt'], P['plep'], P['pleg']
    ne = NEXP if moe else 1
    c_masks, c_tq, c_selm = P['c_masks'], P['c_tq'], P['c_selm']
    c_negtri, c_negones, c_emoba, c_esel, c_overlap = G['c_negtri'], G['c_negones'], G['c_emoba'], G['c_esel'], G['c_overlap']
    KF, VA, do_k = P['KF'], P['VA'], P['do_k']
    QF = S.dram('QF' + sfx, [NQFD, 128, TO], BF16)
    GT = S.dram('GT' + sfx, [TO, 12], F32)
    MIX = S.dram('MIX' + sfx, [TO, DM], F32)
    ident, ptab, kmean, masks = G['ident'], G['ptab'], G['kmean'], G['masks']
    P2, PS, SB2 = G['P2'], G['PS'], G['SB2']
    S.dma('pool', masks[:], c_masks[:].rearrange('m p f -> p m f'), writes=[masks])
    M_HI_LE, M_LO_LE, M_HI_LT, M_LO_LT, M_W4, M_W3 = range(6)
    if cfg.get('zfill'):
        S.push_scope()
        ztile = S.sbuf([128, DM], F32, 'ztile')
        S.memset('pool', ztile, ztile[:], 0.0)
        for tb in range(NSLOT):
            S.dma('sp', MIX[tb * 128:(tb + 1) * 128, :], ztile[:], reads=[ztile], writes=[MIX])
        S.pop_scope()

    def scol(h):
        return (h % 2) * 512 + (h // 2) * 128

    def pcol(h):
        return (h % 2) * 256 + (h // 2) * 128

    def v2(t_):
        return t_[:].rearrange('p (b c) -> p b c', b=2)[:, :, 0:256]

    rr = [0]

    def alt(engs=('act', 'dve')):
        rr[0] += 1
        return engs[rr[0] % len(engs)]

    S.push_scope()
    wk_sb = S.sbuf([128, 8, NKF * 128], BF16, 'wk_sb')
    wq_sb = S.sbuf([128, 8, NQF * 128], BF16, 'wq_sb')
    wkt_sb = S.sbuf([128, 8, 896], BF16, 'wkt_sb')
    wqt_sb = S.sbuf([128, 8, 12], BF16, 'wqt_sb')
    for kc in range(8):
        S.dma('pool', wk_sb[:, kc, :], wkf[kc * 128:(kc + 1) * 128, :], writes=[wk_sb])
        S.dma('pool', wq_sb[:, kc, :], wqf[kc * 128:(kc + 1) * 128, :], writes=[wq_sb])
    S.dma('pool', wkt_sb[:], wkt[:].rearrange('(k p) f -> p k f', p=128), writes=[wkt_sb])
    S.dma('pool', wqt_sb[:], wqt[:].rearrange('(k p) f -> p k f', p=128), writes=[wqt_sb])

    hrow = [S.sbuf([128, DM], F32, 'hrow%d' % i) for i in range(2)]
    hT = [S.sbuf([128, 8, 512], BF16, 'hT%d' % i) for i in range(2)]
    posi = S.sbuf([128, 512], I32, 'posi')
    posf = S.sbuf([128, 512], F32, 'posf')
    rtmp = [S.sbuf([128, 512], F32, 'rtmp%d' % i) for i in range(3)]
    rope = {k: S.sbuf([128, 512], F32, 'rope_' + k) for k in ('cosB', 'sinB', 'cosCD', 'sinCD')}
    ftile = [S.sbuf([128, 512], BF16, 'ftile%d' % i) for i in range(4)]
    ft32 = [S.sbuf([128, 512], F32, 'ft32_%d' % i) for i in range(2)]
    vaug = [S.sbuf([128, 14, 65], BF16, 'vaug%d' % i) for i in range(2)]
    gsb = [S.sbuf([128, 12], F32, 'gsb%d' % i) for i in range(2)]
    kms = S.sbuf([128, 2, 32], F32, 'kms')
    for v_ in vaug:
        S.memset('pool', v_, v_[:], 1.0)

    def build_rope(pos_dram, c0):
        pt_, pap_, three_ = pos_dram(c0)
        S.dma('sp', posi[:].rearrange('p (a j) -> p a j', a=4) if three_ else posi[:], pap_, reads=[pt_], writes=[posi])
        S.cp('dve', posf, posf[:], posi, posi[:])
        for (tabcol, sgncol, ck, sk) in ((0, 2, 'cosB', 'sinB'), (1, 3, 'cosCD', 'sinCD')):
            ang, t1, t2 = rtmp
            S.ts('dve', ang, ang[:], posf, posf[:], ptab[:, tabcol:tabcol + 1], None, ALU.mult, reads=[ptab])
            for (shift, dst, sgn) in ((0.0, rope[sk], True), (math.pi / 2, rope[ck], False)):
                S.ts('dve', t1, t1[:], ang, ang[:], 1.0 / TWO_PI, shift / TWO_PI + MAGIC, ALU.mult, ALU.add)
                S.ts('dve', t1, t1[:], t1, t1[:], -MAGIC, None, ALU.add)
                S.stt(t2, t2[:], t1, t1[:], -TWO_PI_HI, ang, ang[:], ALU.mult, ALU.add)
                S.ts('dve', t2, t2[:], t2, t2[:], shift, None, ALU.add)
                S.stt(t2, t2[:], t1, t1[:], -TWO_PI_LO, t2, t2[:], ALU.mult, ALU.add)
                S.ts('dve', t1, t1[:], t2, t2[:], math.pi, -TWO_PI, ALU.is_gt, ALU.mult)
                S.tt('dve', t2, t2[:], t2, t2[:], t1, t1[:], ALU.add)
                S.ts('dve', t1, t1[:], t2, t2[:], -math.pi, TWO_PI, ALU.is_lt, ALU.mult)
                S.tt('dve', t2, t2[:], t2, t2[:], t1, t1[:], ALU.add)
                S.ts('dve', t2, t2[:], t2, t2[:], 3.14159, -3.14159, ALU.min, ALU.max)
                if sgn:
                    S.act(dst, dst[:], t2, t2[:], AF.Sin, reads=[ptab], scale=ptab[:, sgncol:sgncol + 1])
                else:
                    S.act(dst, dst[:], t2, t2[:], AF.Sin)

    def load_hT(src, r0, buf):
        for tb in range(4):
            hr = hrow[tb % 2]
            st_, sap_ = src(r0 + tb * 128)
            S.dma('sp', hr[:], sap_, reads=[st_], writes=[hr])
            for half in range(2):
                bank = PS[6 + half]
                for k4 in range(4):
                    kc = half * 4 + k4
                    S.tr(bank, bank[:, k4 * 128:(k4 + 1) * 128], hr, hr[:, kc * 128:(kc + 1) * 128], ident, ident[:])
                S.cp(alt(), buf, buf[:, half * 4:half * 4 + 4, tb * 128:(tb + 1) * 128],
                     bank, bank[:].rearrange('p (k t) -> p k t', k=4))

    def proj_tile(w_sb, src_tile, buf, bank):
        for kc in range(8):
            S.mm(bank, bank[:], w_sb, w_sb[:, kc, src_tile * 128:(src_tile + 1) * 128], buf, buf[:, kc, :],
                 start=(kc == 0), stop=(kc == 7))

    fcnt = [0]

    def emit_plain(w_sb, src, buf, dst_dram, dst_tile, c0, scale=None):
        bank = PS[fcnt[0] % 4]
        ft = ftile[fcnt[0] % 4]
        fcnt[0] += 1
        proj_tile(w_sb, src, buf, bank)
        if scale is None:
            S.cp(alt(), ft, ft[:], bank, bank[:])
        else:
            S.act(ft, ft[:], bank, bank[:], AF.Copy, scale=scale)
        S.dma('sp', dst_dram[dst_tile, :, c0:c0 + 512], ft[:], reads=[ft], writes=[dst_dram])
        return ft

    def emit_rope(w_sb, src, src_s, buf, dst_dram, dst_tile, c0, cosk, sink, scale=None, km_slot=None):
        i0 = fcnt[0] % 2
        bank = PS[i0 * 2]
        bank_s = PS[i0 * 2 + 1]
        ft = ftile[fcnt[0] % 4]
        t32 = ft32[i0]
        fcnt[0] += 1
        proj_tile(w_sb, src, buf, bank)
        proj_tile(w_sb, src_s, buf, bank_s)
        S.tt('dve', t32, t32[:], bank, bank[:], rope[cosk], rope[cosk][:], ALU.mult)
        S.tt('dve', bank_s, bank_s[:], bank_s, bank_s[:], rope[sink], rope[sink][:], ALU.mult)
        if km_slot is not None:
            S.tt('dve', t32, t32[:], t32, t32[:], bank_s, bank_s[:], ALU.add)
            S.raw('dve', 'tensor_reduce', reads=[t32], writes=[kms],
                  out=kms[:, km_slot[0], km_slot[1]:km_slot[1] + 2], in_=t32[:].rearrange('p (b k) -> p b k', b=2),
                  axis=AX.X, op=ALU.add)
            S.cp('act', ft, ft[:], t32, t32[:])
        elif scale is None:
            S.tt('dve', ft, ft[:], t32, t32[:], bank_s, bank_s[:], ALU.add)
        else:
            S.tt('dve', t32, t32[:], t32, t32[:], bank_s, bank_s[:], ALU.add)
            S.act(ft, ft[:], t32, t32[:], AF.Copy, scale=scale)
        S.dma('sp', dst_dram[dst_tile, :, c0:c0 + 512], ft[:], reads=[ft], writes=[dst_dram])

    for ch in (range(SEQ // 512) if do_k else []):
        c0 = ch * 512
        buf = hT[ch % 2]
        load_hT(hk_rows, c0, buf)
        build_rope(posk, c0)
        for t in range(2):
            emit_plain(wk_sb, KF_SRC['kA'][t], buf, KF, KF_DST['kA'][t], c0)
        for t in range(2):
            emit_rope(wk_sb, KF_SRC['kB1'][t], KF_SRC['kB1s'][t], buf, KF, KF_DST['kB1'][t], c0, 'cosB', 'sinB')
            emit_rope(wk_sb, KF_SRC['kB2'][t], KF_SRC['kB2s'][t], buf, KF, KF_DST['kB2'][t], c0, 'cosB', 'sinB')
            emit_rope(wk_sb, KF_SRC['kC'][t], KF_SRC['kCs'][t], buf, KF, KF_DST['kC'][t], c0, 'cosCD', 'sinCD',
                      km_slot=(t, 2 * ch))
        emit_rope(wk_sb, KF_SRC['ks'][0], KF_SRC['kss'][0], buf, KF, KF_DST['ks'][0], c0, 'cosCD', 'sinCD')
        emit_rope(wk_sb, KF_SRC['kw'][0], KF_SRC['kws'][0], buf, KF, KF_DST['kw'][0], c0, 'cosCD', 'sinCD')
        emit_plain(wk_sb, KF_SRC['kcvc'][0], buf, KF, KF_DST['kcvc'][0], c0)
        for tb in range(4):
            va = vaug[tb % 2]
            b0, b1 = PS[4], PS[5]
            for kc in range(8):
                S.mm(b0, b0[:], buf, buf[:, kc, tb * 128:(tb + 1) * 128], wkt_sb, wkt_sb[:, kc, 0:512],
                     start=(kc == 0), stop=(kc == 7))
            for kc in range(8):
                S.mm(b1, b1[:, 0:384], buf, buf[:, kc, tb * 128:(tb + 1) * 128], wkt_sb, wkt_sb[:, kc, 512:896],
                     start=(kc == 0), stop=(kc == 7))
            S.cp('act', va, va[:, 0:8, 0:64], b0, b0[:].rearrange('p (h d) -> p h d', h=8))
            S.cp('dve', va, va[:, 8:14, 0:64], b1, b1[:, 0:384].rearrange('p (h d) -> p h d', h=6))
            r0 = c0 + tb * 128
            S.dma('sp', VA[r0:r0 + 128, :], va[:].rearrange('p h d -> p (h d)'), reads=[va], writes=[VA])
    if do_k:
        S.ts('dve', kms, kms[:], kms, kms[:], 1.0 / 256.0, None, ALU.mult)
        S.cp('dve', kmean, kmean[:], kms, kms[:])

    for ch in range(TO // 512):
        c0 = ch * 512
        buf = hT[ch % 2]
        load_hT(hq_rows, c0, buf)
        build_rope(posq, c0)
        for t in range(2):
            emit_plain(wq_sb, QF_SRC['qA'][t], buf, QF, QF_DST['qA'][t], c0, scale=0.125)
            emit_rope(wq_sb, QF_SRC['qB'][t], QF_SRC['qBs'][t], buf, QF, QF_DST['qB'][t], c0, 'cosB', 'sinB',
                      scale=32 ** -0.5)
            emit_rope(wq_sb, QF_SRC['qC'][t], QF_SRC['qCs'][t], buf, QF, QF_DST['qC'][t], c0, 'cosCD', 'sinCD',
                      scale=0.125)
            emit_plain(wq_sb, QF_SRC['qD'][t], buf, QF, QF_DST['qD'][t], c0, scale=0.125)
            emit_rope(wq_sb, QF_SRC['qD'][t], QF_SRC['qDs'][t], buf, QF, QF_DST['qDr'][t], c0, 'cosCD', 'sinCD',
                      scale=0.125)
        for tb in range(4):
            g = gsb[tb % 2]
            b0 = PS[4 + tb % 2]
            for kc in range(8):
                S.mm(b0, b0[:, 0:12], buf, buf[:, kc, tb * 128:(tb + 1) * 128], wqt_sb, wqt_sb[:, kc, :],
                     start=(kc == 0), stop=(kc == 7))
            S.act(g, g[:], b0, b0[:, 0:12], AF.Sigmoid)
            r0 = c0 + tb * 128
            S.dma('sp', GT[r0:r0 + 128, :], g[:], reads=[g], writes=[GT])
    S.pop_scope()
    def view4(bank):
        return bank[:, 0:260].rearrange('p (h d) -> p h d', h=4)

    def load_kt(dst, tiles):
        for i_, t_ in enumerate(tiles):
            for hf in range(2):
                S.dma('sp', dst[:, i_, hf * 4096:(hf + 1) * 4096], KF[t_, :, hf * 4096:(hf + 1) * 4096], writes=[dst])

    def load_q(dst, tiles):
        for i_, t_ in enumerate(tiles):
            S.dma('sp', dst[:, i_, :], QF[t_, :, :], writes=[dst])

    def load_v(dst, s0, ns):
        for q4 in range(16):
            S.dma('sp', dst[:, q4 * 4:(q4 + 1) * 4, :],
                  VA[q4 * 512:(q4 + 1) * 512, s0 * 65:(s0 + ns) * 65].rearrange('(kb p) f -> p kb f', p=128),
                  writes=[dst])

    sbk = [0]
    ptb = [0]

    def attn_step(slot, kb, kt, kt_tile_of_h, q, pts, vfn, obank, first, last, mask=None, bias=None, shared_k=False):
        sb = SB2[sbk[0] % 2]
        sbk[0] += 1
        pt = pts[ptb[0] % len(pts)]
        ptb[0] += 1
        for h in (0, 2, 1, 3):
            b = (h % 2) * 64
            S.mm(sb, sb[:, scol(h):scol(h) + 128], kt, kt[b:b + 64, kt_tile_of_h(h), kb * 128:(kb + 1) * 128],
                 q, q[b:b + 64, h // 2, slot * 128:(slot + 1) * 128], start=(h < 2), stop=(bias is None and h >= 2),
                 skip=True)
        if bias is not None:
            bt_, bap_fn, et_, eap = bias
            for par in range(2):
                S.mm(sb, sb[:, par * 512:par * 512 + 256], et_, eap, bt_, bap_fn(par), start=False, stop=True, skip=True)
        S.act(pt, pt[:].rearrange('p (b c) -> p b c', b=2), sb, v2(sb), AF.Exp)
        if mask is not None:
            S.tt('dve', pt, pt[:], pt, pt[:], masks, masks[:, mask, :], ALU.mult)
        for h in range(4):
            vt, vap = vfn(h, kb)
            S.mm(obank, obank[:, h * 65:(h + 1) * 65], pt, pt[:, pcol(h):pcol(h) + 128], vt, vap,
                 start=(first and h == 0), stop=last, skip=True)

    def causal_mask(slot, kb, strict=False):
        if kb == 2 * slot + 1:
            return M_HI_LT if strict else M_HI_LE
        if kb == 2 * slot:
            return M_LO_LT if strict else M_LO_LE
        return None

    def store_mix(omix, slot, mi):
        S.dma('sp', MIX[slot * 128:(slot + 1) * 128, mi * 256:(mi + 1) * 256], omix[:], reads=[omix], writes=[MIX])

    if 'B' in mixers:
        S.push_scope()
        k1 = S.sbuf([128, 2, SEQ], BF16, 'k1')
        k2 = S.sbuf([128, 2, SEQ], BF16, 'k2')
        qb_ = S.sbuf([128, 2, TO], BF16, 'qB')
        vb = S.sbuf([128, 64, 260], BF16, 'vB')
        load_kt(k1, KF_DST['kB1'])
        load_kt(k2, KF_DST['kB2'])
        load_q(qb_, QF_DST['qB'])
        load_v(vb, 4, 4)
        pts = [S.sbuf([128, 512], BF16, 'ptB%d' % i) for i in range(3)]
        lamt = S.sbuf([128, 128], F32, 'lamt')
        S.dma('sp', lamt[:], dlam[:].partition_broadcast(128), writes=[lamt])
        gainb = S.sbuf([128, 64], F32, 'gainb')
        S.dma('sp', gainb[:], dgain[:].partition_broadcast(128), writes=[gainb])
        S.ts('dve', gainb, gainb[:], gainb, gainb[:], 1.0 - lam_init, None, ALU.mult)
        lp = S.sbuf([128, 64], F32, 'lp')
        ls = S.sbuf([128, 4], F32, 'ls')
        S.tt('dve', lp, lp[:, 0:32], lamt, lamt[:, 0:32], lamt, lamt[:, 32:64], ALU.mult)
        S.tt('dve', lp, lp[:, 32:64], lamt, lamt[:, 64:96], lamt, lamt[:, 96:128], ALU.mult)
        S.raw('dve', 'tensor_reduce', reads=[lp], writes=[ls], out=ls[:, 0:2],
              in_=lp[:].rearrange('p (a b) -> p a b', a=2), axis=AX.X, op=ALU.add)
        S.act(ls, ls[:, 0:2], ls, ls[:, 0:2], AF.Exp)
        S.tt('dve', ls, ls[:, 2:3], ls, ls[:, 1:2], ls, ls[:, 0:1], ALU.subtract)
        S.ts('dve', ls, ls[:, 2:3], ls, ls[:, 2:3], -lam_init, None, ALU.add)
        rd = S.sbuf([128, 8], F32, 'rdB')
        ob = S.sbuf([128, 4, 64], F32, 'obB')
        sq = S.sbuf([128, 4, 64], F32, 'sqB')
        ss = S.sbuf([128, 4], F32, 'ssB')
        omixs = [S.sbuf([128, 256], F32, 'omixB%d' % i) for i in range(2)]
        for si, slot in enumerate(slots):
            nkb = 2 * slot + 2
            for c_, kt_ in enumerate((k1, k2)):
                obank = PS[4 + c_]
                for kb in range(nkb):
                    attn_step(slot, kb, kt_, lambda h: h // 2, qb_, pts, lambda h, kb: (vb, vb[:, kb, h * 65:(h + 1) * 65]),
                              obank, kb == 0, kb == nkb - 1, mask=causal_mask(slot, kb))
            o1, o2 = view4(PS[4]), view4(PS[5])
            S.raw('dve', 'reciprocal', reads=[PS[4]], writes=[rd], out=rd[:, 0:4], in_=o1[:, :, 64])
            S.raw('dve', 'reciprocal', reads=[PS[5]], writes=[rd], out=rd[:, 4:8], in_=o2[:, :, 64])
            S.ts('dve', rd, rd[:, 4:8], rd, rd[:, 4:8], ls[:, 2:3], None, ALU.mult, reads=[ls])
            omix = omixs[si % 2]
            for h in range(4):
                S.ts('dve', ob, ob[:, h, :], PS[4], o1[:, h, 0:64], rd[:, h:h + 1], None, ALU.mult, reads=[rd])
                S.stt(ob, ob[:, h, :], PS[5], o2[:, h, 0:64], rd[:, 4 + h:5 + h], ob, ob[:, h, :], ALU.mult, ALU.add, reads=[rd])
            S.tt('dve', sq, sq[:], ob, ob[:], ob, ob[:], ALU.mult)
            S.raw('dve', 'tensor_reduce', reads=[sq], writes=[ss], out=ss[:], in_=sq[:], axis=AX.X, op=ALU.add)
            S.ts('dve', ss, ss[:], ss, ss[:], 1.0 / 64.0, 1e-5, ALU.mult, ALU.add)
            S.act(ss, ss[:], ss, ss[:], AF.Sqrt)
            S.raw('dve', 'reciprocal', reads=[ss], writes=[ss], out=ss[:], in_=ss[:])
            for h in range(4):
                S.stt(omix, omix[:, h * 64:(h + 1) * 64], ob, ob[:, h, :], ss[:, h:h + 1], gainb, gainb[:],
                      ALU.mult, ALU.mult, reads=[ss])
            store_mix(omix, slot, 1)
        S.pop_scope()

    if 'C' in mixers:
        S.push_scope()
        kc_ = S.sbuf([128, 2, SEQ], BF16, 'kC')
        qc_ = S.sbuf([128, 2, TO], BF16, 'qC')
        vc_ = S.sbuf([128, 64, 260], BF16, 'vC')
        load_kt(kc_, KF_DST['kC'])
        load_q(qc_, QF_DST['qC'])
        load_v(vc_, 8, 4)
        emoba = S.sbuf([128, 64 * 128], BF16, 'emoba')
        S.memset('pool', emoba, emoba[:], 0.0)
        S.dma('pool', emoba[0:32, :], c_emoba[:], writes=[emoba])
        pts = [S.sbuf([128, 512], BF16, 'ptC%d' % i) for i in range(3)]
        gbuf = S.sbuf([128, 4, 32], F32, 'gbuf')
        top8 = S.sbuf([128, 4, 8], F32, 'top8')
        selb = S.sbuf([128, 4, 32], F32, 'selb')
        biasT = S.sbuf([128, 512], BF16, 'biasT')
        S.memset('pool', biasT, biasT[:], 0.0)
        rd = S.sbuf([128, 4], F32, 'rdC')
        omixs = [S.sbuf([128, 256], F32, 'omixC%d' % i) for i in range(2)]
        for si, slot in enumerate(slots):
            own = slot
            nkb = 2 * slot + 2
            if own > 0:
                for h in range(4):
                    b = (h % 2) * 64
                    g = PS[6 + h % 2]
                    S.mm(g, g[:, (h // 2) * 32:(h // 2 + 1) * 32], qc_, qc_[b:b + 64, h // 2, slot * 128:(slot + 1) * 128],
                         kmean, kmean[b:b + 64, h // 2, :], start=True, stop=True)
                S.memset('pool', gbuf, gbuf[:], -BIG)
                for par in range(2):
                    g = PS[6 + par]
                    S.cp('dve', gbuf, gbuf[:, par * 2:par * 2 + 2, 0:own], g,
                         g[:, 0:64].rearrange('p (h n) -> p h n', h=2)[:, :, 0:own])
                for gi in range(4):
                    S.raw('dve', 'max', reads=[gbuf], writes=[top8], out=top8[:, gi, :], in_=gbuf[:, gi, :])
                for gi in range(4):
                    S.ts('dve', selb, selb[:, gi, :], gbuf, gbuf[:, gi, :], top8[:, gi, 2:3], 1.0, ALU.is_ge, ALU.subtract,
                         reads=[top8])
                S.ts('dve', selb, selb[:], selb, selb[:], -NEG, None, ALU.mult)
                tb_ = PS[7]
                for gi in range(4):
                    S.tr(tb_, tb_[0:32, gi * 128:(gi + 1) * 128], selb, selb[:, gi, :], ident, ident[:])
                S.cp('act', biasT, biasT[0:32, :], tb_, tb_[0:32, :])
            obank = PS[4 + si % 2]
            for kb in range(nkb):
                bias = None
                if kb < 2 * own:
                    bias = (biasT, lambda par: biasT[:, par * 256:(par + 1) * 256], emoba, emoba[:, kb * 128:(kb + 1) * 128])
                attn_step(slot, kb, kc_, lambda h: h // 2, qc_, pts, lambda h, kb: (vc_, vc_[:, kb, h * 65:(h + 1) * 65]),
                          obank, kb == 0, kb == nkb - 1, mask=causal_mask(slot, kb), bias=bias)
            o1 = view4(obank)
            S.raw('dve', 'reciprocal', reads=[obank], writes=[rd], out=rd[:], in_=o1[:, :, 64])
            omix = omixs[si % 2]
            for h in range(4):
                S.ts('dve', omix, omix[:, h * 64:(h + 1) * 64], obank, o1[:, h, 0:64], rd[:, h:h + 1], None, ALU.mult,
                     reads=[rd])
            store_mix(omix, slot, 2)
        S.pop_scope()

    if 'A' in mixers:
        S.push_scope()
        ka = S.sbuf([128, 2, SEQ], BF16, 'kA')
        qa = S.sbuf([128, 2, TO], BF16, 'qA')
        va_ = S.sbuf([128, 64, 260], BF16, 'vA')
        load_kt(ka, KF_DST['kA'])
        load_q(qa, QF_DST['qA'])
        load_v(va_, 0, 4)
        negtri = S.sbuf([128, 128], F32, 'negtri')
        negones = S.sbuf([128, 128], F32, 'negones')
        S.dma('sp', negtri[:], c_negtri[:], writes=[negtri])
        S.dma('sp', negones[:], c_negones[:], writes=[negones])
        pts = [S.sbuf([128, 512], BF16, 'ptA%d' % i) for i in range(3)]
        ee = [S.sbuf([128, 512], F32, 'eeA%d' % i) for i in range(2)]
        ll = [S.sbuf([128, 512], F32, 'llA%d' % i) for i in range(2)]
        lacc = [S.sbuf([128, 512], F32, 'laccA%d' % i) for i in range(2)]
        omixs = [S.sbuf([128, 256], F32, 'omixA%d' % i) for i in range(2)]
        stepi = 0
        for si, slot in enumerate(slots):
            nkb = 2 * slot + 2
            obank = PS[4 + si % 2]
            la_cur = None
            for idx, kb in enumerate(range(nkb - 1, -1, -1)):
                zb = SB2[0]
                ab = SB2[1]
                e_ = ee[stepi % 2]
                l_ = ll[stepi % 2]
                pt = pts[stepi % 3]
                stepi += 1
                for h in (0, 2, 1, 3):
                    b = (h % 2) * 64
                    S.mm(zb, zb[:, scol(h):scol(h) + 128], ka, ka[b:b + 64, h // 2, kb * 128:(kb + 1) * 128],
                         qa, qa[b:b + 64, h // 2, slot * 128:(slot + 1) * 128], start=True, stop=True)
                S.act(e_, e_[:].rearrange('p (b c) -> p b c', b=2), zb, v2(zb), AF.Exp)
                S.act(l_, l_[:], e_, e_[:], AF.Ln, bias=1.0)
                m = causal_mask(slot, kb, strict=True)
                if m is not None:
                    S.tt('dve', l_, l_[:], l_, l_[:], masks, masks[:, m, :], ALU.mult)
                for h in (0, 2, 1, 3):
                    b = (h % 2) * 64
                    S.mm(ab, ab[:, scol(h):scol(h) + 128], ka, ka[b:b + 64, h // 2, kb * 128:(kb + 1) * 128],
                         qa, qa[b:b + 64, h // 2, slot * 128:(slot + 1) * 128], start=(h < 2), stop=False, skip=True)
                for par in range(2):
                    S.mm(ab, ab[:, par * 512:par * 512 + 256], negtri, negtri[:], l_, l_[:, par * 256:(par + 1) * 256],
                         start=False, stop=(la_cur is None), skip=True)
                if la_cur is not None:
                    for par in range(2):
                        S.mm(ab, ab[:, par * 512:par * 512 + 256], negones, negones[:], la_cur,
                             la_cur[:, par * 256:(par + 1) * 256], start=False, stop=True, skip=True)
                S.act(pt, pt[:].rearrange('p (b c) -> p b c', b=2), ab, v2(ab), AF.Exp)
                if m is not None:
                    S.tt('dve', pt, pt[:], pt, pt[:], masks, masks[:, m, :], ALU.mult)
                for h in range(4):
                    S.mm(obank, obank[:, h * 65:(h + 1) * 65], pt, pt[:, pcol(h):pcol(h) + 128],
                         va_, va_[:, kb, h * 65:(h + 1) * 65], start=(idx == 0 and h == 0), stop=(kb == 0), skip=True)
                if kb > 0:
                    if la_cur is None:
                        la_new = lacc[0]
                        S.cp('pool', la_new, la_new[:], l_, l_[:])
                    else:
                        la_new = lacc[1] if la_cur is lacc[0] else lacc[0]
                        S.tt('pool', la_new, la_new[:], la_cur, la_cur[:], l_, l_[:], ALU.add)
                    la_cur = la_new
            omix = omixs[si % 2]
            o1 = view4(obank)
            S.cp('dve', omix, omix[:].rearrange('p (h d) -> p h d', h=4), obank, o1[:, :, 0:64])
            store_mix(omix, slot, 0)
        S.pop_scope()
    if 'D' in mixers:
        S.push_scope()
        ktd = S.sbuf([128, 2, SEQ], BF16, 'ktD')
        load_kt(ktd, (KF_DST['ks'][0], KF_DST['kw'][0]))
        qd = S.sbuf([128, 2, TO], BF16, 'qD')
        qdr = S.sbuf([128, 2, TO], BF16, 'qDr')
        load_q(qd, QF_DST['qD'])
        load_q(qdr, QF_DST['qDr'])
        vd = S.sbuf([128, 64, 130], BF16, 'vD')
        load_v(vd, 12, 2)
        esel = S.sbuf([128, 64 * 128], BF16, 'esel')
        for hf in range(2):
            S.dma('pool', esel[:, hf * 4096:(hf + 1) * 4096], c_esel[:, hf * 4096:(hf + 1) * 4096], writes=[esel])
        ovl = S.sbuf([128, 512], BF16, 'ovl')
        S.dma('pool', ovl[:], c_overlap[:], writes=[ovl])
        xkv = S.sbuf([128, SEQ], BF16, 'xkv')
        S.dma('sp', xkv[:], KF[KF_DST['kcvc'][0], :, :], writes=[xkv])
        w1 = S.sbuf([128, 32, 256], BF16, 'cw1')
        posT = S.sbuf([128, 32], BF16, 'cposT')
        w2 = S.sbuf([128, 2, 2, 128], BF16, 'cw2')
        for kv in range(2):
            S.dma('pool', w1[kv * 64:(kv + 1) * 64, :, :], cw1[kv].rearrange('(l d) f -> d l f', d=64), writes=[w1])
            S.dma('pool', posT[kv * 64:(kv + 1) * 64, :], cpos[kv].rearrange('(l d) -> d l', d=64), writes=[posT],
                  allow_slow_non_contiguous=True)
            for dup in range(2):
                S.dma('pool', w2[:, kv, :, dup * 64:(dup + 1) * 64], cw2[kv].rearrange('(hc p) d -> p hc d', p=128),
                      writes=[w2])
        hid = [[S.sbuf([128, 512], BF16, 'hid%d%d' % (kv, hc)) for hc in range(2)] for kv in range(2)]
        pb = S.sbuf([128, 4], F32, 'cpb')
        for kv in range(2):
            base = kv * 64
            x3 = xkv[base:base + 64, :].rearrange('p (n s) -> p n s', s=16)
            for hc in range(2):
                S.memset('pool', hid[kv][hc], hid[kv][hc][:], 0.0)
                bank = PS[kv * 4 + hc]
                bank2 = PS[kv * 4 + 2 + hc]
                for l in range(32):
                    S.mm(bank, bank[:, 0:511], w1, w1[base:base + 64, l, hc * 128:(hc + 1) * 128],
                         xkv, x3[:, (l // 16):(l // 16) + 511, l % 16], start=(l == 0), stop=(l == 31))
                for l in range(32):
                    S.mm(bank2, bank2[:, 0:1], w1, w1[base:base + 64, l, hc * 128:(hc + 1) * 128],
                         posT, posT[base:base + 64, l:l + 1], start=(l == 0), stop=(l == 31))
                col = kv * 2 + hc
                S.cp('dve', pb, pb[:, col:col + 1], bank2, bank2[:, 0:1])
                S.act(hid[kv][hc], hid[kv][hc][:, 0:511], bank, bank[:, 0:511], AF.Silu, reads=[pb],
                      bias=pb[:, col:col + 1])
        kcT = S.sbuf([128, 512], BF16, 'kcT')
        S.memset('pool', kcT, kcT[:], 0.0)
        bank = PS[4]
        for hc in range(2):
            S.mm(bank, bank[:, 0:511], w2, w2[:, 0, hc, :], hid[0][hc], hid[0][hc][:, 0:511], start=(hc == 0), stop=(hc == 1))
        S.cp('dve', kcT, kcT[:, 0:511], bank, bank[:, 0:511])
        vcaug = S.sbuf([128, 4, 65], BF16, 'vcaug')
        S.memset('pool', vcaug, vcaug[:], 1.0)
        for cidx in range(4):
            bank = PS[5 + cidx % 2]
            for hc in range(2):
                S.mm(bank, bank[:, 0:64], hid[1][hc], hid[1][hc][:, cidx * 128:(cidx + 1) * 128], w2, w2[:, 1, hc, 0:64],
                     start=(hc == 0), stop=(hc == 1))
            S.cp('dve', vcaug, vcaug[:, cidx, 0:64], bank, bank[:, 0:64])
        S.barrier()
        pts = [S.sbuf([128, 512], BF16, 'ptD%d' % i) for i in range(3)]
        ec = [S.sbuf([128, 512], BF16, 'ecD%d' % i) for i in range(4)]
        tqb = S.sbuf([128, 128], F32, 'tqb')
        cm = S.sbuf([128, 128], BF16, 'cmD')
        selm = S.sbuf([128, 128], F32, 'selmD')
        imp = S.sbuf([128, 128], F32, 'impD')
        imp3 = S.sbuf([128, 128], F32, 'imp3D')
        t16 = S.sbuf([128, 16], F32, 't16D')
        bsel = S.sbuf([128, 128], F32, 'bselD')
        biasT4 = S.sbuf([128, 512], BF16, 'biasT4')
        rdd = S.sbuf([128, 12], F32, 'rdD')
        gts = S.sbuf([128, 12], F32, 'gtsD')
        coef = S.sbuf([128, 12], F32, 'coefD')
        omixs = [S.sbuf([128, 256], F32, 'omixD%d' % i) for i in range(2)]
        OC, OS, OW, UB = PS[4], PS[5], PS[6], PS[7]
        for si, slot in enumerate(slots):
            S.dma('sp', tqb[:], c_tq[0:1, slot * 128:(slot + 1) * 128].partition_broadcast(128), writes=[tqb])
            S.dma('sp', selm[:], c_selm[:, slot * 128:(slot + 1) * 128], writes=[selm])
            S.dma('sp', gts[:], GT[slot * 128:(slot + 1) * 128, :], writes=[gts])
            cids = [ci for ci in range(4) if 128 * ci <= 16 * slot + 14]
            for n_, ci in enumerate(cids):
                sb = SB2[sbk[0] % 2]
                sbk[0] += 1
                e_ = ec[ci]
                for h in (0, 2, 1, 3):
                    b = (h % 2) * 64
                    S.mm(sb, sb[:, scol(h):scol(h) + 128], kcT, kcT[b:b + 64, ci * 128:(ci + 1) * 128],
                         qd, qd[b:b + 64, h // 2, slot * 128:(slot + 1) * 128], start=True, stop=True)
                S.act(e_, e_[:].rearrange('p (b c) -> p b c', b=2), sb, v2(sb), AF.Exp)
                if 128 * ci + 127 > 16 * slot - 2:
                    S.ts('dve', cm, cm[:], tqb, tqb[:], ptab[:, 4 + ci:5 + ci], None, ALU.is_ge, reads=[ptab])
                    for h in range(4):
                        S.tt('dve', e_, e_[:, h * 128:(h + 1) * 128], e_, e_[:, h * 128:(h + 1) * 128], cm, cm[:], ALU.mult)
                for h in range(4):
                    S.mm(OC, OC[:, h * 65:(h + 1) * 65], e_, e_[:, pcol(h):pcol(h) + 128], vcaug, vcaug[:, ci, :],
                         start=(n_ == 0 and h == 0), stop=(n_ == len(cids) - 1), skip=True)
            for h in range(4):
                for n_, ci in enumerate(cids):
                    S.mm(UB, UB[:, h * 128:(h + 1) * 128], ec[ci], ec[ci][:, pcol(h):pcol(h) + 128],
                         ovl, ovl[:, ci * 128:(ci + 1) * 128], start=(n_ == 0), stop=(n_ == len(cids) - 1))
            oc4 = view4(OC)
            S.ts('dve', rdd, rdd[:, 0:4], OC, oc4[:, :, 64], 1e-30, None, ALU.add)
            S.raw('dve', 'reciprocal', reads=[rdd], writes=[rdd], out=rdd[:, 0:4], in_=rdd[:, 0:4])
            S.ts('dve', imp, imp[:], UB, UB[:, 0:128], rdd[:, 0:1], None, ALU.mult, reads=[rdd])
            for h in range(1, 4):
                S.stt(imp, imp[:], UB, UB[:, h * 128:(h + 1) * 128], rdd[:, h:h + 1], imp, imp[:], ALU.mult, ALU.add,
                      reads=[rdd])
            S.tt('dve', imp, imp[:], imp, imp[:], selm, selm[:], ALU.add)
            S.raw('dve', 'max', reads=[imp], writes=[t16], out=t16[:, 0:8], in_=imp[:])
            S.raw('dve', 'match_replace', reads=[imp, t16], writes=[imp3], out=imp3[:], in_to_replace=t16[:, 0:8],
                  in_values=imp[:], imm_value=-BIG)
            S.raw('dve', 'max', reads=[imp3], writes=[t16], out=t16[:, 8:16], in_=imp3[:])
            S.ts('dve', t16, t16[:, 15:16], t16, t16[:, 15:16], -1e29, None, ALU.max)
            S.ts('dve', bsel, bsel[:], imp, imp[:], t16[:, 15:16], 1.0, ALU.is_ge, ALU.subtract, reads=[t16])
            S.ts('dve', bsel, bsel[:], bsel, bsel[:], -NEG, None, ALU.mult)
            tb_ = UB
            S.tr(tb_, tb_[:, 0:128], bsel, bsel[:], ident, ident[:])
            for h in range(4):
                S.cp(alt(), biasT4, biasT4[:, h * 128:(h + 1) * 128], tb_, tb_[:, 0:128])
            nkb = 2 * slot + 2
            for kb in range(nkb):
                attn_step(slot, kb, ktd, lambda h: 0, qdr, pts, lambda h, kb: (vd, vd[:, kb, 0:65]), OS, kb == 0,
                          kb == nkb - 1, mask=causal_mask(slot, kb),
                          bias=(biasT4, lambda par: biasT4[:, par * 256:(par + 1) * 256], esel,
                                esel[:, kb * 128:(kb + 1) * 128]))
            wk = [kb for kb in range(2 * slot - 4, 2 * slot + 2) if kb >= 0]
            for n_, kb in enumerate(wk):
                m = causal_mask(slot, kb)
                if kb == 2 * slot - 4:
                    m = M_W4
                elif kb == 2 * slot - 3:
                    m = M_W3
                attn_step(slot, kb, ktd, lambda h: 1, qdr, pts, lambda h, kb: (vd, vd[:, kb, 65:130]), OW, n_ == 0,
                          n_ == len(wk) - 1, mask=m)
            os4, ow4 = view4(OS), view4(OW)
            S.raw('dve', 'reciprocal', reads=[OS], writes=[rdd], out=rdd[:, 4:8], in_=os4[:, :, 64])
            S.raw('dve', 'reciprocal', reads=[OW], writes=[rdd], out=rdd[:, 8:12], in_=ow4[:, :, 64])
            g3 = gts[:].rearrange('p (h b) -> p h b', b=3)
            c3 = coef[:].rearrange('p (b h) -> p b h', b=3)
            for br in range(3):
                S.tt('dve', coef, c3[:, br, :], gts, g3[:, :, br], rdd, rdd[:, br * 4:(br + 1) * 4], ALU.mult)
            omix = omixs[si % 2]
            for h in range(4):
                oh = omix[:, h * 64:(h + 1) * 64]
                S.ts('dve', omix, oh, OC, oc4[:, h, 0:64], coef[:, h:h + 1], None, ALU.mult, reads=[coef])
                S.stt(omix, oh, OS, os4[:, h, 0:64], coef[:, 4 + h:5 + h], omix, oh, ALU.mult, ALU.add, reads=[coef])
                S.stt(omix, oh, OW, ow4[:, h, 0:64], coef[:, 8 + h:9 + h], omix, oh, ALU.mult, ALU.add, reads=[coef])
            store_mix(omix, slot, 3)
        S.pop_scope()
    if not do_tail:
        return
    H1 = S.dram('H1' + sfx, [TO, DM], F32)
    H1T = S.dram('H1T' + sfx, [8, 128, TO], BF16)
    S.push_scope()
    lnv = S.sbuf([128, 4, DM], F32, 'lnv')
    for i_ in range(4):
        S.dma('sp', lnv[:, i_, :], vecs[i_, :].partition_broadcast(128), writes=[lnv])
    st = S.sbuf([128, 8], F32, 'lnst')
    junk = S.sbuf([128, DM], F32, 'lnjunk')

    def layer_norm(x, out, gi):
        S.act(junk, junk[:], x, x[:], AF.Copy, writes=[st], accum_out=st[:, 0:1])
        S.act(junk, junk[:], x, x[:], AF.Square, writes=[st], accum_out=st[:, 1:2])
        S.ts('dve', st, st[:, 0:2], st, st[:, 0:2], 1.0 / DM, None, ALU.mult)
        S.tt('dve', st, st[:, 2:3], st, st[:, 0:1], st, st[:, 0:1], ALU.mult)
        S.tt('dve', st, st[:, 3:4], st, st[:, 1:2], st, st[:, 2:3], ALU.subtract)
        S.ts('dve', st, st[:, 3:4], st, st[:, 3:4], 1e-5, None, ALU.add)
        S.act(st, st[:, 4:5], st, st[:, 3:4], AF.Sqrt)
        S.raw('dve', 'reciprocal', reads=[st], writes=[st], out=st[:, 5:6], in_=st[:, 4:5])
        S.tt('dve', st, st[:, 6:7], st, st[:, 0:1], st, st[:, 5:6], ALU.mult)
        S.ts('dve', st, st[:, 6:7], st, st[:, 6:7], -1.0, None, ALU.mult)
        S.act(out, out[:], x, x[:], AF.Identity, reads=[st], scale=st[:, 5:6], bias=st[:, 6:7])
        S.tt('dve', out, out[:], out, out[:], lnv, lnv[:, gi, :], ALU.mult)
        S.tt('pool', out, out[:], out, out[:], lnv, lnv[:, gi + 1, :], ALU.add)

    def transpose_rows(x, ncol, dst, dst_ap_fn):
        for g0 in range(0, ncol, 4):
            n = min(4, ncol - g0)
            bank = PS[6 + (g0 // 4) % 2]
            for k4 in range(n):
                S.tr(bank, bank[:, k4 * 128:(k4 + 1) * 128], x, x[:, (g0 + k4) * 128:(g0 + k4 + 1) * 128], ident, ident[:])
            S.cp(alt(), dst, dst_ap_fn(g0, n), bank, bank[:, 0:n * 128].rearrange('p (k t) -> p k t', k=n))

    S.push_scope()
    wo = S.sbuf([128, 8, DM], BF16, 'wo')
    S.dma('pool', wo[:], wout[:].rearrange('(k p) f -> p k f', p=128), writes=[wo])
    mrow = [S.sbuf([128, DM], F32, 'mrow%d' % i) for i in range(2)]
    hrw = [S.sbuf([128, DM], F32, 'hrw%d' % i) for i in range(2)]
    mT = [S.sbuf([128, 8, 128], BF16, 'mT%d' % i) for i in range(2)]
    yy = [S.sbuf([128, DM], F32, 'yy%d' % i) for i in range(2)]
    h1s = [S.sbuf([128, DM], F32, 'h1s%d' % i) for i in range(2)]
    h1t = [S.sbuf([128, 8, 128], BF16, 'h1t%d' % i) for i in range(2)]
    for tb in range(NSLOT):
        r0 = tb * 128
        mr, hr, mt, y, h1, ht = mrow[tb % 2], hrw[tb % 2], mT[tb % 2], yy[tb % 2], h1s[tb % 2], h1t[tb % 2]
        S.dma('sp', mr[:], MIX[r0:r0 + 128, :], reads=[MIX], writes=[mr])
        st_, sap_ = hq_rows(r0)
        S.dma('sp', hr[:], sap_, reads=[st_], writes=[hr])
        transpose_rows(mr, 8, mt, lambda g0, n, mt=mt: mt[:, g0:g0 + n, :])
        for hd in range(2):
            bank = PS[hd]
            for kc in range(8):
                S.mm(bank, bank[:], mt, mt[:, kc, :], wo, wo[:, kc, hd * 512:(hd + 1) * 512], start=(kc == 0), stop=(kc == 7))
            S.stt(y, y[:, hd * 512:(hd + 1) * 512], hr, hr[:, hd * 512:(hd + 1) * 512], ALPHA, bank, bank[:], ALU.mult, ALU.add)
        layer_norm(y, h1, 0)
        S.dma('sp', H1[r0:r0 + 128, :], h1[:], reads=[h1], writes=[H1])
        transpose_rows(h1, 8, ht, lambda g0, n, ht=ht: ht[:, g0:g0 + n, :])
        S.dma('sp', H1T[:, :, r0:r0 + 128].rearrange('k p t -> p k t'), ht[:], reads=[ht], writes=[H1T])
    S.pop_scope()

    S.push_scope()
    TG = 1024
    NTB = TG // 128
    h1T = S.sbuf([128, 8, TG], BF16, 'h1T')
    facc = S.sbuf([128, NTB, DM], F32, 'facc')
    gT = S.sbuf([128, 7, TG], BF16, 'gT')
    w2sc = [S.sbuf([128, 7, DM], BF16, 'w2sc%d' % i) for i in range(2)]
    w1c = [S.sbuf([128, 8, 128], BF16, 'w1c%d' % i) for i in range(2)]
    w3c = [S.sbuf([128, 8, 128], BF16, 'w3c%d' % i) for i in range(2)]
    sa = [S.sbuf([128, 512], BF16, 'sa%d' % i) for i in range(2)]
    wr_sb = S.sbuf([128, 8, 8], BF16, 'wr_sb')
    S.dma('pool', wr_sb[:], wrt[:].rearrange('(k p) f -> p k f', p=128), writes=[wr_sb])
    lg = S.sbuf([128, 8], F32, 'lg')
    tp8 = S.sbuf([128, 8], F32, 'tp8')
    gg = S.sbuf([128, 4], F32, 'gg')
    m12 = S.sbuf([128, 16], F32, 'm12')
    gate = S.sbuf([128, NTB, 8], F32, 'gate')
    pg = S.sbuf([128, 8, DM], BF16, 'pleg_sb')
    pp = S.sbuf([128, 2, DM], BF16, 'plep_sb')
    S.dma('pool', pg[:], pleg[:].rearrange('(k p) f -> p k f', p=128), writes=[pg])
    S.dma('pool', pp[:], plep[:].rearrange('(k p) f -> p k f', p=128), writes=[pp])
    h1r = [S.sbuf([128, DM], F32, 'h1r%d' % i) for i in range(2)]
    y2 = S.sbuf([128, DM], F32, 'y2')
    h2 = [S.sbuf([128, DM], F32, 'h2_%d' % i) for i in range(2)]
    h2T = S.sbuf([128, 8, 128], BF16, 'h2T')
    prow = S.sbuf([128, 256], F32, 'prow')
    pT = S.sbuf([128, 2, 128], BF16, 'pT')
    sg = S.sbuf([128, 512], F32, 'sg')
    oo = [S.sbuf([128, DM], F32, 'oo%d' % i) for i in range(2)]
    wi = 0
    for grp in range(TO // TG):
        t0 = grp * TG
        S.dma('sp', h1T[:], H1T[:, :, t0:t0 + TG].rearrange('k p t -> p k t'), reads=[H1T], writes=[h1T])
        if moe:
            for tb in range(NTB):
                bank = PS[6 + tb % 2]
                for kc in range(8):
                    S.mm(bank, bank[:, 0:8], h1T, h1T[:, kc, tb * 128:(tb + 1) * 128], wr_sb, wr_sb[:, kc, :],
                         start=(kc == 0), stop=(kc == 7))
                S.cp('dve', lg, lg[:], bank, bank[:, 0:8])
                S.raw('dve', 'max', reads=[lg], writes=[tp8], out=tp8[:], in_=lg[:])
                S.tt('dve', gg, gg[:, 0:1], tp8, tp8[:, 1:2], tp8, tp8[:, 0:1], ALU.subtract)
                S.act(gg, gg[:, 1:2], gg, gg[:, 0:1], AF.Sigmoid)
                S.ts('dve', gg, gg[:, 2:3], gg, gg[:, 1:2], -1.0, 1.0, ALU.mult, ALU.add)
                S.ts('dve', m12, m12[:, 0:8], lg, lg[:], tp8[:, 0:1], gg[:, 2:3], ALU.is_equal, ALU.mult, reads=[tp8, gg])
                S.ts('dve', m12, m12[:, 8:16], lg, lg[:], tp8[:, 1:2], gg[:, 1:2], ALU.is_equal, ALU.mult, reads=[tp8, gg])
                S.tt('dve', gate, gate[:, tb, :], m12, m12[:, 0:8], m12, m12[:, 8:16], ALU.add)
        for e_ in range(ne):
            for sc in range(4):
                w2t = w2sc[(e_ * 4 + sc) % 2]
                S.dma('pool', w2t[:], fw2[e_, sc * 896:(sc + 1) * 896, :].rearrange('(j p) f -> p j f', p=128), writes=[w2t])
                for j in range(7):
                    ffc = sc * 7 + j
                    a_w, b_w = w1c[wi % 2], w3c[wi % 2]
                    wi += 1
                    S.dma('pool', a_w[:], fw1[e_, :, ffc * 128:(ffc + 1) * 128].rearrange('(k p) f -> p k f', p=128), writes=[a_w])
                    S.dma('pool', b_w[:], fw3[e_, :, ffc * 128:(ffc + 1) * 128].rearrange('(k p) f -> p k f', p=128), writes=[b_w])
                    for hf in range(TG // 512):
                        ba, bb = PS[(hf % 2) * 2], PS[(hf % 2) * 2 + 1]
                        for kc in range(8):
                            S.mm(ba, ba[:], a_w, a_w[:, kc, :], h1T, h1T[:, kc, hf * 512:(hf + 1) * 512], start=(kc == 0), stop=(kc == 7))
                        for kc in range(8):
                            S.mm(bb, bb[:], b_w, b_w[:, kc, :], h1T, h1T[:, kc, hf * 512:(hf + 1) * 512], start=(kc == 0), stop=(kc == 7))
                        s_ = sa[hf % 2]
                        S.act(s_, s_[:], ba, ba[:], AF.Silu)
                        S.tt('dve', gT, gT[:, j, hf * 512:(hf + 1) * 512], s_, s_[:], bb, bb[:], ALU.mult)
                for tb in range(NTB):
                    for hd in range(2):
                        bank = PS[4 + (tb * 2 + hd) % 4]
                        for j in range(7):
                            S.mm(bank, bank[:], gT, gT[:, j, tb * 128:(tb + 1) * 128], w2t, w2t[:, j, hd * 512:(hd + 1) * 512],
                                 start=(j == 0), stop=(j == 6))
                        fa = facc[:, tb, hd * 512:(hd + 1) * 512]
                        first = (e_ == 0 and sc == 0)
                        if moe:
                            gsc = gate[:, tb, e_:e_ + 1]
                            if first:
                                S.ts('dve', facc, fa, bank, bank[:], gsc, None, ALU.mult, reads=[gate])
                            else:
                                S.stt(facc, fa, bank, bank[:], gsc, facc, fa, ALU.mult, ALU.add, reads=[gate])
                        else:
                            if first:
                                S.cp('dve', facc, fa, bank, bank[:])
                            else:
                                S.tt('dve', facc, fa, facc, fa, bank, bank[:], ALU.add)
        for tb in range(NTB):
            r0 = t0 + tb * 128
            hr, h2_, o_ = h1r[tb % 2], h2[tb % 2], oo[tb % 2]
            S.dma('sp', hr[:], H1[r0:r0 + 128, :], reads=[H1], writes=[hr])
            st_, sap_ = p_rows(r0)
            S.dma('sp', prow[:], sap_, reads=[st_], writes=[prow])
            S.stt(y2, y2[:], hr, hr[:], ALPHA, facc, facc[:, tb, :], ALU.mult, ALU.add)
            layer_norm(y2, h2_, 2)
            transpose_rows(h2_, 8, h2T, lambda g0, n: h2T[:, g0:g0 + n, :])
            transpose_rows(prow, 2, pT, lambda g0, n: pT[:, g0:g0 + n, :])
            for hd in range(2):
                bg, bp = PS[hd * 2], PS[hd * 2 + 1]
                for kc in range(8):
                    S.mm(bg, bg[:], h2T, h2T[:, kc, :], pg, pg[:, kc, hd * 512:(hd + 1) * 512], start=(kc == 0), stop=(kc == 7))
                for k2 in range(2):
                    S.mm(bp, bp[:], pT, pT[:, k2, :], pp, pp[:, k2, hd * 512:(hd + 1) * 512], start=(k2 == 0), stop=(k2 == 1))
                S.act(sg, sg[:], bg, bg[:], AF.Sigmoid)
                S.tt('dve', sg, sg[:], sg, sg[:], bp, bp[:], ALU.mult)
                S.tt('pool', o_, o_[:, hd * 512:(hd + 1) * 512], sg, sg[:], h2_, h2_[:, hd * 512:(hd + 1) * 512], ALU.add)
            ot_, oap_ = out_rows(r0)
            S.dma('sp', oap_, o_[:], reads=[o_], writes=[ot_])
    S.pop_scope()
    S.pop_scope()
    return


WNAMES = ['wkf', 'wqf', 'wkt', 'wqt', 'wout', 'vecs', 'dlam', 'dgain', 'cpos', 'cw1', 'cw2', 'fw1', 'fw3', 'fw2',
          'wrt', 'plep', 'pleg']


def build_fused(cfg=None):
    nc = bass.Bass("TRN2", target_bir_lowering=False)
    S = Sched(nc)
    EI = 'ExternalInput'
    G = dict(nc=nc, S=S)
    hfull = S.dram('hfull', [SEQ, DM], F32, EI)
    posfull = S.dram('posfull', [SEQ], I32, EI)
    posown = S.dram('posown', [TO], I32, EI)
    pfull0 = S.dram('pfull0', [SEQ, 256], F32, EI)
    pown1 = S.dram('pown1', [TO, 256], F32, EI)
    parv = S.dram('parv', [128, 2], F32, EI)
    hout = S.dram('hout', [TO, DM], F32, 'ExternalOutput')

    def wset(sfx, moe):
        ne = NEXP if moe else 1
        shp = dict(wkf=[DM, NKF * 128], wqf=[DM, NQF * 128], wkt=[DM, 896], wqt=[DM, 12], wout=[DM, DM], vecs=[4, DM],
                   dlam=[128], dgain=[64], cpos=[2, 2048], cw1=[2, 2048, 256], cw2=[2, 256, 64], fw1=[ne, DM, DFF],
                   fw3=[ne, DM, DFF], fw2=[ne, DFF, DM], wrt=[DM, 8], plep=[256, DM], pleg=[DM, DM])
        return {k: S.dram(k + sfx, shp[k], F32, EI) for k in WNAMES}

    W0 = wset('_l0', False)
    W1 = wset('_l1', True)
    c_ident = S.dram('ident', [128, 128], F32, EI)
    c_ptab = S.dram('ptab', [128, 16], F32, EI)
    G['c_negtri'] = S.dram('negtri', [128, 128], F32, EI)
    G['c_negones'] = S.dram('negones', [128, 128], F32, EI)
    G['c_emoba'] = S.dram('emoba', [32, 64 * 128], F32, EI)
    G['c_esel'] = S.dram('esel', [128, 64 * 128], F32, EI)
    G['c_overlap'] = S.dram('overlap', [128, 512], F32, EI)
    CM = {k: S.dram('masks_' + k, [6, 128, 512], F32, EI) for k in ('p0', 'p1', 'own')}
    CT = {k: S.dram('tq_' + k, [1, NSLOT * 128], F32, EI) for k in ('p0', 'p1', 'own')}
    CS = {k: S.dram('selm_' + k, [128, NSLOT * 128], F32, EI) for k in ('p0', 'p1', 'own')}
    H0 = S.dram('H0', [2, TO, DM], F32)
    HO1 = S.dram('HO1', [TO, DM], F32)
    KF0 = S.dram('KF0', [NKFD, 128, SEQ], BF16)
    VA0 = S.dram('VA0', [SEQ, NVA], BF16)
    KF1 = S.dram('KF1', [NKFD, 128, SEQ], BF16)
    VA1 = S.dram('VA1', [SEQ, NVA], BF16)

    ident = S.sbuf([128, 128], F32, 'ident')
    S.dma('sp', ident[:], c_ident[:], writes=[ident])
    ptab = S.sbuf([128, 16], F32, 'ptab')
    S.dma('sp', ptab[:], c_ptab[:], writes=[ptab])
    G['ident'], G['ptab'] = ident, ptab
    G['masks'] = S.sbuf([128, 6, 512], BF16, 'masks')
    G['kmean'] = S.sbuf([128, 2, 32], BF16, 'kmean')
    P2 = [S.psum([128, 1024], F32, 'bank2_%d' % i) for i in range(4)]
    G['P2'] = P2
    G['PS'] = [TileView(P2[i // 2][:, (i % 2) * 512:(i % 2 + 1) * 512], 'bank%d' % i) for i in range(8)]
    G['SB2'] = [TileView(P2[i][:, :], 'sb2_%d' % i) for i in range(2)]

    pos2 = posfull[:].rearrange('(s two j) -> two s j', two=2, j=128)

    def mk_pass0(par):
        P = dict(W0)
        P.update(layer=0, moe=False, sfx='_0%d' % par, KF=KF0, VA=VA0, do_k=(par == 0),
                 c_masks=CM['p%d' % par], c_tq=CT['p%d' % par], c_selm=CS['p%d' % par])
        P['hk'] = lambda r0: (hfull, hfull[r0:r0 + 128, :])
        P['hq'] = lambda r0: (hfull, hfull[(2 * (r0 // 128) + par) * 128:(2 * (r0 // 128) + par + 1) * 128, :])
        P['posk'] = lambda c0: (posfull, posfull[c0:c0 + 512].partition_broadcast(128), False)
        P['posq'] = lambda c0: (posfull, pos2[par, c0 // 128:c0 // 128 + 4, :].partition_broadcast(128), True)
        P['prow'] = lambda r0: (pfull0, pfull0[(2 * (r0 // 128) + par) * 128:(2 * (r0 // 128) + par + 1) * 128, :])
        P['out'] = lambda r0: (H0, H0[par, r0:r0 + 128, :])
        return P

    emit_pass(G, mk_pass0(0), cfg)
    emit_pass(G, mk_pass0(1), cfg)
    S.push_scope()
    pv = S.sbuf([128, 2], F32, 'parv')
    S.dma('sp', pv[:], parv[:], writes=[pv])
    ba = [S.sbuf([128, DM], F32, 'bla%d' % i) for i in range(2)]
    bb = [S.sbuf([128, DM], F32, 'blb%d' % i) for i in range(2)]
    for tb in range(NSLOT):
        a_, b_ = ba[tb % 2], bb[tb % 2]
        S.dma('sp', a_[:], H0[0, tb * 128:(tb + 1) * 128, :], reads=[H0], writes=[a_])
        S.dma('sp', b_[:], H0[1, tb * 128:(tb + 1) * 128, :], reads=[H0], writes=[b_])
        S.ts('dve', a_, a_[:], a_, a_[:], pv[:, 0:1], None, ALU.mult, reads=[pv])
        S.stt(a_, a_[:], b_, b_[:], pv[:, 1:2], a_, a_[:], ALU.mult, ALU.add, reads=[pv])
        S.dma('sp', HO1[tb * 128:(tb + 1) * 128, :], a_[:], reads=[a_], writes=[HO1])
    S.pop_scope()
    P = dict(W1)
    P.update(layer=1, moe=True, sfx='_1', KF=KF1, VA=VA1, do_k=True, c_masks=CM['own'], c_tq=CT['own'], c_selm=CS['own'])
    P['hk'] = lambda r0: (H0, H0[(r0 // 128) % 2, ((r0 // 128) // 2) * 128:((r0 // 128) // 2 + 1) * 128, :])
    P['hq'] = lambda r0: (HO1, HO1[r0:r0 + 128, :])
    P['posk'] = lambda c0: (posfull, posfull[c0:c0 + 512].partition_broadcast(128), False)
    P['posq'] = lambda c0: (posown, posown[c0:c0 + 512].partition_broadcast(128), False)
    P['prow'] = lambda r0: (pown1, pown1[r0:r0 + 128, :])
    P['out'] = lambda r0: (hout, hout[r0:r0 + 128, :])
    emit_pass(G, P, cfg)
    return nc, S


def _own_rows(parity):
    blk = 2 * np.arange(NSLOT) + parity
    return (blk[:, None] * 128 + np.arange(128)[None, :]).reshape(-1)


def _layer_weights(layer, inp, sfx):
    f = np.float32
    w_in = np.asarray(inp['w_in'][layer], f)
    moe = (layer % 2 == 1)
    w = {
        'wkf': _gather_cols(w_in, KF_IDX), 'wqf': _gather_cols(w_in, QF_IDX),
        'wkt': _gather_cols(w_in, KT_IDX), 'wqt': _gather_cols(w_in, QT_IDX),
        'wout': np.ascontiguousarray(inp['w_out'][layer], f),
        'vecs': np.stack([inp['ln_mix_g'][layer], inp['ln_mix_b'][layer], inp['ln_ffn_g'][layer],
                          inp['ln_ffn_b'][layer]]).astype(f),
        'dlam': np.ascontiguousarray(inp['diff_lambda'][layer], f).reshape(128),
        'dgain': np.ascontiguousarray(inp['diff_gain'][layer], f),
        'cpos': np.ascontiguousarray(inp['nsa_cmp_pos'][layer], f).reshape(2, 2048),
        'cw1': np.ascontiguousarray(inp['nsa_cmp_w1'][layer], f),
        'cw2': np.ascontiguousarray(inp['nsa_cmp_w2'][layer], f),
        'plep': np.ascontiguousarray(inp['ple_proj'][layer], f),
        'pleg': np.ascontiguousarray(inp['ple_gate'][layer], f),
    }
    if moe:
        w['fw1'] = np.ascontiguousarray(inp['moe_w1'][layer // 2], f)
        w['fw3'] = np.ascontiguousarray(inp['moe_w3'][layer // 2], f)
        w['fw2'] = np.ascontiguousarray(inp['moe_w2'][layer // 2], f)
        w['wrt'] = np.ascontiguousarray(inp['moe_router'][layer // 2], f)
    else:
        w['fw1'] = np.ascontiguousarray(inp['ffn_w1'][layer // 2], f)[None]
        w['fw3'] = np.ascontiguousarray(inp['ffn_w3'][layer // 2], f)[None]
        w['fw2'] = np.ascontiguousarray(inp['ffn_w2'][layer // 2], f)[None]
        w['wrt'] = np.zeros((DM, 8), f)
    return {k + sfx: v for k, v in w.items()}


def fused_inputs(inp, cores):
    f = np.float32
    shared = {}
    shared.update(_layer_weights(0, inp, '_l0'))
    shared.update(_layer_weights(1, inp, '_l1'))
    consts = [_host_consts(0), _host_consts(1)]
    for k in ('ident', 'ptab', 'negtri', 'negones', 'emoba', 'esel', 'overlap'):
        shared[k] = consts[0][k]
    for par in range(2):
        shared['masks_p%d' % par] = consts[par]['masks']
        shared['tq_p%d' % par] = consts[par]['tq']
        shared['selm_p%d' % par] = consts[par]['selm']
    maps = []
    for core in cores:
        b, par = core // 2, core % 2
        rows = _own_rows(par)
        m = dict(shared)
        m['masks_own'] = consts[par]['masks']
        m['tq_own'] = consts[par]['tq']
        m['selm_own'] = consts[par]['selm']
        m['hfull'] = np.ascontiguousarray(inp['x'][b], f)
        m['posfull'] = np.ascontiguousarray(inp['positions'][b], np.int32)
        m['posown'] = np.ascontiguousarray(inp['positions'][b][rows], np.int32)
        m['pfull0'] = np.ascontiguousarray(inp['p'][0, b], f)
        m['pown1'] = np.ascontiguousarray(inp['p'][1, b][rows], f)
        pv = np.zeros((128, 2), f)
        pv[:, par] = 1.0
        m['parv'] = pv
        maps.append(m)
    return maps


def kernel(**inputs):
    inp = {k: np.asarray(v) for k, v in inputs.items()}
    cores = list(range(8))
    nc, S = build_fused()
    S.finish()
    maps = fused_inputs(inp, cores)
    res = run_bass_kernel_spmd(nc, maps, core_ids=cores)
    out = np.zeros((4, SEQ, DM), np.float32)
    for core in cores:
        b, par = core // 2, core % 2
        out[b][_own_rows(par)] = res.results[core]['hout']
    return out
```

```python
import math
import numpy as np
import ml_dtypes
import concourse.bass as bass
import concourse.mybir as mybir
from contextlib import ExitStack
from concourse.bass_utils import run_bass_kernel_spmd

F32 = mybir.dt.float32
BF16 = mybir.dt.bfloat16
I32 = mybir.dt.int32
AF = mybir.ActivationFunctionType
ALU = mybir.AluOpType
AX = mybir.AxisListType

ENGS = ['pe', 'act', 'dve', 'pool', 'sp']
N_DMA_SEMS = 8
SEM_ROLL = 30000


class Tile:
    __slots__ = ('t', 'lastw', 'readers', 'name')

    def __init__(self, t, name=''):
        self.t = t
        self.lastw = None
        self.readers = {}
        self.name = name

    def __getitem__(self, idx):
        return self.t[idx]


class TileView(Tile):
    __slots__ = ('base',)

    def __init__(self, base_ap, name=''):
        Tile.__init__(self, None, name)
        self.base = base_ap

    def __getitem__(self, idx):
        return self.base[idx]


class Sched:
    def __init__(self, nc):
        self.nc = nc
        self.es = ExitStack()
        self.scopes = []
        self.q = {e: [] for e in ENGS}
        self.cnt = {}
        self.sems = {}
        self.epoch = {}
        for e in ['pe', 'act', 'dve', 'pool']:
            self.epoch[e] = 0
            self._newsem((e, 0))
        self.dma_rr = {}
        for e in ['sp', 'act', 'pool']:
            for i in range(N_DMA_SEMS):
                self.epoch[('d', e, i)] = 0
                self._newsem(('d', e, i, 0))
            self.dma_rr[e] = 0
        self.seen = {e: {} for e in ENGS}
        self.ntiles = 0
        self.ninstr = 0

    def _newsem(self, key):
        self.sems[key] = self.es.enter_context(self.nc.semaphore('s_' + '_'.join(str(k) for k in key)))
        self.cnt[key] = 0

    def _alloc_stack(self):
        return self.scopes[-1] if self.scopes else self.es

    def sbuf(self, shape, dt, name=None):
        self.ntiles += 1
        name = ('sb%d_' % self.ntiles) + (name or '')
        t = self._alloc_stack().enter_context(self.nc.sbuf_tensor(name, list(shape), dt))
        return Tile(t, name)

    def psum(self, shape, dt=F32, name=None):
        self.ntiles += 1
        name = ('ps%d_' % self.ntiles) + (name or '')
        t = self._alloc_stack().enter_context(self.nc.psum_tensor(name, list(shape), dt))
        return Tile(t, name)

    def dram(self, name, shape, dt, kind='Internal'):
        t = self.nc.dram_tensor(name, list(shape), dt, kind=kind)
        return Tile(t, name)

    def sub(self, tile, name=''):
        return Tile(tile.t, name or tile.name)

    def push_scope(self):
        self.scopes.append(ExitStack())

    def pop_scope(self):
        self.barrier()
        self.scopes.pop().close()

    def barrier(self):
        deps = [(k, v) for k, v in self.cnt.items() if v > 0]
        for e in ENGS:
            waits = self._need(e, deps)
            if waits:
                self.q[e].append(([(self.sems[k], v) for (k, v) in waits], None, None, 0))

    def _need(self, eng, deps):
        out = []
        seen = self.seen[eng]
        for (k, v) in deps:
            if seen.get(k, 0) < v:
                seen[k] = v
                out.append((k, v))
        return out

    def op(self, eng, fn, reads=(), writes=()):
        deps = []
        for t in reads:
            if t.lastw is not None:
                deps.append(t.lastw)
        for t in writes:
            if t.lastw is not None:
                deps.append(t.lastw)
            for rk, rv in t.readers.items():
                deps.append((rk, rv))
        if eng == 'pe':
            deps = [d for d in deps if d[0][0] != 'pe']
        waits = self._need(eng, deps)
        key = (eng, self.epoch[eng])
        if self.cnt[key] >= SEM_ROLL:
            self.epoch[eng] += 1
            key = (eng, self.epoch[eng])
            self._newsem(key)
        self.cnt[key] += 1
        v = self.cnt[key]
        self.q[eng].append(([(self.sems[k], val) for (k, val) in waits], fn, self.sems[key], 1))
        self.ninstr += 1
        for t in reads:
            t.readers[key] = v
        for t in writes:
            t.lastw = (key, v)
            t.readers = {}
        return v

    def dma(self, eng, out_ap, in_ap, reads=(), writes=(), **kw):
        i = self.dma_rr[eng]
        self.dma_rr[eng] = (i + 1) % N_DMA_SEMS
        slot = ('d', eng, i)
        k = ('d', eng, i, self.epoch[slot])
        deps = []
        if self.cnt[k] > 0:
            deps.append((k, self.cnt[k]))
        if self.cnt[k] >= SEM_ROLL:
            self.epoch[slot] += 1
            k = ('d', eng, i, self.epoch[slot])
            self._newsem(k)
        for t in reads:
            if t.lastw is not None:
                deps.append(t.lastw)
        for t in writes:
            if t.lastw is not None:
                deps.append(t.lastw)
            deps.extend(t.readers.items())
        waits = self._need(eng, deps)
        self.cnt[k] += 16
        v = self.cnt[k]
        fn = (lambda e, o=out_ap, i_=in_ap, kw=kw: e.dma_start(out=o, in_=i_, **kw))
        self.q[eng].append(([(self.sems[kk], val) for (kk, val) in waits], fn, self.sems[k], 16))
        self.ninstr += 1
        for t in reads:
            t.readers[k] = v
        for t in writes:
            t.lastw = (k, v)
            t.readers = {}
        return (k, v)

    def collective(self, kind, groups, in_t, in_ap, out_t, out_ap):
        eng = 'pool'
        i = self.dma_rr[eng]
        self.dma_rr[eng] = (i + 1) % N_DMA_SEMS
        slot = ('d', eng, i)
        k = ('d', eng, i, self.epoch[slot])
        deps = []
        if self.cnt[k] > 0:
            deps.append((k, self.cnt[k]))
        if in_t.lastw is not None:
            deps.append(in_t.lastw)
        if out_t.lastw is not None:
            deps.append(out_t.lastw)
        deps.extend(out_t.readers.items())
        waits = self._need(eng, deps)
        self.cnt[k] += 16
        v = self.cnt[k]
        fn = (lambda e: e.collective_compute(kind, ALU.bypass, replica_groups=groups, ins=[in_ap], outs=[out_ap]))
        self.q[eng].append(([(self.sems[kk], val) for (kk, val) in waits], fn, self.sems[k], 16))
        self.ninstr += 1
        in_t.readers[k] = v
        out_t.lastw = (k, v)
        out_t.readers = {}

    def finish(self):
        self.barrier()
        q = self.q

        def replay(e, lst):
            for (wl, fn, sem, inc) in lst:
                for (s, v) in wl:
                    e.wait_ge(s, v)
                if fn is not None:
                    fn(e).then_inc(sem, inc)

        with self.nc.Block() as block:
            @block.tensor
            def _(e):
                replay(e, q['pe'])

            @block.scalar
            def _(e):
                replay(e, q['act'])

            @block.vector
            def _(e):
                replay(e, q['dve'])

            @block.gpsimd
            def _(e):
                replay(e, q['pool'])

            @block.sync
            def _(e):
                replay(e, q['sp'])
        self.es.close()

    def mm(self, ot, oap, lt, lap, rt, rap, start=True, stop=True, skip=False):
        if skip:
            return self.op('pe', lambda e: e.matmul(oap, lap, rap, start=start, stop=stop, skip_group_check=True),
                           reads=[lt, rt], writes=[ot])
        return self.op('pe', lambda e: e.matmul(oap, lap, rap, start=start, stop=stop), reads=[lt, rt], writes=[ot])

    def tr(self, ot, oap, it, iap, idt, idap):
        return self.op('pe', lambda e: e.transpose(oap, iap, idap), reads=[it, idt], writes=[ot])

    def act(self, ot, oap, it, iap, func, reads=(), writes=(), **kw):
        return self.op('act', lambda e: e.activation(out=oap, in_=iap, func=func, **kw),
                       reads=[it] + list(reads), writes=[ot] + list(writes))

    def tt(self, eng, ot, oap, at, aap, bt, bap, op):
        return self.op(eng, lambda e: e.tensor_tensor(out=oap, in0=aap, in1=bap, op=op), reads=[at, bt], writes=[ot])

    def ts(self, eng, ot, oap, it, iap, s1, s2, op0, op1=None, reads=()):
        if op1 is None:
            return self.op(eng, lambda e: e.tensor_scalar(out=oap, in0=iap, scalar1=s1, scalar2=None, op0=op0),
                           reads=[it] + list(reads), writes=[ot])
        return self.op(eng, lambda e: e.tensor_scalar(out=oap, in0=iap, scalar1=s1, scalar2=s2, op0=op0, op1=op1),
                       reads=[it] + list(reads), writes=[ot])

    def stt(self, ot, oap, at, aap, scalar, bt, bap, op0, op1, reads=()):
        return self.op('dve', lambda e: e.scalar_tensor_tensor(out=oap, in0=aap, scalar=scalar, in1=bap, op0=op0, op1=op1),
                       reads=[at, bt] + list(reads), writes=[ot])

    def cp(self, eng, ot, oap, it, iap):
        if eng == 'act':
            return self.op('act', lambda e: e.activation(out=oap, in_=iap, func=AF.Copy), reads=[it], writes=[ot])
        return self.op(eng, lambda e: e.tensor_copy(out=oap, in_=iap), reads=[it], writes=[ot])

    def raw(self, eng, meth, reads=(), writes=(), **kw):
        return self.op(eng, lambda e, meth=meth, kw=kw: getattr(e, meth)(**kw), reads=reads, writes=writes)

    def memset(self, eng, t, ap, val):
        return self.op(eng, lambda e, ap=ap, val=val: e.memset(ap, val), writes=[t])


SEQ = 8192
DM = 1024
TO = 4096
NSLOT = 32
DFF = 3584
NFFC = 28
NEXP = 8
ALPHA = 4 ** 0.25
NCMP = 511
NEG = -30000.0
BIG = 1.0e30
TWO_PI = 2.0 * math.pi
TWO_PI_HI = float(np.float32(TWO_PI))
TWO_PI_LO = float(TWO_PI - np.float64(np.float32(TWO_PI)))
MAGIC = 12582912.0

OFF = dict(a_q=0, a_k=256, a_v=512, b_q=768, b_k=1024, b_v=1280, c_q=1536, c_k=1792, c_v=2048, d_q=2304,
           d_kc=2560, d_vc=2624, d_ks=2688, d_vs=2752, d_kw=2816, d_vw=2880, d_g=2944)


def _swap(idx, w):
    idx = np.asarray(idx)
    out = idx.copy().reshape(-1, w)
    h = w // 2
    out = np.concatenate([out[:, h:], out[:, :h]], axis=1)
    return out.reshape(-1)


def _colplan():
    ar = np.arange
    kA = OFF['a_k'] + ar(256)
    bk = OFF['b_k'] + ar(256)
    kB1 = bk.copy().reshape(4, 64)
    kB1[:, 32:] = -1
    kB2 = bk.copy().reshape(4, 64)
    kB2[:, :32] = -1
    kB1s = kB1.copy()
    kB1s[:, :32] = _swap(kB1[:, :32].reshape(-1), 32).reshape(4, 32)
    kB2s = kB2.copy()
    kB2s[:, 32:] = _swap(kB2[:, 32:].reshape(-1), 32).reshape(4, 32)
    kC = OFF['c_k'] + ar(256)
    ks = OFF['d_ks'] + ar(64)
    kw = OFF['d_kw'] + ar(64)
    ksks = np.concatenate([ks, ks])
    kwkw = np.concatenate([kw, kw])
    kcvc = np.concatenate([OFF['d_kc'] + ar(64), OFF['d_vc'] + ar(64)])
    kf = np.concatenate([kA, kB1.reshape(-1), kB1s.reshape(-1), kB2.reshape(-1), kB2s.reshape(-1),
                         kC, _swap(kC, 64), ksks, _swap(ksks, 64), kwkw, _swap(kwkw, 64), kcvc])
    qA = OFF['a_q'] + ar(256)
    qB = OFF['b_q'] + ar(256)
    qC = OFF['c_q'] + ar(256)
    qD = OFF['d_q'] + ar(256)
    qf = np.concatenate([qA, qB, _swap(qB, 32), qC, _swap(qC, 64), qD, _swap(qD, 64)])
    kt = np.concatenate([OFF['a_v'] + ar(256), OFF['b_v'] + ar(256), OFF['c_v'] + ar(256),
                         OFF['d_vs'] + ar(64), OFF['d_vw'] + ar(64)])
    qt = OFF['d_g'] + ar(12)
    return kf, qf, kt, qt


KF_IDX, QF_IDX, KT_IDX, QT_IDX = _colplan()
NKF = len(KF_IDX) // 128
NQF = len(QF_IDX) // 128
KF_SRC = dict(kA=(0, 1), kB1=(2, 3), kB1s=(4, 5), kB2=(6, 7), kB2s=(8, 9), kC=(10, 11), kCs=(12, 13),
              ks=(14,), kss=(15,), kw=(16,), kws=(17,), kcvc=(18,))
KF_DST = dict(kA=(0, 1), kB1=(2, 3), kB2=(4, 5), kC=(6, 7), ks=(8,), kw=(9,), kcvc=(10,))
NKFD = 11
QF_SRC = dict(qA=(0, 1), qB=(2, 3), qBs=(4, 5), qC=(6, 7), qCs=(8, 9), qD=(10, 11), qDs=(12, 13))
QF_DST = dict(qA=(0, 1), qB=(2, 3), qC=(4, 5), qD=(6, 7), qDr=(8, 9))
NQFD = 10
NVA = 14 * 65


def _gather_cols(w, idx):
    out = np.zeros((w.shape[0], len(idx)), dtype=w.dtype)
    m = idx >= 0
    out[:, m] = w[:, idx[m]]
    return out


def _host_consts(parity):
    c = {}
    c['ident'] = np.eye(128, dtype=np.float32)
    j = np.arange(128)[:, None]
    i = np.arange(128)[None, :]
    le = (j <= i).astype(np.float32)
    lt = (j < i).astype(np.float32)
    gt = (j > i).astype(np.float32)
    one = np.ones((128, 128), np.float32)
    zero = np.zeros((128, 128), np.float32)
    if parity == 0:
        ms = [zero, le, zero, lt, gt, one]
    else:
        ms = [le, one, lt, one, zero, gt]
    c['masks'] = np.stack([np.tile(m, (1, 4)) for m in ms], 0).astype(np.float32)
    jj = np.arange(128)[:, None]
    ss = np.arange(128)[None, :]
    c['negtri'] = -(jj >= ss).astype(np.float32)
    c['negones'] = -np.ones((128, 128), np.float32)
    r = np.arange(128)
    tab = np.zeros((128, 16), np.float32)
    tab[:, 0] = 10000.0 ** (-(r % 16) / 16.0)
    tab[:, 1] = 10000.0 ** (-(r % 32) / 32.0)
    tab[:, 2] = np.where((r % 32) < 16, -1.0, 1.0)
    tab[:, 3] = np.where((r % 64) < 32, -1.0, 1.0)
    for ci in range(4):
        tab[:, 4 + ci] = 16.0 * (128 * ci + r) + 31.0
    c['ptab'] = tab
    em = np.zeros((32, 64, 128), np.float32)
    for kb in range(64):
        em[kb // 2, kb, :] = 1.0
    c['emoba'] = em.reshape(32, 64 * 128)
    es = np.zeros((128, 64, 128), np.float32)
    for kb in range(64):
        es[2 * kb, kb, :64] = 1.0
        es[2 * kb + 1, kb, 64:] = 1.0
    c['esel'] = es.reshape(128, 64 * 128)
    n = np.arange(512)[:, None]
    m = np.arange(128)[None, :]
    ov = ((16 * n < 64 * m + 64) & (16 * n + 32 > 64 * m)).astype(np.float32)
    ov[511, :] = 0.0
    c['overlap'] = ov.reshape(4, 128, 128).transpose(1, 0, 2).reshape(128, 512).copy()
    tq = np.zeros((NSLOT, 128), np.float32)
    selm = np.zeros((128, NSLOT, 128), np.float32)
    for s in range(NSLOT):
        qb = 2 * s + parity
        t = qb * 128 + np.arange(128)
        tq[s] = t
        qblk = t // 64
        mm_ = np.arange(128)[None, :]
        forced = (mm_ == 0) | (mm_ == qblk[:, None]) | (mm_ == qblk[:, None] - 1)
        causal = mm_ <= qblk[:, None]
        selm[:, s, :] = np.where(forced, BIG, np.where(causal, 0.0, -BIG))
    c['tq'] = tq.reshape(1, NSLOT * 128)
    c['selm'] = selm.reshape(128, NSLOT * 128)
    return c


def emit_pass(G, P, cfg=None):
    cfg = cfg or {}
    slots = cfg.get('slots', list(range(NSLOT)))
    mixers = cfg.get('mixers', 'ABCD')
    do_tail = cfg.get('tail', True)
    dbg = False
    layer = P['layer']
    moe = P['moe']
    sfx = P['sfx']
    lam_init = 0.8 - 0.6 * math.exp(-0.3 * layer)
    nc, S = G['nc'], G['S']
    hk_rows, hq_rows, posk, posq, p_rows, out_rows = P['hk'], P['hq'], P['posk'], P['posq'], P['prow'], P['out']
    wkf, wqf, wkt, wqt, wout, vecs = P['wkf'], P['wqf'], P['wkt'], P['wqt'], P['wout'], P['vecs']
    dlam, dgain, cpos, cw1, cw2 = P['dlam'], P['dgain'], P['cpos'], P['cw1'], P['cw2']
    fw1, fw3, fw2, wrt, plep, pleg = P['fw1'], P['fw3'], P['fw2'], P['wrt'], P['plep'], P['pleg']
    ne = NEXP if moe else 1
    c_masks, c_tq, c_selm = P['c_masks'], P['c_tq'], P['c_selm']
    c_negtri, c_negones, c_emoba, c_esel, c_overlap = G['c_negtri'], G['c_negones'], G['c_emoba'], G['c_esel'], G['c_overlap']
    KF, VA, do_k = P['KF'], P['VA'], P['do_k']
    QF = S.dram('QF' + sfx, [NQFD, 128, TO], BF16)
    GT = S.dram('GT' + sfx, [TO, 12], F32)
    MIX = S.dram('MIX' + sfx, [TO, DM], F32)
    ident, ptab, kmean, masks = G['ident'], G['ptab'], G['kmean'], G['masks']
    P2, PS, SB2 = G['P2'], G['PS'], G['SB2']
    S.dma('pool', masks[:], c_masks[:].rearrange('m p f -> p m f'), writes=[masks])
    M_HI_LE, M_LO_LE, M_HI_LT, M_LO_LT, M_W4, M_W3 = range(6)
    if cfg.get('zfill'):
        S.push_scope()
        ztile = S.sbuf([128, DM], F32, 'ztile')
        S.memset('pool', ztile, ztile[:], 0.0)
        for tb in range(NSLOT):
            S.dma('sp', MIX[tb * 128:(tb + 1) * 128, :], ztile[:], reads=[ztile], writes=[MIX])
        S.pop_scope()

    def scol(h):
        return (h % 2) * 512 + (h // 2) * 128

    def pcol(h):
        return (h % 2) * 256 + (h // 2) * 128

    def v2(t_):
        return t_[:].rearrange('p (b c) -> p b c', b=2)[:, :, 0:256]

    rr = [0]

    def alt(engs=('act', 'dve')):
        rr[0] += 1
        return engs[rr[0] % len(engs)]

    S.push_scope()
    wk_sb = S.sbuf([128, 8, NKF * 128], BF16, 'wk_sb')
    wq_sb = S.sbuf([128, 8, NQF * 128], BF16, 'wq_sb')
    wkt_sb = S.sbuf([128, 8, 896], BF16, 'wkt_sb')
    wqt_sb = S.sbuf([128, 8, 12], BF16, 'wqt_sb')
    for kc in range(8):
        S.dma('pool', wk_sb[:, kc, :], wkf[kc * 128:(kc + 1) * 128, :], writes=[wk_sb])
        S.dma('pool', wq_sb[:, kc, :], wqf[kc * 128:(kc + 1) * 128, :], writes=[wq_sb])
    S.dma('pool', wkt_sb[:], wkt[:].rearrange('(k p) f -> p k f', p=128), writes=[wkt_sb])
    S.dma('pool', wqt_sb[:], wqt[:].rearrange('(k p) f -> p k f', p=128), writes=[wqt_sb])

    hrow = [S.sbuf([128, DM], F32, 'hrow%d' % i) for i in range(2)]
    hT = [S.sbuf([128, 8, 512], BF16, 'hT%d' % i) for i in range(2)]
    posi = S.sbuf([128, 512], I32, 'posi')
    posf = S.sbuf([128, 512], F32, 'posf')
    rtmp = [S.sbuf([128, 512], F32, 'rtmp%d' % i) for i in range(3)]
    rope = {k: S.sbuf([128, 512], F32, 'rope_' + k) for k in ('cosB', 'sinB', 'cosCD', 'sinCD')}
    ftile = [S.sbuf([128, 512], BF16, 'ftile%d' % i) for i in range(4)]
    ft32 = [S.sbuf([128, 512], F32, 'ft32_%d' % i) for i in range(2)]
    vaug = [S.sbuf([128, 14, 65], BF16, 'vaug%d' % i) for i in range(2)]
    gsb = [S.sbuf([128, 12], F32, 'gsb%d' % i) for i in range(2)]
    kms = S.sbuf([128, 2, 32], F32, 'kms')
    for v_ in vaug:
        S.memset('pool', v_, v_[:], 1.0)

    def build_rope(pos_dram, c0):
        pt_, pap_, three_ = pos_dram(c0)
        S.dma('sp', posi[:].rearrange('p (a j) -> p a j', a=4) if three_ else posi[:], pap_, reads=[pt_], writes=[posi])
        S.cp('dve', posf, posf[:], posi, posi[:])
        for (tabcol, sgncol, ck, sk) in ((0, 2, 'cosB', 'sinB'), (1, 3, 'cosCD', 'sinCD')):
            ang, t1, t2 = rtmp
            S.ts('dve', ang, ang[:], posf, posf[:], ptab[:, tabcol:tabcol + 1], None, ALU.mult, reads=[ptab])
            for (shift, dst, sgn) in ((0.0, rope[sk], True), (math.pi / 2, rope[ck], False)):
                S.ts('dve', t1, t1[:], ang, ang[:], 1.0 / TWO_PI, shift / TWO_PI + MAGIC, ALU.mult, ALU.add)
                S.ts('dve', t1, t1[:], t1, t1[:], -MAGIC, None, ALU.add)
                S.stt(t2, t2[:], t1, t1[:], -TWO_PI_HI, ang, ang[:], ALU.mult, ALU.add)
                S.ts('dve', t2, t2[:], t2, t2[:], shift, None, ALU.add)
                S.stt(t2, t2[:], t1, t1[:], -TWO_PI_LO, t2, t2[:], ALU.mult, ALU.add)
                S.ts('dve', t1, t1[:], t2, t2[:], math.pi, -TWO_PI, ALU.is_gt, ALU.mult)
                S.tt('dve', t2, t2[:], t2, t2[:], t1, t1[:], ALU.add)
                S.ts('dve', t1, t1[:], t2, t2[:], -math.pi, TWO_PI, ALU.is_lt, ALU.mult)
                S.tt('dve', t2, t2[:], t2, t2[:], t1, t1[:], ALU.add)
                S.ts('dve', t2, t2[:], t2, t2[:], 3.14159, -3.14159, ALU.min, ALU.max)
                if sgn:
                    S.act(dst, dst[:], t2, t2[:], AF.Sin, reads=[ptab], scale=ptab[:, sgncol:sgncol + 1])
                else:
                    S.act(dst, dst[:], t2, t2[:], AF.Sin)

    def load_hT(src, r0, buf):
        for tb in range(4):
            hr = hrow[tb % 2]
            st_, sap_ = src(r0 + tb * 128)
            S.dma('sp', hr[:], sap_, reads=[st_], writes=[hr])
            for half in range(2):
                bank = PS[6 + half]
                for k4 in range(4):
                    kc = half * 4 + k4
                    S.tr(bank, bank[:, k4 * 128:(k4 + 1) * 128], hr, hr[:, kc * 128:(kc + 1) * 128], ident, ident[:])
                S.cp(alt(), buf, buf[:, half * 4:half * 4 + 4, tb * 128:(tb + 1) * 128],
                     bank, bank[:].rearrange('p (k t) -> p k t', k=4))

    def proj_tile(w_sb, src_tile, buf, bank):
        for kc in range(8):
            S.mm(bank, bank[:], w_sb, w_sb[:, kc, src_tile * 128:(src_tile + 1) * 128], buf, buf[:, kc, :],
                 start=(kc == 0), stop=(kc == 7))

    fcnt = [0]

    def emit_plain(w_sb, src, buf, dst_dram, dst_tile, c0, scale=None):
        bank = PS[fcnt[0] % 4]
        ft = ftile[fcnt[0] % 4]
        fcnt[0] += 1
        proj_tile(w_sb, src, buf, bank)
        if scale is None:
            S.cp(alt(), ft, ft[:], bank, bank[:])
        else:
            S.act(ft, ft[:], bank, bank[:], AF.Copy, scale=scale)
        S.dma('sp', dst_dram[dst_tile, :, c0:c0 + 512], ft[:], reads=[ft], writes=[dst_dram])
        return ft

    def emit_rope(w_sb, src, src_s, buf, dst_dram, dst_tile, c0, cosk, sink, scale=None, km_slot=None):
        i0 = fcnt[0] % 2
        bank = PS[i0 * 2]
        bank_s = PS[i0 * 2 + 1]
        ft = ftile[fcnt[0] % 4]
        t32 = ft32[i0]
        fcnt[0] += 1
        proj_tile(w_sb, src, buf, bank)
        proj_tile(w_sb, src_s, buf, bank_s)
        S.tt('dve', t32, t32[:], bank, bank[:], rope[cosk], rope[cosk][:], ALU.mult)
        S.tt('dve', bank_s, bank_s[:], bank_s, bank_s[:], rope[sink], rope[sink][:], ALU.mult)
        if km_slot is not None:
            S.tt('dve', t32, t32[:], t32, t32[:], bank_s, bank_s[:], ALU.add)
            S.raw('dve', 'tensor_reduce', reads=[t32], writes=[kms],
                  out=kms[:, km_slot[0], km_slot[1]:km_slot[1] + 2], in_=t32[:].rearrange('p (b k) -> p b k', b=2),
                  axis=AX.X, op=ALU.add)
            S.cp('act', ft, ft[:], t32, t32[:])
        elif scale is None:
            S.tt('dve', ft, ft[:], t32, t32[:], bank_s, bank_s[:], ALU.add)
        else:
            S.tt('dve', t32, t32[:], t32, t32[:], bank_s, bank_s[:], ALU.add)
            S.act(ft, ft[:], t32, t32[:], AF.Copy, scale=scale)
        S.dma('sp', dst_dram[dst_tile, :, c0:c0 + 512], ft[:], reads=[ft], writes=[dst_dram])

    for ch in (range(SEQ // 512) if do_k else []):
        c0 = ch * 512
        buf = hT[ch % 2]
        load_hT(hk_rows, c0, buf)
        build_rope(posk, c0)
        for t in range(2):
            emit_plain(wk_sb, KF_SRC['kA'][t], buf, KF, KF_DST['kA'][t], c0)
        for t in range(2):
            emit_rope(wk_sb, KF_SRC['kB1'][t], KF_SRC['kB1s'][t], buf, KF, KF_DST['kB1'][t], c0, 'cosB', 'sinB')
            emit_rope(wk_sb, KF_SRC['kB2'][t], KF_SRC['kB2s'][t], buf, KF, KF_DST['kB2'][t], c0, 'cosB', 'sinB')
            emit_rope(wk_sb, KF_SRC['kC'][t], KF_SRC['kCs'][t], buf, KF, KF_DST['kC'][t], c0, 'cosCD', 'sinCD',
                      km_slot=(t, 2 * ch))
        emit_rope(wk_sb, KF_SRC['ks'][0], KF_SRC['kss'][0], buf, KF, KF_DST['ks'][0], c0, 'cosCD', 'sinCD')
        emit_rope(wk_sb, KF_SRC['kw'][0], KF_SRC['kws'][0], buf, KF, KF_DST['kw'][0], c0, 'cosCD', 'sinCD')
        emit_plain(wk_sb, KF_SRC['kcvc'][0], buf, KF, KF_DST['kcvc'][0], c0)
        for tb in range(4):
            va = vaug[tb % 2]
            b0, b1 = PS[4], PS[5]
            for kc in range(8):
                S.mm(b0, b0[:], buf, buf[:, kc, tb * 128:(tb + 1) * 128], wkt_sb, wkt_sb[:, kc, 0:512],
                     start=(kc == 0), stop=(kc == 7))
            for kc in range(8):
                S.mm(b1, b1[:, 0:384], buf, buf[:, kc, tb * 128:(tb + 1) * 128], wkt_sb, wkt_sb[:, kc, 512:896],
                     start=(kc == 0), stop=(kc == 7))
            S.cp('act', va, va[:, 0:8, 0:64], b0, b0[:].rearrange('p (h d) -> p h d', h=8))
            S.cp('dve', va, va[:, 8:14, 0:64], b1, b1[:, 0:384].rearrange('p (h d) -> p h d', h=6))
            r0 = c0 + tb * 128
            S.dma('sp', VA[r0:r0 + 128, :], va[:].rearrange('p h d -> p (h d)'), reads=[va], writes=[VA])
    if do_k:
        S.ts('dve', kms, kms[:], kms, kms[:], 1.0 / 256.0, None, ALU.mult)
        S.cp('dve', kmean, kmean[:], kms, kms[:])

    for ch in range(TO // 512):
        c0 = ch * 512
        buf = hT[ch % 2]
        load_hT(hq_rows, c0, buf)
        build_rope(posq, c0)
        for t in range(2):
            emit_plain(wq_sb, QF_SRC['qA'][t], buf, QF, QF_DST['qA'][t], c0, scale=0.125)
            emit_rope(wq_sb, QF_SRC['qB'][t], QF_SRC['qBs'][t], buf, QF, QF_DST['qB'][t], c0, 'cosB', 'sinB',
                      scale=32 ** -0.5)
            emit_rope(wq_sb, QF_SRC['qC'][t], QF_SRC['qCs'][t], buf, QF, QF_DST['qC'][t], c0, 'cosCD', 'sinCD',
                      scale=0.125)
            emit_plain(wq_sb, QF_SRC['qD'][t], buf, QF, QF_DST['qD'][t], c0, scale=0.125)
            emit_rope(wq_sb, QF_SRC['qD'][t], QF_SRC['qDs'][t], buf, QF, QF_DST['qDr'][t], c0, 'cosCD', 'sinCD',
                      scale=0.125)
        for tb in range(4):
            g = gsb[tb % 2]
            b0 = PS[4 + tb % 2]
            for kc in range(8):
                S.mm(b0, b0[:, 0:12], buf, buf[:, kc, tb * 128:(tb + 1) * 128], wqt_sb, wqt_sb[:, kc, :],
                     start=(kc == 0), stop=(kc == 7))
            S.act(g, g[:], b0, b0[:, 0:12], AF.Sigmoid)
            r0 = c0 + tb * 128
            S.dma('sp', GT[r0:r0 + 128, :], g[:], reads=[g], writes=[GT])
    S.pop_scope()
    def view4(bank):
        return bank[:, 0:260].rearrange('p (h d) -> p h d', h=4)

    def load_kt(dst, tiles):
        for i_, t_ in enumerate(tiles):
            for hf in range(2):
                S.dma('sp', dst[:, i_, hf * 4096:(hf + 1) * 4096], KF[t_, :, hf * 4096:(hf + 1) * 4096], writes=[dst])

    def load_q(dst, tiles):
        for i_, t_ in enumerate(tiles):
            S.dma('sp', dst[:, i_, :], QF[t_, :, :], writes=[dst])

    def load_v(dst, s0, ns):
        for q4 in range(16):
            S.dma('sp', dst[:, q4 * 4:(q4 + 1) * 4, :],
                  VA[q4 * 512:(q4 + 1) * 512, s0 * 65:(s0 + ns) * 65].rearrange('(kb p) f -> p kb f', p=128),
                  writes=[dst])

    sbk = [0]
    ptb = [0]

    pend = [None]

    def attn_flush():
        if pend[0] is not None:
            f_ = pend[0]
            pend[0] = None
            f_()

    def attn_step(slot, kb, kt, kt_tile_of_h, q, pts, vfn, obank, first, last, mask=None, bias=None, shared_k=False):
        sb = SB2[sbk[0] % 2]
        sbk[0] += 1
        pt = pts[ptb[0] % len(pts)]
        ptb[0] += 1
        for h in (0, 2, 1, 3):
            b = (h % 2) * 64
            S.mm(sb, sb[:, scol(h):scol(h) + 128], kt, kt[b:b + 64, kt_tile_of_h(h), kb * 128:(kb + 1) * 128],
                 q, q[b:b + 64, h // 2, slot * 128:(slot + 1) * 128], start=(h < 2), stop=(bias is None and h >= 2),
                 skip=True)
        if bias is not None:
            bt_, bap_fn, et_, eap = bias
            for par in range(2):
                S.mm(sb, sb[:, par * 512:par * 512 + 256], et_, eap, bt_, bap_fn(par), start=False, stop=True, skip=True)

        def rest():
            S.act(pt, pt[:].rearrange('p (b c) -> p b c', b=2), sb, v2(sb), AF.Exp)
            if mask is not None:
                S.tt('dve', pt, pt[:], pt, pt[:], masks, masks[:, mask, :], ALU.mult)
            for h in range(4):
                vt, vap = vfn(h, kb)
                S.mm(obank, obank[:, h * 65:(h + 1) * 65], pt, pt[:, pcol(h):pcol(h) + 128], vt, vap,
                     start=(first and h == 0), stop=last, skip=True)

        prev = pend[0]
        pend[0] = rest
        if prev is not None:
            prev()

    def causal_mask(slot, kb, strict=False):
        if kb == 2 * slot + 1:
            return M_HI_LT if strict else M_HI_LE
        if kb == 2 * slot:
            return M_LO_LT if strict else M_LO_LE
        return None

    def store_mix(omix, slot, mi):
        S.dma('sp', MIX[slot * 128:(slot + 1) * 128, mi * 256:(mi + 1) * 256], omix[:], reads=[omix], writes=[MIX])

    if 'B' in mixers:
        S.push_scope()
        k1 = S.sbuf([128, 2, SEQ], BF16, 'k1')
        k2 = S.sbuf([128, 2, SEQ], BF16, 'k2')
        qb_ = S.sbuf([128, 2, TO], BF16, 'qB')
        vb = S.sbuf([128, 64, 260], BF16, 'vB')
        load_kt(k1, KF_DST['kB1'])
        load_kt(k2, KF_DST['kB2'])
        load_q(qb_, QF_DST['qB'])
        load_v(vb, 4, 4)
        pts = [S.sbuf([128, 512], BF16, 'ptB%d' % i) for i in range(3)]
        lamt = S.sbuf([128, 128], F32, 'lamt')
        S.dma('sp', lamt[:], dlam[:].partition_broadcast(128), writes=[lamt])
        gainb = S.sbuf([128, 64], F32, 'gainb')
        S.dma('sp', gainb[:], dgain[:].partition_broadcast(128), writes=[gainb])
        S.ts('dve', gainb, gainb[:], gainb, gainb[:], 1.0 - lam_init, None, ALU.mult)
        lp = S.sbuf([128, 64], F32, 'lp')
        ls = S.sbuf([128, 4], F32, 'ls')
        S.tt('dve', lp, lp[:, 0:32], lamt, lamt[:, 0:32], lamt, lamt[:, 32:64], ALU.mult)
        S.tt('dve', lp, lp[:, 32:64], lamt, lamt[:, 64:96], lamt, lamt[:, 96:128], ALU.mult)
        S.raw('dve', 'tensor_reduce', reads=[lp], writes=[ls], out=ls[:, 0:2],
              in_=lp[:].rearrange('p (a b) -> p a b', a=2), axis=AX.X, op=ALU.add)
        S.act(ls, ls[:, 0:2], ls, ls[:, 0:2], AF.Exp)
        S.tt('dve', ls, ls[:, 2:3], ls, ls[:, 1:2], ls, ls[:, 0:1], ALU.subtract)
        S.ts('dve', ls, ls[:, 2:3], ls, ls[:, 2:3], -lam_init, None, ALU.add)
        rd = S.sbuf([128, 8], F32, 'rdB')
        ob = S.sbuf([128, 4, 64], F32, 'obB')
        sq = S.sbuf([128, 4, 64], F32, 'sqB')
        ss = S.sbuf([128, 4], F32, 'ssB')
        omixs = [S.sbuf([128, 256], F32, 'omixB%d' % i) for i in range(2)]
        for si, slot in enumerate(slots):
            nkb = 2 * slot + 2
            for c_, kt_ in enumerate((k1, k2)):
                obank = PS[4 + c_]
                for kb in range(nkb):
                    attn_step(slot, kb, kt_, lambda h: h // 2, qb_, pts, lambda h, kb: (vb, vb[:, kb, h * 65:(h + 1) * 65]),
                              obank, kb == 0, kb == nkb - 1, mask=causal_mask(slot, kb))
            attn_flush()
            o1, o2 = view4(PS[4]), view4(PS[5])
            S.raw('dve', 'reciprocal', reads=[PS[4]], writes=[rd], out=rd[:, 0:4], in_=o1[:, :, 64])
            S.raw('dve', 'reciprocal', reads=[PS[5]], writes=[rd], out=rd[:, 4:8], in_=o2[:, :, 64])
            S.ts('dve', rd, rd[:, 4:8], rd, rd[:, 4:8], ls[:, 2:3], None, ALU.mult, reads=[ls])
            omix = omixs[si % 2]
            for h in range(4):
                S.ts('dve', ob, ob[:, h, :], PS[4], o1[:, h, 0:64], rd[:, h:h + 1], None, ALU.mult, reads=[rd])
                S.stt(ob, ob[:, h, :], PS[5], o2[:, h, 0:64], rd[:, 4 + h:5 + h], ob, ob[:, h, :], ALU.mult, ALU.add, reads=[rd])
            S.tt('dve', sq, sq[:], ob, ob[:], ob, ob[:], ALU.mult)
            S.raw('dve', 'tensor_reduce', reads=[sq], writes=[ss], out=ss[:], in_=sq[:], axis=AX.X, op=ALU.add)
            S.ts('dve', ss, ss[:], ss, ss[:], 1.0 / 64.0, 1e-5, ALU.mult, ALU.add)
            S.act(ss, ss[:], ss, ss[:], AF.Sqrt)
            S.raw('dve', 'reciprocal', reads=[ss], writes=[ss], out=ss[:], in_=ss[:])
            for h in range(4):
                S.stt(omix, omix[:, h * 64:(h + 1) * 64], ob, ob[:, h, :], ss[:, h:h + 1], gainb, gainb[:],
                      ALU.mult, ALU.mult, reads=[ss])
            store_mix(omix, slot, 1)
        S.pop_scope()

    if 'C' in mixers:
        S.push_scope()
        kc_ = S.sbuf([128, 2, SEQ], BF16, 'kC')
        qc_ = S.sbuf([128, 2, TO], BF16, 'qC')
        vc_ = S.sbuf([128, 64, 260], BF16, 'vC')
        load_kt(kc_, KF_DST['kC'])
        load_q(qc_, QF_DST['qC'])
        load_v(vc_, 8, 4)
        emoba = S.sbuf([128, 64 * 128], BF16, 'emoba')
        S.memset('pool', emoba, emoba[:], 0.0)
        S.dma('pool', emoba[0:32, :], c_emoba[:], writes=[emoba])
        pts = [S.sbuf([128, 512], BF16, 'ptC%d' % i) for i in range(3)]
        gbuf = S.sbuf([128, 4, 32], F32, 'gbuf')
        top8 = S.sbuf([128, 4, 8], F32, 'top8')
        selb = S.sbuf([128, 4, 32], F32, 'selb')
        biasT = S.sbuf([128, 512], BF16, 'biasT')
        S.memset('pool', biasT, biasT[:], 0.0)
        rd = S.sbuf([128, 4], F32, 'rdC')
        omixs = [S.sbuf([128, 256], F32, 'omixC%d' % i) for i in range(2)]
        for si, slot in enumerate(slots):
            own = slot
            nkb = 2 * slot + 2
            if own > 0:
                for h in range(4):
                    b = (h % 2) * 64
                    g = PS[6 + h % 2]
                    S.mm(g, g[:, (h // 2) * 32:(h // 2 + 1) * 32], qc_, qc_[b:b + 64, h // 2, slot * 128:(slot + 1) * 128],
                         kmean, kmean[b:b + 64, h // 2, :], start=True, stop=True)
                S.memset('pool', gbuf, gbuf[:], -BIG)
                for par in range(2):
                    g = PS[6 + par]
                    S.cp('dve', gbuf, gbuf[:, par * 2:par * 2 + 2, 0:own], g,
                         g[:, 0:64].rearrange('p (h n) -> p h n', h=2)[:, :, 0:own])
                for gi in range(4):
                    S.raw('dve', 'max', reads=[gbuf], writes=[top8], out=top8[:, gi, :], in_=gbuf[:, gi, :])
                for gi in range(4):
                    S.ts('dve', selb, selb[:, gi, :], gbuf, gbuf[:, gi, :], top8[:, gi, 2:3], 1.0, ALU.is_ge, ALU.subtract,
                         reads=[top8])
                S.ts('dve', selb, selb[:], selb, selb[:], -NEG, None, ALU.mult)
                tb_ = PS[7]
                for gi in range(4):
                    S.tr(tb_, tb_[0:32, gi * 128:(gi + 1) * 128], selb, selb[:, gi, :], ident, ident[:])
                S.cp('act', biasT, biasT[0:32, :], tb_, tb_[0:32, :])
            obank = PS[4 + si % 2]
            for kb in range(nkb):
                bias = None
                if kb < 2 * own:
                    bias = (biasT, lambda par: biasT[:, par * 256:(par + 1) * 256], emoba, emoba[:, kb * 128:(kb + 1) * 128])
                attn_step(slot, kb, kc_, lambda h: h // 2, qc_, pts, lambda h, kb: (vc_, vc_[:, kb, h * 65:(h + 1) * 65]),
                          obank, kb == 0, kb == nkb - 1, mask=causal_mask(slot, kb), bias=bias)
            attn_flush()
            o1 = view4(obank)
            S.raw('dve', 'reciprocal', reads=[obank], writes=[rd], out=rd[:], in_=o1[:, :, 64])
            omix = omixs[si % 2]
            for h in range(4):
                S.ts('dve', omix, omix[:, h * 64:(h + 1) * 64], obank, o1[:, h, 0:64], rd[:, h:h + 1], None, ALU.mult,
                     reads=[rd])
            store_mix(omix, slot, 2)
        S.pop_scope()

    if 'A' in mixers:
        S.push_scope()
        ka = S.sbuf([128, 2, SEQ], BF16, 'kA')
        qa = S.sbuf([128, 2, TO], BF16, 'qA')
        va_ = S.sbuf([128, 64, 260], BF16, 'vA')
        load_kt(ka, KF_DST['kA'])
        load_q(qa, QF_DST['qA'])
        load_v(va_, 0, 4)
        negtri = S.sbuf([128, 128], BF16, 'negtri')
        negones = S.sbuf([128, 128], BF16, 'negones')
        S.dma('pool', negtri[:], c_negtri[:], writes=[negtri])
        S.dma('pool', negones[:], c_negones[:], writes=[negones])
        pts = [S.sbuf([128, 512], BF16, 'ptA%d' % i) for i in range(3)]
        ee = [S.sbuf([128, 512], F32, 'eeA%d' % i) for i in range(2)]
        ll = [S.sbuf([128, 512], BF16, 'llA%d' % i) for i in range(3)]
        lacc = [S.sbuf([128, 512], BF16, 'laccA%d' % i) for i in range(2)]
        omixs = [S.sbuf([128, 256], F32, 'omixA%d' % i) for i in range(2)]
        stepi = 0
        for si, slot in enumerate(slots):
            nkb = 2 * slot + 2
            obank = PS[4 + si % 2]
            kbs = list(range(nkb - 1, -1, -1))
            N_ = len(kbs)
            base = stepi
            stepi += N_
            la = [None] * N_

            def ph1(n):
                kb = kbs[n]
                g = base + n
                zb, e_, l_ = SB2[g % 2], ee[g % 2], ll[g % 3]
                for h in (0, 2, 1, 3):
                    b = (h % 2) * 64
                    S.mm(zb, zb[:, scol(h):scol(h) + 128], ka, ka[b:b + 64, h // 2, kb * 128:(kb + 1) * 128],
                         qa, qa[b:b + 64, h // 2, slot * 128:(slot + 1) * 128], start=(h < 2), stop=False, skip=True)
                S.act(e_, e_[:].rearrange('p (b c) -> p b c', b=2), zb, v2(zb), AF.Exp)
                S.act(l_, l_[:], e_, e_[:], AF.Ln, bias=1.0)
                m = causal_mask(slot, kb, strict=True)
                if m is not None:
                    S.tt('dve', l_, l_[:], l_, l_[:], masks, masks[:, m, :], ALU.mult)

            def ph2(n):
                kb = kbs[n]
                g = base + n
                zb, l_, pt = SB2[g % 2], ll[g % 3], pts[g % 3]
                prev = la[n - 1] if n > 0 else None
                for par in range(2):
                    S.mm(zb, zb[:, par * 512:par * 512 + 256], negtri, negtri[:], l_, l_[:, par * 256:(par + 1) * 256],
                         start=False, stop=(prev is None), skip=True)
                if prev is not None:
                    for par in range(2):
                        S.mm(zb, zb[:, par * 512:par * 512 + 256], negones, negones[:], prev,
                             prev[:, par * 256:(par + 1) * 256], start=False, stop=True, skip=True)
                S.act(pt, pt[:].rearrange('p (b c) -> p b c', b=2), zb, v2(zb), AF.Exp)
                m = causal_mask(slot, kb, strict=True)
                if m is not None:
                    S.tt('dve', pt, pt[:], pt, pt[:], masks, masks[:, m, :], ALU.mult)
                if n < N_ - 1:
                    cur = lacc[n % 2]
                    if prev is None:
                        S.cp('pool', cur, cur[:], l_, l_[:])
                    else:
                        S.tt('pool', cur, cur[:], prev, prev[:], l_, l_[:], ALU.add)
                    la[n] = cur

            def ph3(n):
                kb = kbs[n]
                pt = pts[(base + n) % 3]
                for h in range(4):
                    S.mm(obank, obank[:, h * 65:(h + 1) * 65], pt, pt[:, pcol(h):pcol(h) + 128],
                         va_, va_[:, kb, h * 65:(h + 1) * 65], start=(n == 0 and h == 0), stop=(n == N_ - 1), skip=True)

            ph1(0)
            for n in range(N_):
                if n + 1 < N_:
                    ph1(n + 1)
                ph2(n)
                if n >= 1:
                    ph3(n - 1)
            ph3(N_ - 1)
            omix = omixs[si % 2]
            o1 = view4(obank)
            S.cp('dve', omix, omix[:].rearrange('p (h d) -> p h d', h=4), obank, o1[:, :, 0:64])
            store_mix(omix, slot, 0)
        S.pop_scope()
    if 'D' in mixers:
        S.push_scope()
        ktd = S.sbuf([128, 2, SEQ], BF16, 'ktD')
        load_kt(ktd, (KF_DST['ks'][0], KF_DST['kw'][0]))
        qd = S.sbuf([128, 2, TO], BF16, 'qD')
        qdr = S.sbuf([128, 2, TO], BF16, 'qDr')
        load_q(qd, QF_DST['qD'])
        load_q(qdr, QF_DST['qDr'])
        vd = S.sbuf([128, 64, 130], BF16, 'vD')
        load_v(vd, 12, 2)
        esel = S.sbuf([128, 64 * 128], BF16, 'esel')
        for hf in range(2):
            S.dma('pool', esel[:, hf * 4096:(hf + 1) * 4096], c_esel[:, hf * 4096:(hf + 1) * 4096], writes=[esel])
        ovl = S.sbuf([128, 512], BF16, 'ovl')
        S.dma('pool', ovl[:], c_overlap[:], writes=[ovl])
        xkv = S.sbuf([128, SEQ], BF16, 'xkv')
        S.dma('sp', xkv[:], KF[KF_DST['kcvc'][0], :, :], writes=[xkv])
        w1 = S.sbuf([128, 32, 256], BF16, 'cw1')
        posT = S.sbuf([128, 32], BF16, 'cposT')
        w2 = S.sbuf([128, 2, 2, 128], BF16, 'cw2')
        for kv in range(2):
            S.dma('pool', w1[kv * 64:(kv + 1) * 64, :, :], cw1[kv].rearrange('(l d) f -> d l f', d=64), writes=[w1])
            S.dma('pool', posT[kv * 64:(kv + 1) * 64, :], cpos[kv].rearrange('(l d) -> d l', d=64), writes=[posT],
                  allow_slow_non_contiguous=True)
            for dup in range(2):
                S.dma('pool', w2[:, kv, :, dup * 64:(dup + 1) * 64], cw2[kv].rearrange('(hc p) d -> p hc d', p=128),
                      writes=[w2])
        hid = [[S.sbuf([128, 512], BF16, 'hid%d%d' % (kv, hc)) for hc in range(2)] for kv in range(2)]
        pb = S.sbuf([128, 4], F32, 'cpb')
        for kv in range(2):
            base = kv * 64
            x3 = xkv[base:base + 64, :].rearrange('p (n s) -> p n s', s=16)
            for hc in range(2):
                S.memset('pool', hid[kv][hc], hid[kv][hc][:], 0.0)
                bank = PS[kv * 4 + hc]
                bank2 = PS[kv * 4 + 2 + hc]
                for l in range(32):
                    S.mm(bank, bank[:, 0:511], w1, w1[base:base + 64, l, hc * 128:(hc + 1) * 128],
                         xkv, x3[:, (l // 16):(l // 16) + 511, l % 16], start=(l == 0), stop=(l == 31))
                for l in range(32):
                    S.mm(bank2, bank2[:, 0:1], w1, w1[base:base + 64, l, hc * 128:(hc + 1) * 128],
                         posT, posT[base:base + 64, l:l + 1], start=(l == 0), stop=(l == 31))
                col = kv * 2 + hc
                S.cp('dve', pb, pb[:, col:col + 1], bank2, bank2[:, 0:1])
                S.act(hid[kv][hc], hid[kv][hc][:, 0:511], bank, bank[:, 0:511], AF.Silu, reads=[pb],
                      bias=pb[:, col:col + 1])
        kcT = S.sbuf([128, 512], BF16, 'kcT')
        S.memset('pool', kcT, kcT[:], 0.0)
        bank = PS[4]
        for hc in range(2):
            S.mm(bank, bank[:, 0:511], w2, w2[:, 0, hc, :], hid[0][hc], hid[0][hc][:, 0:511], start=(hc == 0), stop=(hc == 1))
        S.cp('dve', kcT, kcT[:, 0:511], bank, bank[:, 0:511])
        vcaug = S.sbuf([128, 4, 65], BF16, 'vcaug')
        S.memset('pool', vcaug, vcaug[:], 1.0)
        for cidx in range(4):
            bank = PS[5 + cidx % 2]
            for hc in range(2):
                S.mm(bank, bank[:, 0:64], hid[1][hc], hid[1][hc][:, cidx * 128:(cidx + 1) * 128], w2, w2[:, 1, hc, 0:64],
                     start=(hc == 0), stop=(hc == 1))
            S.cp('dve', vcaug, vcaug[:, cidx, 0:64], bank, bank[:, 0:64])
        S.barrier()
        pts = [S.sbuf([128, 512], BF16, 'ptD%d' % i) for i in range(3)]
        ec = [S.sbuf([128, 512], BF16, 'ecD%d' % i) for i in range(4)]
        tqb = S.sbuf([128, 128], F32, 'tqb')
        cm = S.sbuf([128, 128], BF16, 'cmD')
        selm = S.sbuf([128, 128], F32, 'selmD')
        imp = S.sbuf([128, 128], F32, 'impD')
        imp3 = S.sbuf([128, 128], F32, 'imp3D')
        t16 = S.sbuf([128, 16], F32, 't16D')
        bsel = S.sbuf([128, 128], F32, 'bselD')
        biasT4 = S.sbuf([128, 512], BF16, 'biasT4')
        rdd = S.sbuf([128, 12], F32, 'rdD')
        gts = S.sbuf([128, 12], F32, 'gtsD')
        coef = S.sbuf([128, 12], F32, 'coefD')
        omixs = [S.sbuf([128, 256], F32, 'omixD%d' % i) for i in range(2)]
        OC, OS, OW, UB = PS[4], PS[5], PS[6], PS[7]
        for si, slot in enumerate(slots):
            S.dma('sp', tqb[:], c_tq[0:1, slot * 128:(slot + 1) * 128].partition_broadcast(128), writes=[tqb])
            S.dma('sp', selm[:], c_selm[:, slot * 128:(slot + 1) * 128], writes=[selm])
            S.dma('sp', gts[:], GT[slot * 128:(slot + 1) * 128, :], writes=[gts])
            cids = [ci for ci in range(4) if 128 * ci <= 16 * slot + 14]
            for n_, ci in enumerate(cids):
                sb = SB2[sbk[0] % 2]
                sbk[0] += 1
                e_ = ec[ci]
                for h in (0, 2, 1, 3):
                    b = (h % 2) * 64
                    S.mm(sb, sb[:, scol(h):scol(h) + 128], kcT, kcT[b:b + 64, ci * 128:(ci + 1) * 128],
                         qd, qd[b:b + 64, h // 2, slot * 128:(slot + 1) * 128], start=True, stop=True)
                S.act(e_, e_[:].rearrange('p (b c) -> p b c', b=2), sb, v2(sb), AF.Exp)
                if 128 * ci + 127 > 16 * slot - 2:
                    S.ts('dve', cm, cm[:], tqb, tqb[:], ptab[:, 4 + ci:5 + ci], None, ALU.is_ge, reads=[ptab])
                    for h in range(4):
                        S.tt('dve', e_, e_[:, h * 128:(h + 1) * 128], e_, e_[:, h * 128:(h + 1) * 128], cm, cm[:], ALU.mult)
                for h in range(4):
                    S.mm(OC, OC[:, h * 65:(h + 1) * 65], e_, e_[:, pcol(h):pcol(h) + 128], vcaug, vcaug[:, ci, :],
                         start=(n_ == 0 and h == 0), stop=(n_ == len(cids) - 1), skip=True)
            for h in range(4):
                for n_, ci in enumerate(cids):
                    S.mm(UB, UB[:, h * 128:(h + 1) * 128], ec[ci], ec[ci][:, pcol(h):pcol(h) + 128],
                         ovl, ovl[:, ci * 128:(ci + 1) * 128], start=(n_ == 0), stop=(n_ == len(cids) - 1))
            oc4 = view4(OC)
            S.ts('dve', rdd, rdd[:, 0:4], OC, oc4[:, :, 64], 1e-30, None, ALU.add)
            S.raw('dve', 'reciprocal', reads=[rdd], writes=[rdd], out=rdd[:, 0:4], in_=rdd[:, 0:4])
            S.ts('dve', imp, imp[:], UB, UB[:, 0:128], rdd[:, 0:1], None, ALU.mult, reads=[rdd])
            for h in range(1, 4):
                S.stt(imp, imp[:], UB, UB[:, h * 128:(h + 1) * 128], rdd[:, h:h + 1], imp, imp[:], ALU.mult, ALU.add,
                      reads=[rdd])
            S.tt('dve', imp, imp[:], imp, imp[:], selm, selm[:], ALU.add)
            S.raw('dve', 'max', reads=[imp], writes=[t16], out=t16[:, 0:8], in_=imp[:])
            S.raw('dve', 'match_replace', reads=[imp, t16], writes=[imp3], out=imp3[:], in_to_replace=t16[:, 0:8],
                  in_values=imp[:], imm_value=-BIG)
            S.raw('dve', 'max', reads=[imp3], writes=[t16], out=t16[:, 8:16], in_=imp3[:])
            S.ts('dve', t16, t16[:, 15:16], t16, t16[:, 15:16], -1e29, None, ALU.max)
            S.ts('dve', bsel, bsel[:], imp, imp[:], t16[:, 15:16], 1.0, ALU.is_ge, ALU.subtract, reads=[t16])
            S.ts('dve', bsel, bsel[:], bsel, bsel[:], -NEG, None, ALU.mult)
            tb_ = UB
            S.tr(tb_, tb_[:, 0:128], bsel, bsel[:], ident, ident[:])
            for h in range(4):
                S.cp(alt(), biasT4, biasT4[:, h * 128:(h + 1) * 128], tb_, tb_[:, 0:128])
            nkb = 2 * slot + 2
            for kb in range(nkb):
                attn_step(slot, kb, ktd, lambda h: 0, qdr, pts, lambda h, kb: (vd, vd[:, kb, 0:65]), OS, kb == 0,
                          kb == nkb - 1, mask=causal_mask(slot, kb),
                          bias=(biasT4, lambda par: biasT4[:, par * 256:(par + 1) * 256], esel,
                                esel[:, kb * 128:(kb + 1) * 128]))
            wk = [kb for kb in range(2 * slot - 4, 2 * slot + 2) if kb >= 0]
            for n_, kb in enumerate(wk):
                m = causal_mask(slot, kb)
                if kb == 2 * slot - 4:
                    m = M_W4
                elif kb == 2 * slot - 3:
                    m = M_W3
                attn_step(slot, kb, ktd, lambda h: 1, qdr, pts, lambda h, kb: (vd, vd[:, kb, 65:130]), OW, n_ == 0,
                          n_ == len(wk) - 1, mask=m)
            attn_flush()
            os4, ow4 = view4(OS), view4(OW)
            S.raw('dve', 'reciprocal', reads=[OS], writes=[rdd], out=rdd[:, 4:8], in_=os4[:, :, 64])
            S.raw('dve', 'reciprocal', reads=[OW], writes=[rdd], out=rdd[:, 8:12], in_=ow4[:, :, 64])
            g3 = gts[:].rearrange('p (h b) -> p h b', b=3)
            c3 = coef[:].rearrange('p (b h) -> p b h', b=3)
            for br in range(3):
                S.tt('dve', coef, c3[:, br, :], gts, g3[:, :, br], rdd, rdd[:, br * 4:(br + 1) * 4], ALU.mult)
            omix = omixs[si % 2]
            for h in range(4):
                oh = omix[:, h * 64:(h + 1) * 64]
                S.ts('dve', omix, oh, OC, oc4[:, h, 0:64], coef[:, h:h + 1], None, ALU.mult, reads=[coef])
                S.stt(omix, oh, OS, os4[:, h, 0:64], coef[:, 4 + h:5 + h], omix, oh, ALU.mult, ALU.add, reads=[coef])
                S.stt(omix, oh, OW, ow4[:, h, 0:64], coef[:, 8 + h:9 + h], omix, oh, ALU.mult, ALU.add, reads=[coef])
            store_mix(omix, slot, 3)
        S.pop_scope()
    if not do_tail:
        return
    H1 = S.dram('H1' + sfx, [TO, DM], F32)
    H1T = S.dram('H1T' + sfx, [8, 128, TO], BF16)
    S.push_scope()
    lnv = S.sbuf([128, 4, DM], F32, 'lnv')
    for i_ in range(4):
        S.dma('sp', lnv[:, i_, :], vecs[i_, :].partition_broadcast(128), writes=[lnv])
    st = S.sbuf([128, 8], F32, 'lnst')
    junk = S.sbuf([128, DM], F32, 'lnjunk')

    def layer_norm(x, out, gi):
        S.act(junk, junk[:], x, x[:], AF.Copy, writes=[st], accum_out=st[:, 0:1])
        S.act(junk, junk[:], x, x[:], AF.Square, writes=[st], accum_out=st[:, 1:2])
        S.ts('dve', st, st[:, 0:2], st, st[:, 0:2], 1.0 / DM, None, ALU.mult)
        S.tt('dve', st, st[:, 2:3], st, st[:, 0:1], st, st[:, 0:1], ALU.mult)
        S.tt('dve', st, st[:, 3:4], st, st[:, 1:2], st, st[:, 2:3], ALU.subtract)
        S.ts('dve', st, st[:, 3:4], st, st[:, 3:4], 1e-5, None, ALU.add)
        S.act(st, st[:, 4:5], st, st[:, 3:4], AF.Sqrt)
        S.raw('dve', 'reciprocal', reads=[st], writes=[st], out=st[:, 5:6], in_=st[:, 4:5])
        S.tt('dve', st, st[:, 6:7], st, st[:, 0:1], st, st[:, 5:6], ALU.mult)
        S.ts('dve', st, st[:, 6:7], st, st[:, 6:7], -1.0, None, ALU.mult)
        S.act(out, out[:], x, x[:], AF.Identity, reads=[st], scale=st[:, 5:6], bias=st[:, 6:7])
        S.tt('dve', out, out[:], out, out[:], lnv, lnv[:, gi, :], ALU.mult)
        S.tt('pool', out, out[:], out, out[:], lnv, lnv[:, gi + 1, :], ALU.add)

    def transpose_rows(x, ncol, dst, dst_ap_fn):
        for g0 in range(0, ncol, 4):
            n = min(4, ncol - g0)
            bank = PS[6 + (g0 // 4) % 2]
            for k4 in range(n):
                S.tr(bank, bank[:, k4 * 128:(k4 + 1) * 128], x, x[:, (g0 + k4) * 128:(g0 + k4 + 1) * 128], ident, ident[:])
            S.cp(alt(), dst, dst_ap_fn(g0, n), bank, bank[:, 0:n * 128].rearrange('p (k t) -> p k t', k=n))

    S.push_scope()
    wo = S.sbuf([128, 8, DM], BF16, 'wo')
    S.dma('pool', wo[:], wout[:].rearrange('(k p) f -> p k f', p=128), writes=[wo])
    mrow = [S.sbuf([128, DM], F32, 'mrow%d' % i) for i in range(2)]
    hrw = [S.sbuf([128, DM], F32, 'hrw%d' % i) for i in range(2)]
    mT = [S.sbuf([128, 8, 128], BF16, 'mT%d' % i) for i in range(2)]
    yy = [S.sbuf([128, DM], F32, 'yy%d' % i) for i in range(2)]
    h1s = [S.sbuf([128, DM], F32, 'h1s%d' % i) for i in range(2)]
    h1t = [S.sbuf([128, 8, 128], BF16, 'h1t%d' % i) for i in range(2)]
    for tb in range(NSLOT):
        r0 = tb * 128
        mr, hr, mt, y, h1, ht = mrow[tb % 2], hrw[tb % 2], mT[tb % 2], yy[tb % 2], h1s[tb % 2], h1t[tb % 2]
        S.dma('sp', mr[:], MIX[r0:r0 + 128, :], reads=[MIX], writes=[mr])
        st_, sap_ = hq_rows(r0)
        S.dma('sp', hr[:], sap_, reads=[st_], writes=[hr])
        transpose_rows(mr, 8, mt, lambda g0, n, mt=mt: mt[:, g0:g0 + n, :])
        for hd in range(2):
            bank = PS[hd]
            for kc in range(8):
                S.mm(bank, bank[:], mt, mt[:, kc, :], wo, wo[:, kc, hd * 512:(hd + 1) * 512], start=(kc == 0), stop=(kc == 7))
            S.stt(y, y[:, hd * 512:(hd + 1) * 512], hr, hr[:, hd * 512:(hd + 1) * 512], ALPHA, bank, bank[:], ALU.mult, ALU.add)
        layer_norm(y, h1, 0)
        S.dma('sp', H1[r0:r0 + 128, :], h1[:], reads=[h1], writes=[H1])
        transpose_rows(h1, 8, ht, lambda g0, n, ht=ht: ht[:, g0:g0 + n, :])
        S.dma('sp', H1T[:, :, r0:r0 + 128].rearrange('k p t -> p k t'), ht[:], reads=[ht], writes=[H1T])
    S.pop_scope()

    S.push_scope()
    TG = 1024
    NTB = TG // 128
    h1T = S.sbuf([128, 8, TG], BF16, 'h1T')
    facc = S.sbuf([128, NTB, DM], F32, 'facc')
    gT = S.sbuf([128, 7, TG], BF16, 'gT')
    w2sc = [S.sbuf([128, 7, DM], BF16, 'w2sc%d' % i) for i in range(2)]
    w1c = [S.sbuf([128, 8, 128], BF16, 'w1c%d' % i) for i in range(2)]
    w3c = [S.sbuf([128, 8, 128], BF16, 'w3c%d' % i) for i in range(2)]
    sa = [S.sbuf([128, 512], BF16, 'sa%d' % i) for i in range(2)]
    wr_sb = S.sbuf([128, 8, 8], BF16, 'wr_sb')
    S.dma('pool', wr_sb[:], wrt[:].rearrange('(k p) f -> p k f', p=128), writes=[wr_sb])
    lg = S.sbuf([128, 8], F32, 'lg')
    tp8 = S.sbuf([128, 8], F32, 'tp8')
    gg = S.sbuf([128, 4], F32, 'gg')
    m12 = S.sbuf([128, 16], F32, 'm12')
    gate = S.sbuf([128, NTB, 8], F32, 'gate')
    pg = S.sbuf([128, 8, DM], BF16, 'pleg_sb')
    pp = S.sbuf([128, 2, DM], BF16, 'plep_sb')
    S.dma('pool', pg[:], pleg[:].rearrange('(k p) f -> p k f', p=128), writes=[pg])
    S.dma('pool', pp[:], plep[:].rearrange('(k p) f -> p k f', p=128), writes=[pp])
    h1r = [S.sbuf([128, DM], F32, 'h1r%d' % i) for i in range(2)]
    y2 = S.sbuf([128, DM], F32, 'y2')
    h2 = [S.sbuf([128, DM], F32, 'h2_%d' % i) for i in range(2)]
    h2T = S.sbuf([128, 8, 128], BF16, 'h2T')
    prow = S.sbuf([128, 256], F32, 'prow')
    pT = S.sbuf([128, 2, 128], BF16, 'pT')
    sg = S.sbuf([128, 512], F32, 'sg')
    oo = [S.sbuf([128, DM], F32, 'oo%d' % i) for i in range(2)]
    wi = 0
    for grp in range(TO // TG):
        t0 = grp * TG
        S.dma('sp', h1T[:], H1T[:, :, t0:t0 + TG].rearrange('k p t -> p k t'), reads=[H1T], writes=[h1T])
        if moe:
            for tb in range(NTB):
                bank = PS[6 + tb % 2]
                for kc in range(8):
                    S.mm(bank, bank[:, 0:8], h1T, h1T[:, kc, tb * 128:(tb + 1) * 128], wr_sb, wr_sb[:, kc, :],
                         start=(kc == 0), stop=(kc == 7))
                S.cp('dve', lg, lg[:], bank, bank[:, 0:8])
                S.raw('dve', 'max', reads=[lg], writes=[tp8], out=tp8[:], in_=lg[:])
                S.tt('dve', gg, gg[:, 0:1], tp8, tp8[:, 1:2], tp8, tp8[:, 0:1], ALU.subtract)
                S.act(gg, gg[:, 1:2], gg, gg[:, 0:1], AF.Sigmoid)
                S.ts('dve', gg, gg[:, 2:3], gg, gg[:, 1:2], -1.0, 1.0, ALU.mult, ALU.add)
                S.ts('dve', m12, m12[:, 0:8], lg, lg[:], tp8[:, 0:1], gg[:, 2:3], ALU.is_equal, ALU.mult, reads=[tp8, gg])
                S.ts('dve', m12, m12[:, 8:16], lg, lg[:], tp8[:, 1:2], gg[:, 1:2], ALU.is_equal, ALU.mult, reads=[tp8, gg])
                S.tt('dve', gate, gate[:, tb, :], m12, m12[:, 0:8], m12, m12[:, 8:16], ALU.add)
        for e_ in range(ne):
            for sc in range(4):
                w2t = w2sc[(e_ * 4 + sc) % 2]
                S.dma('pool', w2t[:], fw2[e_, sc * 896:(sc + 1) * 896, :].rearrange('(j p) f -> p j f', p=128), writes=[w2t])
                for j in range(7):
                    ffc = sc * 7 + j
                    a_w, b_w = w1c[wi % 2], w3c[wi % 2]
                    wi += 1
                    S.dma('pool', a_w[:], fw1[e_, :, ffc * 128:(ffc + 1) * 128].rearrange('(k p) f -> p k f', p=128), writes=[a_w])
                    S.dma('pool', b_w[:], fw3[e_, :, ffc * 128:(ffc + 1) * 128].rearrange('(k p) f -> p k f', p=128), writes=[b_w])
                    for hf in range(TG // 512):
                        ba, bb = PS[(hf % 2) * 2], PS[(hf % 2) * 2 + 1]
                        for kc in range(8):
                            S.mm(ba, ba[:], a_w, a_w[:, kc, :], h1T, h1T[:, kc, hf * 512:(hf + 1) * 512], start=(kc == 0), stop=(kc == 7))
                        for kc in range(8):
                            S.mm(bb, bb[:], b_w, b_w[:, kc, :], h1T, h1T[:, kc, hf * 512:(hf + 1) * 512], start=(kc == 0), stop=(kc == 7))
                        s_ = sa[hf % 2]
                        S.act(s_, s_[:], ba, ba[:], AF.Silu)
                        S.tt('dve', gT, gT[:, j, hf * 512:(hf + 1) * 512], s_, s_[:], bb, bb[:], ALU.mult)
                for tb in range(NTB):
                    for hd in range(2):
                        bank = PS[4 + (tb * 2 + hd) % 4]
                        for j in range(7):
                            S.mm(bank, bank[:], gT, gT[:, j, tb * 128:(tb + 1) * 128], w2t, w2t[:, j, hd * 512:(hd + 1) * 512],
                                 start=(j == 0), stop=(j == 6))
                        fa = facc[:, tb, hd * 512:(hd + 1) * 512]
                        first = (e_ == 0 and sc == 0)
                        if moe:
                            gsc = gate[:, tb, e_:e_ + 1]
                            if first:
                                S.ts('dve', facc, fa, bank, bank[:], gsc, None, ALU.mult, reads=[gate])
                            else:
                                S.stt(facc, fa, bank, bank[:], gsc, facc, fa, ALU.mult, ALU.add, reads=[gate])
                        else:
                            if first:
                                S.cp('dve', facc, fa, bank, bank[:])
                            else:
                                S.tt('dve', facc, fa, facc, fa, bank, bank[:], ALU.add)
        for tb in range(NTB):
            r0 = t0 + tb * 128
            hr, h2_, o_ = h1r[tb % 2], h2[tb % 2], oo[tb % 2]
            S.dma('sp', hr[:], H1[r0:r0 + 128, :], reads=[H1], writes=[hr])
            st_, sap_ = p_rows(r0)
            S.dma('sp', prow[:], sap_, reads=[st_], writes=[prow])
            S.stt(y2, y2[:], hr, hr[:], ALPHA, facc, facc[:, tb, :], ALU.mult, ALU.add)
            layer_norm(y2, h2_, 2)
            transpose_rows(h2_, 8, h2T, lambda g0, n: h2T[:, g0:g0 + n, :])
            transpose_rows(prow, 2, pT, lambda g0, n: pT[:, g0:g0 + n, :])
            for hd in range(2):
                bg, bp = PS[hd * 2], PS[hd * 2 + 1]
                for kc in range(8):
                    S.mm(bg, bg[:], h2T, h2T[:, kc, :], pg, pg[:, kc, hd * 512:(hd + 1) * 512], start=(kc == 0), stop=(kc == 7))
                for k2 in range(2):
                    S.mm(bp, bp[:], pT, pT[:, k2, :], pp, pp[:, k2, hd * 512:(hd + 1) * 512], start=(k2 == 0), stop=(k2 == 1))
                S.act(sg, sg[:], bg, bg[:], AF.Sigmoid)
                S.tt('dve', sg, sg[:], sg, sg[:], bp, bp[:], ALU.mult)
                S.tt('pool', o_, o_[:, hd * 512:(hd + 1) * 512], sg, sg[:], h2_, h2_[:, hd * 512:(hd + 1) * 512], ALU.add)
            ot_, oap_ = out_rows(r0)
            S.dma('sp', oap_, o_[:], reads=[o_], writes=[ot_])
    S.pop_scope()
    S.pop_scope()
    return


WNAMES = ['wkf', 'wqf', 'wkt', 'wqt', 'wout', 'vecs', 'dlam', 'dgain', 'cpos', 'cw1', 'cw2', 'fw1', 'fw3', 'fw2',
          'wrt', 'plep', 'pleg']


def build_fused(cfg=None):
    nc = bass.Bass("TRN2", target_bir_lowering=False)
    S = Sched(nc)
    EI = 'ExternalInput'
    G = dict(nc=nc, S=S)
    hfull = S.dram('hfull', [SEQ, DM], F32, EI)
    posfull = S.dram('posfull', [SEQ], I32, EI)
    posown = S.dram('posown', [TO], I32, EI)
    pfull0 = S.dram('pfull0', [SEQ, 256], F32, EI)
    pown1 = S.dram('pown1', [TO, 256], F32, EI)
    parv = S.dram('parv', [128, 2], F32, EI)
    hout = S.dram('hout', [TO, DM], F32, 'ExternalOutput')

    def wset(sfx, moe):
        ne = NEXP if moe else 1
        shp = dict(wkf=[DM, NKF * 128], wqf=[DM, NQF * 128], wkt=[DM, 896], wqt=[DM, 12], wout=[DM, DM], vecs=[4, DM],
                   dlam=[128], dgain=[64], cpos=[2, 2048], cw1=[2, 2048, 256], cw2=[2, 256, 64], fw1=[ne, DM, DFF],
                   fw3=[ne, DM, DFF], fw2=[ne, DFF, DM], wrt=[DM, 8], plep=[256, DM], pleg=[DM, DM])
        return {k: S.dram(k + sfx, shp[k], F32, EI) for k in WNAMES}

    W0 = wset('_l0', False)
    W1 = wset('_l1', True)
    c_ident = S.dram('ident', [128, 128], F32, EI)
    c_ptab = S.dram('ptab', [128, 16], F32, EI)
    G['c_negtri'] = S.dram('negtri', [128, 128], F32, EI)
    G['c_negones'] = S.dram('negones', [128, 128], F32, EI)
    G['c_emoba'] = S.dram('emoba', [32, 64 * 128], F32, EI)
    G['c_esel'] = S.dram('esel', [128, 64 * 128], F32, EI)
    G['c_overlap'] = S.dram('overlap', [128, 512], F32, EI)
    CM = {k: S.dram('masks_' + k, [6, 128, 512], F32, EI) for k in ('p0', 'p1', 'own')}
    CT = {k: S.dram('tq_' + k, [1, NSLOT * 128], F32, EI) for k in ('p0', 'p1', 'own')}
    CS = {k: S.dram('selm_' + k, [128, NSLOT * 128], F32, EI) for k in ('p0', 'p1', 'own')}
    H0 = S.dram('H0', [2, TO, DM], F32)
    HO1 = S.dram('HO1', [TO, DM], F32)
    KF0 = S.dram('KF0', [NKFD, 128, SEQ], BF16)
    VA0 = S.dram('VA0', [SEQ, NVA], BF16)
    KF1 = S.dram('KF1', [NKFD, 128, SEQ], BF16)
    VA1 = S.dram('VA1', [SEQ, NVA], BF16)

    ident = S.sbuf([128, 128], F32, 'ident')
    S.dma('sp', ident[:], c_ident[:], writes=[ident])
    ptab = S.sbuf([128, 16], F32, 'ptab')
    S.dma('sp', ptab[:], c_ptab[:], writes=[ptab])
    G['ident'], G['ptab'] = ident, ptab
    G['masks'] = S.sbuf([128, 6, 512], BF16, 'masks')
    G['kmean'] = S.sbuf([128, 2, 32], BF16, 'kmean')
    P2 = [S.psum([128, 1024], F32, 'bank2_%d' % i) for i in range(4)]
    G['P2'] = P2
    G['PS'] = [TileView(P2[i // 2][:, (i % 2) * 512:(i % 2 + 1) * 512], 'bank%d' % i) for i in range(8)]
    G['SB2'] = [TileView(P2[i][:, :], 'sb2_%d' % i) for i in range(2)]

    pos2 = posfull[:].rearrange('(s two j) -> two s j', two=2, j=128)

    def mk_pass0(par):
        P = dict(W0)
        P.update(layer=0, moe=False, sfx='_0%d' % par, KF=KF0, VA=VA0, do_k=(par == 0),
                 c_masks=CM['p%d' % par], c_tq=CT['p%d' % par], c_selm=CS['p%d' % par])
        P['hk'] = lambda r0: (hfull, hfull[r0:r0 + 128, :])
        P['hq'] = lambda r0: (hfull, hfull[(2 * (r0 // 128) + par) * 128:(2 * (r0 // 128) + par + 1) * 128, :])
        P['posk'] = lambda c0: (posfull, posfull[c0:c0 + 512].partition_broadcast(128), False)
        P['posq'] = lambda c0: (posfull, pos2[par, c0 // 128:c0 // 128 + 4, :].partition_broadcast(128), True)
        P['prow'] = lambda r0: (pfull0, pfull0[(2 * (r0 // 128) + par) * 128:(2 * (r0 // 128) + par + 1) * 128, :])
        P['out'] = lambda r0: (H0, H0[par, r0:r0 + 128, :])
        return P

    emit_pass(G, mk_pass0(0), cfg)
    if cfg and cfg.get('only_pass0'):
        return nc, S
    emit_pass(G, mk_pass0(1), cfg)
    S.push_scope()
    pv = S.sbuf([128, 2], F32, 'parv')
    S.dma('sp', pv[:], parv[:], writes=[pv])
    ba = [S.sbuf([128, DM], F32, 'bla%d' % i) for i in range(2)]
    bb = [S.sbuf([128, DM], F32, 'blb%d' % i) for i in range(2)]
    for tb in range(NSLOT):
        a_, b_ = ba[tb % 2], bb[tb % 2]
        S.dma('sp', a_[:], H0[0, tb * 128:(tb + 1) * 128, :], reads=[H0], writes=[a_])
        S.dma('sp', b_[:], H0[1, tb * 128:(tb + 1) * 128, :], reads=[H0], writes=[b_])
        S.ts('dve', a_, a_[:], a_, a_[:], pv[:, 0:1], None, ALU.mult, reads=[pv])
        S.stt(a_, a_[:], b_, b_[:], pv[:, 1:2], a_, a_[:], ALU.mult, ALU.add, reads=[pv])
        S.dma('sp', HO1[tb * 128:(tb + 1) * 128, :], a_[:], reads=[a_], writes=[HO1])
    S.pop_scope()
    P = dict(W1)
    P.update(layer=1, moe=True, sfx='_1', KF=KF1, VA=VA1, do_k=True, c_masks=CM['own'], c_tq=CT['own'], c_selm=CS['own'])
    P['hk'] = lambda r0: (H0, H0[(r0 // 128) % 2, ((r0 // 128) // 2) * 128:((r0 // 128) // 2 + 1) * 128, :])
    P['hq'] = lambda r0: (HO1, HO1[r0:r0 + 128, :])
    P['posk'] = lambda c0: (posfull, posfull[c0:c0 + 512].partition_broadcast(128), False)
    P['posq'] = lambda c0: (posown, posown[c0:c0 + 512].partition_broadcast(128), False)
    P['prow'] = lambda r0: (pown1, pown1[r0:r0 + 128, :])
    P['out'] = lambda r0: (hout, hout[r0:r0 + 128, :])
    emit_pass(G, P, cfg)
    return nc, S


def _own_rows(parity):
    blk = 2 * np.arange(NSLOT) + parity
    return (blk[:, None] * 128 + np.arange(128)[None, :]).reshape(-1)


def _layer_weights(layer, inp, sfx):
    f = np.float32
    w_in = np.asarray(inp['w_in'][layer], f)
    moe = (layer % 2 == 1)
    w = {
        'wkf': _gather_cols(w_in, KF_IDX), 'wqf': _gather_cols(w_in, QF_IDX),
        'wkt': _gather_cols(w_in, KT_IDX), 'wqt': _gather_cols(w_in, QT_IDX),
        'wout': np.ascontiguousarray(inp['w_out'][layer], f),
        'vecs': np.stack([inp['ln_mix_g'][layer], inp['ln_mix_b'][layer], inp['ln_ffn_g'][layer],
                          inp['ln_ffn_b'][layer]]).astype(f),
        'dlam': np.ascontiguousarray(inp['diff_lambda'][layer], f).reshape(128),
        'dgain': np.ascontiguousarray(inp['diff_gain'][layer], f),
        'cpos': np.ascontiguousarray(inp['nsa_cmp_pos'][layer], f).reshape(2, 2048),
        'cw1': np.ascontiguousarray(inp['nsa_cmp_w1'][layer], f),
        'cw2': np.ascontiguousarray(inp['nsa_cmp_w2'][layer], f),
        'plep': np.ascontiguousarray(inp['ple_proj'][layer], f),
        'pleg': np.ascontiguousarray(inp['ple_gate'][layer], f),
    }
    if moe:
        w['fw1'] = np.ascontiguousarray(inp['moe_w1'][layer // 2], f)
        w['fw3'] = np.ascontiguousarray(inp['moe_w3'][layer // 2], f)
        w['fw2'] = np.ascontiguousarray(inp['moe_w2'][layer // 2], f)
        w['wrt'] = np.ascontiguousarray(inp['moe_router'][layer // 2], f)
    else:
        w['fw1'] = np.ascontiguousarray(inp['ffn_w1'][layer // 2], f)[None]
        w['fw3'] = np.ascontiguousarray(inp['ffn_w3'][layer // 2], f)[None]
        w['fw2'] = np.ascontiguousarray(inp['ffn_w2'][layer // 2], f)[None]
        w['wrt'] = np.zeros((DM, 8), f)
    return {k + sfx: v for k, v in w.items()}


def fused_inputs(inp, cores):
    f = np.float32
    shared = {}
    shared.update(_layer_weights(0, inp, '_l0'))
    shared.update(_layer_weights(1, inp, '_l1'))
    consts = [_host_consts(0), _host_consts(1)]
    for k in ('ident', 'ptab', 'negtri', 'negones', 'emoba', 'esel', 'overlap'):
        shared[k] = consts[0][k]
    for par in range(2):
        shared['masks_p%d' % par] = consts[par]['masks']
        shared['tq_p%d' % par] = consts[par]['tq']
        shared['selm_p%d' % par] = consts[par]['selm']
    maps = []
    for core in cores:
        b, par = core // 2, core % 2
        rows = _own_rows(par)
        m = dict(shared)
        m['masks_own'] = consts[par]['masks']
        m['tq_own'] = consts[par]['tq']
        m['selm_own'] = consts[par]['selm']
        m['hfull'] = np.ascontiguousarray(inp['x'][b], f)
        m['posfull'] = np.ascontiguousarray(inp['positions'][b], np.int32)
        m['posown'] = np.ascontiguousarray(inp['positions'][b][rows], np.int32)
        m['pfull0'] = np.ascontiguousarray(inp['p'][0, b], f)
        m['pown1'] = np.ascontiguousarray(inp['p'][1, b][rows], f)
        pv = np.zeros((128, 2), f)
        pv[:, par] = 1.0
        m['parv'] = pv
        maps.append(m)
    return maps


def kernel(**inputs):
    inp = {k: np.asarray(v) for k, v in inputs.items()}
    cores = list(range(8))
    nc, S = build_fused()
    S.finish()
    maps = fused_inputs(inp, cores)
    res = run_bass_kernel_spmd(nc, maps, core_ids=cores)
    out = np.zeros((4, SEQ, DM), np.float32)
    for core in cores:
        b, par = core // 2, core % 2
        out[b][_own_rows(par)] = res.results[core]['hout']
    return out
```

```python
import math
import numpy as np
import ml_dtypes
import concourse.bass as bass
import concourse.mybir as mybir
from contextlib import ExitStack
from concourse.bass_utils import run_bass_kernel_spmd

F32 = mybir.dt.float32
BF16 = mybir.dt.bfloat16
I32 = mybir.dt.int32
AF = mybir.ActivationFunctionType
ALU = mybir.AluOpType
AX = mybir.AxisListType

ENGS = ['pe', 'act', 'dve', 'pool', 'sp']
N_DMA_SEMS = 8
SEM_ROLL = 30000


class Tile:
    __slots__ = ('t', 'lastw', 'readers', 'name')

    def __init__(self, t, name=''):
        self.t = t
        self.lastw = None
        self.readers = {}
        self.name = name

    def __getitem__(self, idx):
        return self.t[idx]


class TileView(Tile):
    __slots__ = ('base',)

    def __init__(self, base_ap, name=''):
        Tile.__init__(self, None, name)
        self.base = base_ap

    def __getitem__(self, idx):
        return self.base[idx]


class Sched:
    def __init__(self, nc):
        self.nc = nc
        self.es = ExitStack()
        self.scopes = []
        self.q = {e: [] for e in ENGS}
        self.cnt = {}
        self.sems = {}
        self.epoch = {}
        for e in ['pe', 'act', 'dve', 'pool']:
            self.epoch[e] = 0
            self._newsem((e, 0))
        self.dma_rr = {}
        for e in ['sp', 'act', 'pool']:
            for i in range(N_DMA_SEMS):
                self.epoch[('d', e, i)] = 0
                self._newsem(('d', e, i, 0))
            self.dma_rr[e] = 0
        self.seen = {e: {} for e in ENGS}
        self.ntiles = 0
        self.ninstr = 0

    def _newsem(self, key):
        self.sems[key] = self.es.enter_context(self.nc.semaphore('s_' + '_'.join(str(k) for k in key)))
        self.cnt[key] = 0

    def _alloc_stack(self):
        return self.scopes[-1] if self.scopes else self.es

    def sbuf(self, shape, dt, name=None):
        self.ntiles += 1
        name = ('sb%d_' % self.ntiles) + (name or '')
        t = self._alloc_stack().enter_context(self.nc.sbuf_tensor(name, list(shape), dt))
        return Tile(t, name)

    def psum(self, shape, dt=F32, name=None):
        self.ntiles += 1
        name = ('ps%d_' % self.ntiles) + (name or '')
        t = self._alloc_stack().enter_context(self.nc.psum_tensor(name, list(shape), dt))
        return Tile(t, name)

    def dram(self, name, shape, dt, kind='Internal'):
        t = self.nc.dram_tensor(name, list(shape), dt, kind=kind)
        return Tile(t, name)

    def sub(self, tile, name=''):
        return Tile(tile.t, name or tile.name)

    def push_scope(self):
        self.scopes.append(ExitStack())

    def pop_scope(self):
        self.barrier()
        self.scopes.pop().close()

    def barrier(self):
        deps = [(k, v) for k, v in self.cnt.items() if v > 0]
        for e in ENGS:
            waits = self._need(e, deps)
            if waits:
                self.q[e].append(([(self.sems[k], v) for (k, v) in waits], None, None, 0))

    def _need(self, eng, deps):
        out = []
        seen = self.seen[eng]
        for (k, v) in deps:
            if seen.get(k, 0) < v:
                seen[k] = v
                out.append((k, v))
        return out

    def op(self, eng, fn, reads=(), writes=()):
        deps = []
        for t in reads:
            if t.lastw is not None:
                deps.append(t.lastw)
        for t in writes:
            if t.lastw is not None:
                deps.append(t.lastw)
            for rk, rv in t.readers.items():
                deps.append((rk, rv))
        if eng == 'pe':
            deps = [d for d in deps if d[0][0] != 'pe']
        waits = self._need(eng, deps)
        key = (eng, self.epoch[eng])
        if self.cnt[key] >= SEM_ROLL:
            self.epoch[eng] += 1
            key = (eng, self.epoch[eng])
            self._newsem(key)
        self.cnt[key] += 1
        v = self.cnt[key]
        self.q[eng].append(([(self.sems[k], val) for (k, val) in waits], fn, self.sems[key], 1))
        self.ninstr += 1
        for t in reads:
            t.readers[key] = v
        for t in writes:
            t.lastw = (key, v)
            t.readers = {}
        return v

    def dma(self, eng, out_ap, in_ap, reads=(), writes=(), **kw):
        i = self.dma_rr[eng]
        self.dma_rr[eng] = (i + 1) % N_DMA_SEMS
        slot = ('d', eng, i)
        k = ('d', eng, i, self.epoch[slot])
        deps = []
        if self.cnt[k] > 0:
            deps.append((k, self.cnt[k]))
        if self.cnt[k] >= SEM_ROLL:
            self.epoch[slot] += 1
            k = ('d', eng, i, self.epoch[slot])
            self._newsem(k)
        for t in reads:
            if t.lastw is not None:
                deps.append(t.lastw)
        for t in writes:
            if t.lastw is not None:
                deps.append(t.lastw)
            deps.extend(t.readers.items())
        waits = self._need(eng, deps)
        self.cnt[k] += 16
        v = self.cnt[k]
        fn = (lambda e, o=out_ap, i_=in_ap, kw=kw: e.dma_start(out=o, in_=i_, **kw))
        self.q[eng].append(([(self.sems[kk], val) for (kk, val) in waits], fn, self.sems[k], 16))
        self.ninstr += 1
        for t in reads:
            t.readers[k] = v
        for t in writes:
            t.lastw = (k, v)
            t.readers = {}
        return (k, v)

    def collective(self, kind, groups, in_t, in_ap, out_t, out_ap):
        eng = 'pool'
        k = ('cc', 0)
        if k not in self.sems:
            self._newsem(k)
        deps = []
        if self.cnt[k] > 0:
            deps.append((k, self.cnt[k]))
        if in_t.lastw is not None:
            deps.append(in_t.lastw)
        if out_t.lastw is not None:
            deps.append(out_t.lastw)
        deps.extend(out_t.readers.items())
        waits = self._need(eng, deps)
        self.cnt[k] += 1
        v = self.cnt[k]
        fn = (lambda e: e.collective_compute(kind, ALU.bypass, replica_groups=groups, ins=[in_ap], outs=[out_ap]))
        self.q[eng].append(([(self.sems[kk], val) for (kk, val) in waits], fn, self.sems[k], 1))
        self.ninstr += 1
        in_t.readers[k] = v
        out_t.lastw = (k, v)
        out_t.readers = {}

    def finish(self):
        self.barrier()
        q = self.q

        def replay(e, lst):
            for (wl, fn, sem, inc) in lst:
                for (s, v) in wl:
                    e.wait_ge(s, v)
                if fn is not None:
                    fn(e).then_inc(sem, inc)

        with self.nc.Block() as block:
            @block.tensor
            def _(e):
                replay(e, q['pe'])

            @block.scalar
            def _(e):
                replay(e, q['act'])

            @block.vector
            def _(e):
                replay(e, q['dve'])

            @block.gpsimd
            def _(e):
                replay(e, q['pool'])

            @block.sync
            def _(e):
                replay(e, q['sp'])
        self.es.close()

    def mm(self, ot, oap, lt, lap, rt, rap, start=True, stop=True, skip=False):
        if skip:
            return self.op('pe', lambda e: e.matmul(oap, lap, rap, start=start, stop=stop, skip_group_check=True),
                           reads=[lt, rt], writes=[ot])
        return self.op('pe', lambda e: e.matmul(oap, lap, rap, start=start, stop=stop), reads=[lt, rt], writes=[ot])

    def tr(self, ot, oap, it, iap, idt, idap):
        return self.op('pe', lambda e: e.transpose(oap, iap, idap), reads=[it, idt], writes=[ot])

    def act(self, ot, oap, it, iap, func, reads=(), writes=(), **kw):
        return self.op('act', lambda e: e.activation(out=oap, in_=iap, func=func, **kw),
                       reads=[it] + list(reads), writes=[ot] + list(writes))

    def tt(self, eng, ot, oap, at, aap, bt, bap, op):
        return self.op(eng, lambda e: e.tensor_tensor(out=oap, in0=aap, in1=bap, op=op), reads=[at, bt], writes=[ot])

    def ts(self, eng, ot, oap, it, iap, s1, s2, op0, op1=None, reads=()):
        if op1 is None:
            return self.op(eng, lambda e: e.tensor_scalar(out=oap, in0=iap, scalar1=s1, scalar2=None, op0=op0),
                           reads=[it] + list(reads), writes=[ot])
        return self.op(eng, lambda e: e.tensor_scalar(out=oap, in0=iap, scalar1=s1, scalar2=s2, op0=op0, op1=op1),
                       reads=[it] + list(reads), writes=[ot])

    def stt(self, ot, oap, at, aap, scalar, bt, bap, op0, op1, reads=()):
        return self.op('dve', lambda e: e.scalar_tensor_tensor(out=oap, in0=aap, scalar=scalar, in1=bap, op0=op0, op1=op1),
                       reads=[at, bt] + list(reads), writes=[ot])

    def cp(self, eng, ot, oap, it, iap):
        if eng == 'act':
            return self.op('act', lambda e: e.activation(out=oap, in_=iap, func=AF.Copy), reads=[it], writes=[ot])
        return self.op(eng, lambda e: e.tensor_copy(out=oap, in_=iap), reads=[it], writes=[ot])

    def raw(self, eng, meth, reads=(), writes=(), **kw):
        return self.op(eng, lambda e, meth=meth, kw=kw: getattr(e, meth)(**kw), reads=reads, writes=writes)

    def memset(self, eng, t, ap, val):
        return self.op(eng, lambda e, ap=ap, val=val: e.memset(ap, val), writes=[t])


SEQ = 8192
DM = 1024
TO = 4096
NSLOT = 32
DFF = 3584
NFFC = 28
NEXP = 8
ALPHA = 4 ** 0.25
NCMP = 511
NEG = -30000.0
BIG = 1.0e30
TWO_PI = 2.0 * math.pi
TWO_PI_HI = float(np.float32(TWO_PI))
TWO_PI_LO = float(TWO_PI - np.float64(np.float32(TWO_PI)))
MAGIC = 12582912.0

OFF = dict(a_q=0, a_k=256, a_v=512, b_q=768, b_k=1024, b_v=1280, c_q=1536, c_k=1792, c_v=2048, d_q=2304,
           d_kc=2560, d_vc=2624, d_ks=2688, d_vs=2752, d_kw=2816, d_vw=2880, d_g=2944)


def _swap(idx, w):
    idx = np.asarray(idx)
    out = idx.copy().reshape(-1, w)
    h = w // 2
    out = np.concatenate([out[:, h:], out[:, :h]], axis=1)
    return out.reshape(-1)


def _colplan():
    ar = np.arange
    kA = OFF['a_k'] + ar(256)
    bk = OFF['b_k'] + ar(256)
    kB1 = bk.copy().reshape(4, 64)
    kB1[:, 32:] = -1
    kB2 = bk.copy().reshape(4, 64)
    kB2[:, :32] = -1
    kB1s = kB1.copy()
    kB1s[:, :32] = _swap(kB1[:, :32].reshape(-1), 32).reshape(4, 32)
    kB2s = kB2.copy()
    kB2s[:, 32:] = _swap(kB2[:, 32:].reshape(-1), 32).reshape(4, 32)
    kC = OFF['c_k'] + ar(256)
    ks = OFF['d_ks'] + ar(64)
    kw = OFF['d_kw'] + ar(64)
    ksks = np.concatenate([ks, ks])
    kwkw = np.concatenate([kw, kw])
    kcvc = np.concatenate([OFF['d_kc'] + ar(64), OFF['d_vc'] + ar(64)])
    kf = np.concatenate([kA, kB1.reshape(-1), kB1s.reshape(-1), kB2.reshape(-1), kB2s.reshape(-1),
                         kC, _swap(kC, 64), ksks, _swap(ksks, 64), kwkw, _swap(kwkw, 64), kcvc])
    qA = OFF['a_q'] + ar(256)
    qB = OFF['b_q'] + ar(256)
    qC = OFF['c_q'] + ar(256)
    qD = OFF['d_q'] + ar(256)
    qf = np.concatenate([qA, qB, _swap(qB, 32), qC, _swap(qC, 64), qD, _swap(qD, 64)])
    kt = np.concatenate([OFF['a_v'] + ar(256), OFF['b_v'] + ar(256), OFF['c_v'] + ar(256),
                         OFF['d_vs'] + ar(64), OFF['d_vw'] + ar(64)])
    qt = OFF['d_g'] + ar(12)
    return kf, qf, kt, qt


KF_IDX, QF_IDX, KT_IDX, QT_IDX = _colplan()
NKF = len(KF_IDX) // 128
NQF = len(QF_IDX) // 128
KF_SRC = dict(kA=(0, 1), kB1=(2, 3), kB1s=(4, 5), kB2=(6, 7), kB2s=(8, 9), kC=(10, 11), kCs=(12, 13),
              ks=(14,), kss=(15,), kw=(16,), kws=(17,), kcvc=(18,))
KF_DST = dict(kA=(0, 1), kB1=(2, 3), kB2=(4, 5), kC=(6, 7), ks=(8,), kw=(9,), kcvc=(10,))
NKFD = 11
QF_SRC = dict(qA=(0, 1), qB=(2, 3), qBs=(4, 5), qC=(6, 7), qCs=(8, 9), qD=(10, 11), qDs=(12, 13))
QF_DST = dict(qA=(0, 1), qB=(2, 3), qC=(4, 5), qD=(6, 7), qDr=(8, 9))
NQFD = 10
NVA = 14 * 65


def _gather_cols(w, idx):
    out = np.zeros((w.shape[0], len(idx)), dtype=w.dtype)
    m = idx >= 0
    out[:, m] = w[:, idx[m]]
    return out


def _host_consts(parity):
    c = {}
    c['ident'] = np.eye(128, dtype=np.float32)
    j = np.arange(128)[:, None]
    i = np.arange(128)[None, :]
    le = (j <= i).astype(np.float32)
    lt = (j < i).astype(np.float32)
    gt = (j > i).astype(np.float32)
    one = np.ones((128, 128), np.float32)
    zero = np.zeros((128, 128), np.float32)
    if parity == 0:
        ms = [zero, le, zero, lt, gt, one]
    else:
        ms = [le, one, lt, one, zero, gt]
    c['masks'] = np.stack([np.tile(m, (1, 4)) for m in ms], 0).astype(np.float32)
    jj = np.arange(128)[:, None]
    ss = np.arange(128)[None, :]
    c['negtri'] = -(jj >= ss).astype(np.float32)
    c['negones'] = -np.ones((128, 128), np.float32)
    r = np.arange(128)
    tab = np.zeros((128, 16), np.float32)
    tab[:, 0] = 10000.0 ** (-(r % 16) / 16.0)
    tab[:, 1] = 10000.0 ** (-(r % 32) / 32.0)
    tab[:, 2] = np.where((r % 32) < 16, -1.0, 1.0)
    tab[:, 3] = np.where((r % 64) < 32, -1.0, 1.0)
    for ci in range(4):
        tab[:, 4 + ci] = 16.0 * (128 * ci + r) + 31.0
    c['ptab'] = tab
    em = np.zeros((32, 64, 128), np.float32)
    for kb in range(64):
        em[kb // 2, kb, :] = 1.0
    c['emoba'] = em.reshape(32, 64 * 128)
    es = np.zeros((128, 64, 128), np.float32)
    for kb in range(64):
        es[2 * kb, kb, :64] = 1.0
        es[2 * kb + 1, kb, 64:] = 1.0
    c['esel'] = es.reshape(128, 64 * 128)
    n = np.arange(512)[:, None]
    m = np.arange(128)[None, :]
    ov = ((16 * n < 64 * m + 64) & (16 * n + 32 > 64 * m)).astype(np.float32)
    ov[511, :] = 0.0
    c['overlap'] = ov.reshape(4, 128, 128).transpose(1, 0, 2).reshape(128, 512).copy()
    tq = np.zeros((NSLOT, 128), np.float32)
    selm = np.zeros((128, NSLOT, 128), np.float32)
    for s in range(NSLOT):
        qb = 2 * s + parity
        t = qb * 128 + np.arange(128)
        tq[s] = t
        qblk = t // 64
        mm_ = np.arange(128)[None, :]
        forced = (mm_ == 0) | (mm_ == qblk[:, None]) | (mm_ == qblk[:, None] - 1)
        causal = mm_ <= qblk[:, None]
        selm[:, s, :] = np.where(forced, BIG, np.where(causal, 0.0, -BIG))
    c['tq'] = tq.reshape(1, NSLOT * 128)
    c['selm'] = selm.reshape(128, NSLOT * 128)
    return c


def emit_pass(G, P, cfg=None):
    cfg = cfg or {}
    slots = cfg.get('slots', list(range(NSLOT)))
    mixers = cfg.get('mixers', 'ABCD')
    do_tail = cfg.get('tail', True)
    dbg = False
    layer = P['layer']
    moe = P['moe']
    sfx = P['sfx']
    lam_init = 0.8 - 0.6 * math.exp(-0.3 * layer)
    nc, S = G['nc'], G['S']
    hk_rows, hq_rows, posk, posq, p_rows, out_rows = P['hk'], P['hq'], P['posk'], P['posq'], P['prow'], P['out']
    wkf, wqf, wkt, wqt, wout, vecs = P['wkf'], P['wqf'], P['wkt'], P['wqt'], P['wout'], P['vecs']
    dlam, dgain, cpos, cw1, cw2 = P['dlam'], P['dgain'], P['cpos'], P['cw1'], P['cw2']
    fw1, fw3, fw2, wrt, plep, pleg = P['fw1'], P['fw3'], P['fw2'], P['wrt'], P['plep'], P['pleg']
    ne = NEXP if moe else 1
    c_masks, c_tq, c_selm = P['c_masks'], P['c_tq'], P['c_selm']
    c_negtri, c_negones, c_emoba, c_esel, c_overlap = G['c_negtri'], G['c_negones'], G['c_emoba'], G['c_esel'], G['c_overlap']
    KF, VA, do_k = P['KF'], P['VA'], P['do_k']
    QF = S.dram('QF' + sfx, [NQFD, 128, TO], BF16)
    GT = S.dram('GT' + sfx, [TO, 12], F32)
    MIX = S.dram('MIX' + sfx, [TO, DM], F32)
    ident, ptab, kmean, masks = G['ident'], G['ptab'], G['kmean'], G['masks']
    P2, PS, SB2 = G['P2'], G['PS'], G['SB2']
    S.dma('pool', masks[:], c_masks[:].rearrange('m p f -> p m f'), writes=[masks])
    M_HI_LE, M_LO_LE, M_HI_LT, M_LO_LT, M_W4, M_W3 = range(6)
    if cfg.get('zfill'):
        S.push_scope()
        ztile = S.sbuf([128, DM], F32, 'ztile')
        S.memset('pool', ztile, ztile[:], 0.0)
        for tb in range(NSLOT):
            S.dma('sp', MIX[tb * 128:(tb + 1) * 128, :], ztile[:], reads=[ztile], writes=[MIX])
        S.pop_scope()

    def scol(h):
        return (h % 2) * 512 + (h // 2) * 128

    def pcol(h):
        return (h % 2) * 256 + (h // 2) * 128

    def v2(t_):
        return t_[:].rearrange('p (b c) -> p b c', b=2)[:, :, 0:256]

    rr = [0]

    def alt(engs=('act', 'dve')):
        rr[0] += 1
        return engs[rr[0] % len(engs)]

    S.push_scope()
    wk_sb = S.sbuf([128, 8, NKF * 128], BF16, 'wk_sb')
    wq_sb = S.sbuf([128, 8, NQF * 128], BF16, 'wq_sb')
    wkt_sb = S.sbuf([128, 8, 896], BF16, 'wkt_sb')
    wqt_sb = S.sbuf([128, 8, 12], BF16, 'wqt_sb')
    for kc in range(8):
        S.dma('pool', wk_sb[:, kc, :], wkf[kc * 128:(kc + 1) * 128, :], writes=[wk_sb])
        S.dma('pool', wq_sb[:, kc, :], wqf[kc * 128:(kc + 1) * 128, :], writes=[wq_sb])
    S.dma('pool', wkt_sb[:], wkt[:].rearrange('(k p) f -> p k f', p=128), writes=[wkt_sb])
    S.dma('pool', wqt_sb[:], wqt[:].rearrange('(k p) f -> p k f', p=128), writes=[wqt_sb])

    hrow = [S.sbuf([128, DM], F32, 'hrow%d' % i) for i in range(2)]
    hT = [S.sbuf([128, 8, 512], BF16, 'hT%d' % i) for i in range(2)]
    posi = S.sbuf([128, 512], I32, 'posi')
    posf = S.sbuf([128, 512], F32, 'posf')
    rtmp = [S.sbuf([128, 512], F32, 'rtmp%d' % i) for i in range(3)]
    rope = {k: S.sbuf([128, 512], F32, 'rope_' + k) for k in ('cosB', 'sinB', 'cosCD', 'sinCD')}
    ftile = [S.sbuf([128, 512], BF16, 'ftile%d' % i) for i in range(4)]
    ft32 = [S.sbuf([128, 512], F32, 'ft32_%d' % i) for i in range(2)]
    vaug = [S.sbuf([128, 14, 65], BF16, 'vaug%d' % i) for i in range(2)]
    gsb = [S.sbuf([128, 12], F32, 'gsb%d' % i) for i in range(2)]
    kms = S.sbuf([128, 2, 32], F32, 'kms')
    for v_ in vaug:
        S.memset('pool', v_, v_[:], 1.0)

    def build_rope(pos_dram, c0):
        pt_, pap_, three_ = pos_dram(c0)
        S.dma('sp', posi[:].rearrange('p (a j) -> p a j', a=4) if three_ else posi[:], pap_, reads=[pt_], writes=[posi])
        S.cp('dve', posf, posf[:], posi, posi[:])
        for (tabcol, sgncol, ck, sk) in ((0, 2, 'cosB', 'sinB'), (1, 3, 'cosCD', 'sinCD')):
            ang, t1, t2 = rtmp
            S.ts('dve', ang, ang[:], posf, posf[:], ptab[:, tabcol:tabcol + 1], None, ALU.mult, reads=[ptab])
            for (shift, dst, sgn) in ((0.0, rope[sk], True), (math.pi / 2, rope[ck], False)):
                S.ts('dve', t1, t1[:], ang, ang[:], 1.0 / TWO_PI, shift / TWO_PI + MAGIC, ALU.mult, ALU.add)
                S.ts('dve', t1, t1[:], t1, t1[:], -MAGIC, None, ALU.add)
                S.stt(t2, t2[:], t1, t1[:], -TWO_PI_HI, ang, ang[:], ALU.mult, ALU.add)
                S.ts('dve', t2, t2[:], t2, t2[:], shift, None, ALU.add)
                S.stt(t2, t2[:], t1, t1[:], -TWO_PI_LO, t2, t2[:], ALU.mult, ALU.add)
                S.ts('dve', t1, t1[:], t2, t2[:], math.pi, -TWO_PI, ALU.is_gt, ALU.mult)
                S.tt('dve', t2, t2[:], t2, t2[:], t1, t1[:], ALU.add)
                S.ts('dve', t1, t1[:], t2, t2[:], -math.pi, TWO_PI, ALU.is_lt, ALU.mult)
                S.tt('dve', t2, t2[:], t2, t2[:], t1, t1[:], ALU.add)
                S.ts('dve', t2, t2[:], t2, t2[:], 3.14159, -3.14159, ALU.min, ALU.max)
                if sgn:
                    S.act(dst, dst[:], t2, t2[:], AF.Sin, reads=[ptab], scale=ptab[:, sgncol:sgncol + 1])
                else:
                    S.act(dst, dst[:], t2, t2[:], AF.Sin)

    def load_hT(src, r0, buf):
        for tb in range(4):
            hr = hrow[tb % 2]
            st_, sap_ = src(r0 + tb * 128)
            S.dma('sp', hr[:], sap_, reads=[st_], writes=[hr])
            for half in range(2):
                bank = PS[6 + half]
                for k4 in range(4):
                    kc = half * 4 + k4
                    S.tr(bank, bank[:, k4 * 128:(k4 + 1) * 128], hr, hr[:, kc * 128:(kc + 1) * 128], ident, ident[:])
                S.cp(alt(), buf, buf[:, half * 4:half * 4 + 4, tb * 128:(tb + 1) * 128],
                     bank, bank[:].rearrange('p (k t) -> p k t', k=4))

    def proj_tile(w_sb, src_tile, buf, bank):
        for kc in range(8):
            S.mm(bank, bank[:], w_sb, w_sb[:, kc, src_tile * 128:(src_tile + 1) * 128], buf, buf[:, kc, :],
                 start=(kc == 0), stop=(kc == 7))

    fcnt = [0]

    def emit_plain(w_sb, src, buf, dst_dram, dst_tile, c0, scale=None):
        bank = PS[fcnt[0] % 4]
        ft = ftile[fcnt[0] % 4]
        fcnt[0] += 1
        proj_tile(w_sb, src, buf, bank)
        if scale is None:
            S.cp(alt(), ft, ft[:], bank, bank[:])
        else:
            S.act(ft, ft[:], bank, bank[:], AF.Copy, scale=scale)
        S.dma('sp', dst_dram[dst_tile, :, c0:c0 + 512], ft[:], reads=[ft], writes=[dst_dram])
        return ft

    def emit_rope(w_sb, src, src_s, buf, dst_dram, dst_tile, c0, cosk, sink, scale=None, km_slot=None):
        i0 = fcnt[0] % 2
        bank = PS[i0 * 2]
        bank_s = PS[i0 * 2 + 1]
        ft = ftile[fcnt[0] % 4]
        t32 = ft32[i0]
        fcnt[0] += 1
        proj_tile(w_sb, src, buf, bank)
        proj_tile(w_sb, src_s, buf, bank_s)
        S.tt('dve', t32, t32[:], bank, bank[:], rope[cosk], rope[cosk][:], ALU.mult)
        S.tt('dve', bank_s, bank_s[:], bank_s, bank_s[:], rope[sink], rope[sink][:], ALU.mult)
        if km_slot is not None:
            S.tt('dve', t32, t32[:], t32, t32[:], bank_s, bank_s[:], ALU.add)
            S.raw('dve', 'tensor_reduce', reads=[t32], writes=[kms],
                  out=kms[:, km_slot[0], km_slot[1]:km_slot[1] + 2], in_=t32[:].rearrange('p (b k) -> p b k', b=2),
                  axis=AX.X, op=ALU.add)
            S.cp('act', ft, ft[:], t32, t32[:])
        elif scale is None:
            S.tt('dve', ft, ft[:], t32, t32[:], bank_s, bank_s[:], ALU.add)
        else:
            S.tt('dve', t32, t32[:], t32, t32[:], bank_s, bank_s[:], ALU.add)
            S.act(ft, ft[:], t32, t32[:], AF.Copy, scale=scale)
        S.dma('sp', dst_dram[dst_tile, :, c0:c0 + 512], ft[:], reads=[ft], writes=[dst_dram])

    for ch in (range(SEQ // 512) if do_k else []):
        c0 = ch * 512
        buf = hT[ch % 2]
        load_hT(hk_rows, c0, buf)
        build_rope(posk, c0)
        for t in range(2):
            emit_plain(wk_sb, KF_SRC['kA'][t], buf, KF, KF_DST['kA'][t], c0)
        for t in range(2):
            emit_rope(wk_sb, KF_SRC['kB1'][t], KF_SRC['kB1s'][t], buf, KF, KF_DST['kB1'][t], c0, 'cosB', 'sinB')
            emit_rope(wk_sb, KF_SRC['kB2'][t], KF_SRC['kB2s'][t], buf, KF, KF_DST['kB2'][t], c0, 'cosB', 'sinB')
            emit_rope(wk_sb, KF_SRC['kC'][t], KF_SRC['kCs'][t], buf, KF, KF_DST['kC'][t], c0, 'cosCD', 'sinCD',
                      km_slot=(t, 2 * ch))
        emit_rope(wk_sb, KF_SRC['ks'][0], KF_SRC['kss'][0], buf, KF, KF_DST['ks'][0], c0, 'cosCD', 'sinCD')
        emit_rope(wk_sb, KF_SRC['kw'][0], KF_SRC['kws'][0], buf, KF, KF_DST['kw'][0], c0, 'cosCD', 'sinCD')
        emit_plain(wk_sb, KF_SRC['kcvc'][0], buf, KF, KF_DST['kcvc'][0], c0)
        for tb in range(4):
            va = vaug[tb % 2]
            b0, b1 = PS[4], PS[5]
            for kc in range(8):
                S.mm(b0, b0[:], buf, buf[:, kc, tb * 128:(tb + 1) * 128], wkt_sb, wkt_sb[:, kc, 0:512],
                     start=(kc == 0), stop=(kc == 7))
            for kc in range(8):
                S.mm(b1, b1[:, 0:384], buf, buf[:, kc, tb * 128:(tb + 1) * 128], wkt_sb, wkt_sb[:, kc, 512:896],
                     start=(kc == 0), stop=(kc == 7))
            S.cp('act', va, va[:, 0:8, 0:64], b0, b0[:].rearrange('p (h d) -> p h d', h=8))
            S.cp('dve', va, va[:, 8:14, 0:64], b1, b1[:, 0:384].rearrange('p (h d) -> p h d', h=6))
            r0 = c0 + tb * 128
            S.dma('sp', VA[r0:r0 + 128, :], va[:].rearrange('p h d -> p (h d)'), reads=[va], writes=[VA])
    if do_k:
        S.ts('dve', kms, kms[:], kms, kms[:], 1.0 / 256.0, None, ALU.mult)
        S.cp('dve', kmean, kmean[:], kms, kms[:])

    for ch in range(TO // 512):
        c0 = ch * 512
        buf = hT[ch % 2]
        load_hT(hq_rows, c0, buf)
        build_rope(posq, c0)
        for t in range(2):
            emit_plain(wq_sb, QF_SRC['qA'][t], buf, QF, QF_DST['qA'][t], c0, scale=0.125)
            emit_rope(wq_sb, QF_SRC['qB'][t], QF_SRC['qBs'][t], buf, QF, QF_DST['qB'][t], c0, 'cosB', 'sinB',
                      scale=32 ** -0.5)
            emit_rope(wq_sb, QF_SRC['qC'][t], QF_SRC['qCs'][t], buf, QF, QF_DST['qC'][t], c0, 'cosCD', 'sinCD',
                      scale=0.125)
            emit_plain(wq_sb, QF_SRC['qD'][t], buf, QF, QF_DST['qD'][t], c0, scale=0.125)
            emit_rope(wq_sb, QF_SRC['qD'][t], QF_SRC['qDs'][t], buf, QF, QF_DST['qDr'][t], c0, 'cosCD', 'sinCD',
                      scale=0.125)
        for tb in range(4):
            g = gsb[tb % 2]
            b0 = PS[4 + tb % 2]
            for kc in range(8):
                S.mm(b0, b0[:, 0:12], buf, buf[:, kc, tb * 128:(tb + 1) * 128], wqt_sb, wqt_sb[:, kc, :],
                     start=(kc == 0), stop=(kc == 7))
            S.act(g, g[:], b0, b0[:, 0:12], AF.Sigmoid)
            r0 = c0 + tb * 128
            S.dma('sp', GT[r0:r0 + 128, :], g[:], reads=[g], writes=[GT])
    S.pop_scope()
    def view4(bank):
        return bank[:, 0:260].rearrange('p (h d) -> p h d', h=4)

    def load_kt(dst, tiles):
        for i_, t_ in enumerate(tiles):
            for hf in range(2):
                S.dma('sp', dst[:, i_, hf * 4096:(hf + 1) * 4096], KF[t_, :, hf * 4096:(hf + 1) * 4096], writes=[dst])

    def load_q(dst, tiles):
        for i_, t_ in enumerate(tiles):
            S.dma('sp', dst[:, i_, :], QF[t_, :, :], writes=[dst])

    def load_v(dst, s0, ns):
        for q4 in range(16):
            S.dma('sp', dst[:, q4 * 4:(q4 + 1) * 4, :],
                  VA[q4 * 512:(q4 + 1) * 512, s0 * 65:(s0 + ns) * 65].rearrange('(kb p) f -> p kb f', p=128),
                  writes=[dst])

    sbk = [0]
    ptb = [0]

    pend = [None]

    def attn_flush():
        if pend[0] is not None:
            f_ = pend[0]
            pend[0] = None
            f_()

    def attn_step(slot, kb, kt, kt_tile_of_h, q, pts, vfn, obank, first, last, mask=None, bias=None, shared_k=False):
        sb = SB2[sbk[0] % 2]
        sbk[0] += 1
        pt = pts[ptb[0] % len(pts)]
        ptb[0] += 1
        for h in (0, 2, 1, 3):
            b = (h % 2) * 64
            S.mm(sb, sb[:, scol(h):scol(h) + 128], kt, kt[b:b + 64, kt_tile_of_h(h), kb * 128:(kb + 1) * 128],
                 q, q[b:b + 64, h // 2, slot * 128:(slot + 1) * 128], start=(h < 2), stop=(bias is None and h >= 2),
                 skip=True)
        if bias is not None:
            bt_, bap_fn, et_, eap = bias
            for par in range(2):
                S.mm(sb, sb[:, par * 512:par * 512 + 256], et_, eap, bt_, bap_fn(par), start=False, stop=True, skip=True)

        def rest():
            S.act(pt, pt[:].rearrange('p (b c) -> p b c', b=2), sb, v2(sb), AF.Exp)
            if mask is not None:
                S.tt('dve', pt, pt[:], pt, pt[:], masks, masks[:, mask, :], ALU.mult)
            for h in range(4):
                vt, vap = vfn(h, kb)
                S.mm(obank, obank[:, h * 65:(h + 1) * 65], pt, pt[:, pcol(h):pcol(h) + 128], vt, vap,
                     start=(first and h == 0), stop=last, skip=True)

        prev = pend[0]
        pend[0] = rest
        if prev is not None:
            prev()

    def causal_mask(slot, kb, strict=False):
        if kb == 2 * slot + 1:
            return M_HI_LT if strict else M_HI_LE
        if kb == 2 * slot:
            return M_LO_LT if strict else M_LO_LE
        return None

    def store_mix(omix, slot, mi):
        S.dma('sp', MIX[slot * 128:(slot + 1) * 128, mi * 256:(mi + 1) * 256], omix[:], reads=[omix], writes=[MIX])

    if 'B' in mixers:
        S.push_scope()
        k1 = S.sbuf([128, 2, SEQ], BF16, 'k1')
        k2 = S.sbuf([128, 2, SEQ], BF16, 'k2')
        qb_ = S.sbuf([128, 2, TO], BF16, 'qB')
        vb = S.sbuf([128, 64, 260], BF16, 'vB')
        load_kt(k1, KF_DST['kB1'])
        load_kt(k2, KF_DST['kB2'])
        load_q(qb_, QF_DST['qB'])
        load_v(vb, 4, 4)
        pts = [S.sbuf([128, 512], BF16, 'ptB%d' % i) for i in range(3)]
        lamt = S.sbuf([128, 128], F32, 'lamt')
        S.dma('sp', lamt[:], dlam[:].partition_broadcast(128), writes=[lamt])
        gainb = S.sbuf([128, 64], F32, 'gainb')
        S.dma('sp', gainb[:], dgain[:].partition_broadcast(128), writes=[gainb])
        S.ts('dve', gainb, gainb[:], gainb, gainb[:], 1.0 - lam_init, None, ALU.mult)
        lp = S.sbuf([128, 64], F32, 'lp')
        ls = S.sbuf([128, 4], F32, 'ls')
        S.tt('dve', lp, lp[:, 0:32], lamt, lamt[:, 0:32], lamt, lamt[:, 32:64], ALU.mult)
        S.tt('dve', lp, lp[:, 32:64], lamt, lamt[:, 64:96], lamt, lamt[:, 96:128], ALU.mult)
        S.raw('dve', 'tensor_reduce', reads=[lp], writes=[ls], out=ls[:, 0:2],
              in_=lp[:].rearrange('p (a b) -> p a b', a=2), axis=AX.X, op=ALU.add)
        S.act(ls, ls[:, 0:2], ls, ls[:, 0:2], AF.Exp)
        S.tt('dve', ls, ls[:, 2:3], ls, ls[:, 1:2], ls, ls[:, 0:1], ALU.subtract)
        S.ts('dve', ls, ls[:, 2:3], ls, ls[:, 2:3], -lam_init, None, ALU.add)
        rd = S.sbuf([128, 8], F32, 'rdB')
        ob = S.sbuf([128, 4, 64], F32, 'obB')
        sq = S.sbuf([128, 4, 64], F32, 'sqB')
        ss = S.sbuf([128, 4], F32, 'ssB')
        omixs = [S.sbuf([128, 256], F32, 'omixB%d' % i) for i in range(2)]
        for si, slot in enumerate(slots):
            nkb = 2 * slot + 2
            for c_, kt_ in enumerate((k1, k2)):
                obank = PS[4 + c_]
                for kb in range(nkb):
                    attn_step(slot, kb, kt_, lambda h: h // 2, qb_, pts, lambda h, kb: (vb, vb[:, kb, h * 65:(h + 1) * 65]),
                              obank, kb == 0, kb == nkb - 1, mask=causal_mask(slot, kb))
            attn_flush()
            o1, o2 = view4(PS[4]), view4(PS[5])
            S.raw('dve', 'reciprocal', reads=[PS[4]], writes=[rd], out=rd[:, 0:4], in_=o1[:, :, 64])
            S.raw('dve', 'reciprocal', reads=[PS[5]], writes=[rd], out=rd[:, 4:8], in_=o2[:, :, 64])
            S.ts('dve', rd, rd[:, 4:8], rd, rd[:, 4:8], ls[:, 2:3], None, ALU.mult, reads=[ls])
            omix = omixs[si % 2]
            for h in range(4):
                S.ts('dve', ob, ob[:, h, :], PS[4], o1[:, h, 0:64], rd[:, h:h + 1], None, ALU.mult, reads=[rd])
                S.stt(ob, ob[:, h, :], PS[5], o2[:, h, 0:64], rd[:, 4 + h:5 + h], ob, ob[:, h, :], ALU.mult, ALU.add, reads=[rd])
            S.tt('dve', sq, sq[:], ob, ob[:], ob, ob[:], ALU.mult)
            S.raw('dve', 'tensor_reduce', reads=[sq], writes=[ss], out=ss[:], in_=sq[:], axis=AX.X, op=ALU.add)
            S.ts('dve', ss, ss[:], ss, ss[:], 1.0 / 64.0, 1e-5, ALU.mult, ALU.add)
            S.act(ss, ss[:], ss, ss[:], AF.Sqrt)
            S.raw('dve', 'reciprocal', reads=[ss], writes=[ss], out=ss[:], in_=ss[:])
            for h in range(4):
                S.stt(omix, omix[:, h * 64:(h + 1) * 64], ob, ob[:, h, :], ss[:, h:h + 1], gainb, gainb[:],
                      ALU.mult, ALU.mult, reads=[ss])
            store_mix(omix, slot, 1)
        S.pop_scope()

    if 'C' in mixers:
        S.push_scope()
        kc_ = S.sbuf([128, 2, SEQ], BF16, 'kC')
        qc_ = S.sbuf([128, 2, TO], BF16, 'qC')
        vc_ = S.sbuf([128, 64, 260], BF16, 'vC')
        load_kt(kc_, KF_DST['kC'])
        load_q(qc_, QF_DST['qC'])
        load_v(vc_, 8, 4)
        emoba = S.sbuf([128, 64 * 128], BF16, 'emoba')
        S.memset('pool', emoba, emoba[:], 0.0)
        S.dma('pool', emoba[0:32, :], c_emoba[:], writes=[emoba])
        pts = [S.sbuf([128, 512], BF16, 'ptC%d' % i) for i in range(3)]
        gbuf = S.sbuf([128, 4, 32], F32, 'gbuf')
        top8 = S.sbuf([128, 4, 8], F32, 'top8')
        selb = S.sbuf([128, 4, 32], F32, 'selb')
        biasT = S.sbuf([128, 512], BF16, 'biasT')
        S.memset('pool', biasT, biasT[:], 0.0)
        rd = S.sbuf([128, 4], F32, 'rdC')
        omixs = [S.sbuf([128, 256], F32, 'omixC%d' % i) for i in range(2)]
        for si, slot in enumerate(slots):
            own = slot
            nkb = 2 * slot + 2
            if own > 0:
                for h in range(4):
                    b = (h % 2) * 64
                    g = PS[6 + h % 2]
                    S.mm(g, g[:, (h // 2) * 32:(h // 2 + 1) * 32], qc_, qc_[b:b + 64, h // 2, slot * 128:(slot + 1) * 128],
                         kmean, kmean[b:b + 64, h // 2, :], start=True, stop=True)
                S.memset('pool', gbuf, gbuf[:], -BIG)
                for par in range(2):
                    g = PS[6 + par]
                    S.cp('dve', gbuf, gbuf[:, par * 2:par * 2 + 2, 0:own], g,
                         g[:, 0:64].rearrange('p (h n) -> p h n', h=2)[:, :, 0:own])
                for gi in range(4):
                    S.raw('dve', 'max', reads=[gbuf], writes=[top8], out=top8[:, gi, :], in_=gbuf[:, gi, :])
                for gi in range(4):
                    S.ts('dve', selb, selb[:, gi, :], gbuf, gbuf[:, gi, :], top8[:, gi, 2:3], 1.0, ALU.is_ge, ALU.subtract,
                         reads=[top8])
                S.ts('dve', selb, selb[:], selb, selb[:], -NEG, None, ALU.mult)
                tb_ = PS[7]
                for gi in range(4):
                    S.tr(tb_, tb_[0:32, gi * 128:(gi + 1) * 128], selb, selb[:, gi, :], ident, ident[:])
                S.cp('act', biasT, biasT[0:32, :], tb_, tb_[0:32, :])
            obank = PS[4 + si % 2]
            for kb in range(nkb):
                bias = None
                if kb < 2 * own:
                    bias = (biasT, lambda par: biasT[:, par * 256:(par + 1) * 256], emoba, emoba[:, kb * 128:(kb + 1) * 128])
                attn_step(slot, kb, kc_, lambda h: h // 2, qc_, pts, lambda h, kb: (vc_, vc_[:, kb, h * 65:(h + 1) * 65]),
                          obank, kb == 0, kb == nkb - 1, mask=causal_mask(slot, kb), bias=bias)
            attn_flush()
            o1 = view4(obank)
            S.raw('dve', 'reciprocal', reads=[obank], writes=[rd], out=rd[:], in_=o1[:, :, 64])
            omix = omixs[si % 2]
            for h in range(4):
                S.ts('dve', omix, omix[:, h * 64:(h + 1) * 64], obank, o1[:, h, 0:64], rd[:, h:h + 1], None, ALU.mult,
                     reads=[rd])
            store_mix(omix, slot, 2)
        S.pop_scope()

    if 'A' in mixers:
        S.push_scope()
        ka = S.sbuf([128, 2, SEQ], BF16, 'kA')
        qa = S.sbuf([128, 2, TO], BF16, 'qA')
        va_ = S.sbuf([128, 64, 260], BF16, 'vA')
        load_kt(ka, KF_DST['kA'])
        load_q(qa, QF_DST['qA'])
        load_v(va_, 0, 4)
        negtri = S.sbuf([128, 128], BF16, 'negtri')
        negones = S.sbuf([128, 128], BF16, 'negones')
        S.dma('pool', negtri[:], c_negtri[:], writes=[negtri])
        S.dma('pool', negones[:], c_negones[:], writes=[negones])
        pts = [S.sbuf([128, 512], BF16, 'ptA%d' % i) for i in range(3)]
        ee = [S.sbuf([128, 512], F32, 'eeA%d' % i) for i in range(2)]
        ll = [S.sbuf([128, 512], BF16, 'llA%d' % i) for i in range(3)]
        lacc = [S.sbuf([128, 512], BF16, 'laccA%d' % i) for i in range(2)]
        omixs = [S.sbuf([128, 256], F32, 'omixA%d' % i) for i in range(2)]
        stepi = 0
        for si, slot in enumerate(slots):
            nkb = 2 * slot + 2
            obank = PS[4 + si % 2]
            kbs = list(range(nkb - 1, -1, -1))
            N_ = len(kbs)
            base = stepi
            stepi += N_
            la = [None] * N_

            def ph1(n):
                kb = kbs[n]
                g = base + n
                zb, e_, l_ = SB2[g % 2], ee[g % 2], ll[g % 3]
                for h in (0, 2, 1, 3):
                    b = (h % 2) * 64
                    S.mm(zb, zb[:, scol(h):scol(h) + 128], ka, ka[b:b + 64, h // 2, kb * 128:(kb + 1) * 128],
                         qa, qa[b:b + 64, h // 2, slot * 128:(slot + 1) * 128], start=(h < 2), stop=False, skip=True)
                S.act(e_, e_[:].rearrange('p (b c) -> p b c', b=2), zb, v2(zb), AF.Exp)
                S.act(l_, l_[:], e_, e_[:], AF.Ln, bias=1.0)
                m = causal_mask(slot, kb, strict=True)
                if m is not None:
                    S.tt('dve', l_, l_[:], l_, l_[:], masks, masks[:, m, :], ALU.mult)

            def ph2(n):
                kb = kbs[n]
                g = base + n
                zb, l_, pt = SB2[g % 2], ll[g % 3], pts[g % 3]
                prev = la[n - 1] if n > 0 else None
                for par in range(2):
                    S.mm(zb, zb[:, par * 512:par * 512 + 256], negtri, negtri[:], l_, l_[:, par * 256:(par + 1) * 256],
                         start=False, stop=(prev is None), skip=True)
                if prev is not None:
                    for par in range(2):
                        S.mm(zb, zb[:, par * 512:par * 512 + 256], negones, negones[:], prev,
                             prev[:, par * 256:(par + 1) * 256], start=False, stop=True, skip=True)
                S.act(pt, pt[:].rearrange('p (b c) -> p b c', b=2), zb, v2(zb), AF.Exp)
                m = causal_mask(slot, kb, strict=True)
                if m is not None:
                    S.tt('dve', pt, pt[:], pt, pt[:], masks, masks[:, m, :], ALU.mult)
                if n < N_ - 1:
                    cur = lacc[n % 2]
                    if prev is None:
                        S.cp('pool', cur, cur[:], l_, l_[:])
                    else:
                        S.tt('pool', cur, cur[:], prev, prev[:], l_, l_[:], ALU.add)
                    la[n] = cur

            def ph3(n):
                kb = kbs[n]
                pt = pts[(base + n) % 3]
                for h in range(4):
                    S.mm(obank, obank[:, h * 65:(h + 1) * 65], pt, pt[:, pcol(h):pcol(h) + 128],
                         va_, va_[:, kb, h * 65:(h + 1) * 65], start=(n == 0 and h == 0), stop=(n == N_ - 1), skip=True)

            ph1(0)
            for n in range(N_):
                if n + 1 < N_:
                    ph1(n + 1)
                ph2(n)
                if n >= 1:
                    ph3(n - 1)
            ph3(N_ - 1)
            omix = omixs[si % 2]
            o1 = view4(obank)
            S.cp('dve', omix, omix[:].rearrange('p (h d) -> p h d', h=4), obank, o1[:, :, 0:64])
            store_mix(omix, slot, 0)
        S.pop_scope()
    if 'D' in mixers:
        S.push_scope()
        ktd = S.sbuf([128, 2, SEQ], BF16, 'ktD')
        load_kt(ktd, (KF_DST['ks'][0], KF_DST['kw'][0]))
        qd = S.sbuf([128, 2, TO], BF16, 'qD')
        qdr = S.sbuf([128, 2, TO], BF16, 'qDr')
        load_q(qd, QF_DST['qD'])
        load_q(qdr, QF_DST['qDr'])
        vd = S.sbuf([128, 64, 130], BF16, 'vD')
        load_v(vd, 12, 2)
        esel = S.sbuf([128, 64 * 128], BF16, 'esel')
        for hf in range(2):
            S.dma('pool', esel[:, hf * 4096:(hf + 1) * 4096], c_esel[:, hf * 4096:(hf + 1) * 4096], writes=[esel])
        ovl = S.sbuf([128, 512], BF16, 'ovl')
        S.dma('pool', ovl[:], c_overlap[:], writes=[ovl])
        xkv = S.sbuf([128, SEQ], BF16, 'xkv')
        S.dma('sp', xkv[:], KF[KF_DST['kcvc'][0], :, :], writes=[xkv])
        w1 = S.sbuf([128, 32, 256], BF16, 'cw1')
        posT = S.sbuf([128, 32], BF16, 'cposT')
        w2 = S.sbuf([128, 2, 2, 128], BF16, 'cw2')
        for kv in range(2):
            S.dma('pool', w1[kv * 64:(kv + 1) * 64, :, :], cw1[kv].rearrange('(l d) f -> d l f', d=64), writes=[w1])
            S.dma('pool', posT[kv * 64:(kv + 1) * 64, :], cpos[kv].rearrange('(l d) -> d l', d=64), writes=[posT],
                  allow_slow_non_contiguous=True)
            for dup in range(2):
                S.dma('pool', w2[:, kv, :, dup * 64:(dup + 1) * 64], cw2[kv].rearrange('(hc p) d -> p hc d', p=128),
                      writes=[w2])
        hid = [[S.sbuf([128, 512], BF16, 'hid%d%d' % (kv, hc)) for hc in range(2)] for kv in range(2)]
        pb = S.sbuf([128, 4], F32, 'cpb')
        for kv in range(2):
            base = kv * 64
            x3 = xkv[base:base + 64, :].rearrange('p (n s) -> p n s', s=16)
            for hc in range(2):
                S.memset('pool', hid[kv][hc], hid[kv][hc][:], 0.0)
                bank = PS[kv * 4 + hc]
                bank2 = PS[kv * 4 + 2 + hc]
                for l in range(32):
                    S.mm(bank, bank[:, 0:511], w1, w1[base:base + 64, l, hc * 128:(hc + 1) * 128],
                         xkv, x3[:, (l // 16):(l // 16) + 511, l % 16], start=(l == 0), stop=(l == 31))
                for l in range(32):
                    S.mm(bank2, bank2[:, 0:1], w1, w1[base:base + 64, l, hc * 128:(hc + 1) * 128],
                         posT, posT[base:base + 64, l:l + 1], start=(l == 0), stop=(l == 31))
                col = kv * 2 + hc
                S.cp('dve', pb, pb[:, col:col + 1], bank2, bank2[:, 0:1])
                S.act(hid[kv][hc], hid[kv][hc][:, 0:511], bank, bank[:, 0:511], AF.Silu, reads=[pb],
                      bias=pb[:, col:col + 1])
        kcT = S.sbuf([128, 512], BF16, 'kcT')
        S.memset('pool', kcT, kcT[:], 0.0)
        bank = PS[4]
        for hc in range(2):
            S.mm(bank, bank[:, 0:511], w2, w2[:, 0, hc, :], hid[0][hc], hid[0][hc][:, 0:511], start=(hc == 0), stop=(hc == 1))
        S.cp('dve', kcT, kcT[:, 0:511], bank, bank[:, 0:511])
        vcaug = S.sbuf([128, 4, 65], BF16, 'vcaug')
        S.memset('pool', vcaug, vcaug[:], 1.0)
        for cidx in range(4):
            bank = PS[5 + cidx % 2]
            for hc in range(2):
                S.mm(bank, bank[:, 0:64], hid[1][hc], hid[1][hc][:, cidx * 128:(cidx + 1) * 128], w2, w2[:, 1, hc, 0:64],
                     start=(hc == 0), stop=(hc == 1))
            S.cp('dve', vcaug, vcaug[:, cidx, 0:64], bank, bank[:, 0:64])
        S.barrier()
        pts = [S.sbuf([128, 512], BF16, 'ptD%d' % i) for i in range(3)]
        ec = [S.sbuf([128, 512], BF16, 'ecD%d' % i) for i in range(4)]
        tqb = S.sbuf([128, 128], F32, 'tqb')
        cm = S.sbuf([128, 128], BF16, 'cmD')
        selm = S.sbuf([128, 128], F32, 'selmD')
        imp = S.sbuf([128, 128], F32, 'impD')
        imp3 = S.sbuf([128, 128], F32, 'imp3D')
        t16 = S.sbuf([128, 16], F32, 't16D')
        bsel = S.sbuf([128, 128], F32, 'bselD')
        biasT4 = S.sbuf([128, 512], BF16, 'biasT4')
        rdd = S.sbuf([128, 12], F32, 'rdD')
        gts = S.sbuf([128, 12], F32, 'gtsD')
        coef = S.sbuf([128, 12], F32, 'coefD')
        omixs = [S.sbuf([128, 256], F32, 'omixD%d' % i) for i in range(2)]
        OC, OS, OW, UB = PS[4], PS[5], PS[6], PS[7]
        for si, slot in enumerate(slots):
            S.dma('sp', tqb[:], c_tq[0:1, slot * 128:(slot + 1) * 128].partition_broadcast(128), writes=[tqb])
            S.dma('sp', selm[:], c_selm[:, slot * 128:(slot + 1) * 128], writes=[selm])
            S.dma('sp', gts[:], GT[slot * 128:(slot + 1) * 128, :], writes=[gts])
            cids = [ci for ci in range(4) if 128 * ci <= 16 * slot + 14]
            for n_, ci in enumerate(cids):
                sb = SB2[sbk[0] % 2]
                sbk[0] += 1
                e_ = ec[ci]
                for h in (0, 2, 1, 3):
                    b = (h % 2) * 64
                    S.mm(sb, sb[:, scol(h):scol(h) + 128], kcT, kcT[b:b + 64, ci * 128:(ci + 1) * 128],
                         qd, qd[b:b + 64, h // 2, slot * 128:(slot + 1) * 128], start=True, stop=True)
                S.act(e_, e_[:].rearrange('p (b c) -> p b c', b=2), sb, v2(sb), AF.Exp)
                if 128 * ci + 127 > 16 * slot - 2:
                    S.ts('dve', cm, cm[:], tqb, tqb[:], ptab[:, 4 + ci:5 + ci], None, ALU.is_ge, reads=[ptab])
                    for h in range(4):
                        S.tt('dve', e_, e_[:, h * 128:(h + 1) * 128], e_, e_[:, h * 128:(h + 1) * 128], cm, cm[:], ALU.mult)
                for h in range(4):
                    S.mm(OC, OC[:, h * 65:(h + 1) * 65], e_, e_[:, pcol(h):pcol(h) + 128], vcaug, vcaug[:, ci, :],
                         start=(n_ == 0 and h == 0), stop=(n_ == len(cids) - 1), skip=True)
            for h in range(4):
                for n_, ci in enumerate(cids):
                    S.mm(UB, UB[:, h * 128:(h + 1) * 128], ec[ci], ec[ci][:, pcol(h):pcol(h) + 128],
                         ovl, ovl[:, ci * 128:(ci + 1) * 128], start=(n_ == 0), stop=(n_ == len(cids) - 1))
            oc4 = view4(OC)
            S.ts('dve', rdd, rdd[:, 0:4], OC, oc4[:, :, 64], 1e-30, None, ALU.add)
            S.raw('dve', 'reciprocal', reads=[rdd], writes=[rdd], out=rdd[:, 0:4], in_=rdd[:, 0:4])
            S.ts('dve', imp, imp[:], UB, UB[:, 0:128], rdd[:, 0:1], None, ALU.mult, reads=[rdd])
            for h in range(1, 4):
                S.stt(imp, imp[:], UB, UB[:, h * 128:(h + 1) * 128], rdd[:, h:h + 1], imp, imp[:], ALU.mult, ALU.add,
                      reads=[rdd])
            S.tt('dve', imp, imp[:], imp, imp[:], selm, selm[:], ALU.add)
            S.raw('dve', 'max', reads=[imp], writes=[t16], out=t16[:, 0:8], in_=imp[:])
            S.raw('dve', 'match_replace', reads=[imp, t16], writes=[imp3], out=imp3[:], in_to_replace=t16[:, 0:8],
                  in_values=imp[:], imm_value=-BIG)
            S.raw('dve', 'max', reads=[imp3], writes=[t16], out=t16[:, 8:16], in_=imp3[:])
            S.ts('dve', t16, t16[:, 15:16], t16, t16[:, 15:16], -1e29, None, ALU.max)
            S.ts('dve', bsel, bsel[:], imp, imp[:], t16[:, 15:16], 1.0, ALU.is_ge, ALU.subtract, reads=[t16])
            S.ts('dve', bsel, bsel[:], bsel, bsel[:], -NEG, None, ALU.mult)
            tb_ = UB
            S.tr(tb_, tb_[:, 0:128], bsel, bsel[:], ident, ident[:])
            for h in range(4):
                S.cp(alt(), biasT4, biasT4[:, h * 128:(h + 1) * 128], tb_, tb_[:, 0:128])
            nkb = 2 * slot + 2
            for kb in range(nkb):
                attn_step(slot, kb, ktd, lambda h: 0, qdr, pts, lambda h, kb: (vd, vd[:, kb, 0:65]), OS, kb == 0,
                          kb == nkb - 1, mask=causal_mask(slot, kb),
                          bias=(biasT4, lambda par: biasT4[:, par * 256:(par + 1) * 256], esel,
                                esel[:, kb * 128:(kb + 1) * 128]))
            wk = [kb for kb in range(2 * slot - 4, 2 * slot + 2) if kb >= 0]
            for n_, kb in enumerate(wk):
                m = causal_mask(slot, kb)
                if kb == 2 * slot - 4:
                    m = M_W4
                elif kb == 2 * slot - 3:
                    m = M_W3
                attn_step(slot, kb, ktd, lambda h: 1, qdr, pts, lambda h, kb: (vd, vd[:, kb, 65:130]), OW, n_ == 0,
                          n_ == len(wk) - 1, mask=m)
            attn_flush()
            os4, ow4 = view4(OS), view4(OW)
            S.raw('dve', 'reciprocal', reads=[OS], writes=[rdd], out=rdd[:, 4:8], in_=os4[:, :, 64])
            S.raw('dve', 'reciprocal', reads=[OW], writes=[rdd], out=rdd[:, 8:12], in_=ow4[:, :, 64])
            g3 = gts[:].rearrange('p (h b) -> p h b', b=3)
            c3 = coef[:].rearrange('p (b h) -> p b h', b=3)
            for br in range(3):
                S.tt('dve', coef, c3[:, br, :], gts, g3[:, :, br], rdd, rdd[:, br * 4:(br + 1) * 4], ALU.mult)
            omix = omixs[si % 2]
            for h in range(4):
                oh = omix[:, h * 64:(h + 1) * 64]
                S.ts('dve', omix, oh, OC, oc4[:, h, 0:64], coef[:, h:h + 1], None, ALU.mult, reads=[coef])
                S.stt(omix, oh, OS, os4[:, h, 0:64], coef[:, 4 + h:5 + h], omix, oh, ALU.mult, ALU.add, reads=[coef])
                S.stt(omix, oh, OW, ow4[:, h, 0:64], coef[:, 8 + h:9 + h], omix, oh, ALU.mult, ALU.add, reads=[coef])
            store_mix(omix, slot, 3)
        S.pop_scope()
    if not do_tail:
        return
    H1 = S.dram('H1' + sfx, [TO, DM], F32)
    H1T = S.dram('H1T' + sfx, [8, 128, TO], BF16)
    S.push_scope()
    lnv = S.sbuf([128, 4, DM], F32, 'lnv')
    for i_ in range(4):
        S.dma('sp', lnv[:, i_, :], vecs[i_, :].partition_broadcast(128), writes=[lnv])
    st = S.sbuf([128, 8], F32, 'lnst')
    junk = S.sbuf([128, DM], F32, 'lnjunk')

    def layer_norm(x, out, gi):
        S.act(junk, junk[:], x, x[:], AF.Copy, writes=[st], accum_out=st[:, 0:1])
        S.act(junk, junk[:], x, x[:], AF.Square, writes=[st], accum_out=st[:, 1:2])
        S.ts('dve', st, st[:, 0:2], st, st[:, 0:2], 1.0 / DM, None, ALU.mult)
        S.tt('dve', st, st[:, 2:3], st, st[:, 0:1], st, st[:, 0:1], ALU.mult)
        S.tt('dve', st, st[:, 3:4], st, st[:, 1:2], st, st[:, 2:3], ALU.subtract)
        S.ts('dve', st, st[:, 3:4], st, st[:, 3:4], 1e-5, None, ALU.add)
        S.act(st, st[:, 4:5], st, st[:, 3:4], AF.Sqrt)
        S.raw('dve', 'reciprocal', reads=[st], writes=[st], out=st[:, 5:6], in_=st[:, 4:5])
        S.tt('dve', st, st[:, 6:7], st, st[:, 0:1], st, st[:, 5:6], ALU.mult)
        S.ts('dve', st, st[:, 6:7], st, st[:, 6:7], -1.0, None, ALU.mult)
        S.act(out, out[:], x, x[:], AF.Identity, reads=[st], scale=st[:, 5:6], bias=st[:, 6:7])
        S.tt('dve', out, out[:], out, out[:], lnv, lnv[:, gi, :], ALU.mult)
        S.tt('pool', out, out[:], out, out[:], lnv, lnv[:, gi + 1, :], ALU.add)

    def transpose_rows(x, ncol, dst, dst_ap_fn):
        for g0 in range(0, ncol, 4):
            n = min(4, ncol - g0)
            bank = PS[6 + (g0 // 4) % 2]
            for k4 in range(n):
                S.tr(bank, bank[:, k4 * 128:(k4 + 1) * 128], x, x[:, (g0 + k4) * 128:(g0 + k4 + 1) * 128], ident, ident[:])
            S.cp(alt(), dst, dst_ap_fn(g0, n), bank, bank[:, 0:n * 128].rearrange('p (k t) -> p k t', k=n))

    S.push_scope()
    wo = S.sbuf([128, 8, DM], BF16, 'wo')
    S.dma('pool', wo[:], wout[:].rearrange('(k p) f -> p k f', p=128), writes=[wo])
    mrow = [S.sbuf([128, DM], F32, 'mrow%d' % i) for i in range(2)]
    hrw = [S.sbuf([128, DM], F32, 'hrw%d' % i) for i in range(2)]
    mT = [S.sbuf([128, 8, 128], BF16, 'mT%d' % i) for i in range(2)]
    yy = [S.sbuf([128, DM], F32, 'yy%d' % i) for i in range(2)]
    h1s = [S.sbuf([128, DM], F32, 'h1s%d' % i) for i in range(2)]
    h1t = [S.sbuf([128, 8, 128], BF16, 'h1t%d' % i) for i in range(2)]
    for tb in range(NSLOT):
        r0 = tb * 128
        mr, hr, mt, y, h1, ht = mrow[tb % 2], hrw[tb % 2], mT[tb % 2], yy[tb % 2], h1s[tb % 2], h1t[tb % 2]
        S.dma('sp', mr[:], MIX[r0:r0 + 128, :], reads=[MIX], writes=[mr])
        st_, sap_ = hq_rows(r0)
        S.dma('sp', hr[:], sap_, reads=[st_], writes=[hr])
        transpose_rows(mr, 8, mt, lambda g0, n, mt=mt: mt[:, g0:g0 + n, :])
        for hd in range(2):
            bank = PS[hd]
            for kc in range(8):
                S.mm(bank, bank[:], mt, mt[:, kc, :], wo, wo[:, kc, hd * 512:(hd + 1) * 512], start=(kc == 0), stop=(kc == 7))
            S.stt(y, y[:, hd * 512:(hd + 1) * 512], hr, hr[:, hd * 512:(hd + 1) * 512], ALPHA, bank, bank[:], ALU.mult, ALU.add)
        layer_norm(y, h1, 0)
        S.dma('sp', H1[r0:r0 + 128, :], h1[:], reads=[h1], writes=[H1])
        transpose_rows(h1, 8, ht, lambda g0, n, ht=ht: ht[:, g0:g0 + n, :])
        S.dma('sp', H1T[:, :, r0:r0 + 128].rearrange('k p t -> p k t'), ht[:], reads=[ht], writes=[H1T])
    S.pop_scope()

    S.push_scope()
    TG = 1024
    NTB = TG // 128
    h1T = S.sbuf([128, 8, TG], BF16, 'h1T')
    facc = S.sbuf([128, NTB, DM], F32, 'facc')
    gT = S.sbuf([128, 7, TG], BF16, 'gT')
    w2sc = [S.sbuf([128, 7, DM], BF16, 'w2sc%d' % i) for i in range(2)]
    w1c = [S.sbuf([128, 8, 128], BF16, 'w1c%d' % i) for i in range(2)]
    w3c = [S.sbuf([128, 8, 128], BF16, 'w3c%d' % i) for i in range(2)]
    sa = [S.sbuf([128, 512], BF16, 'sa%d' % i) for i in range(2)]
    wr_sb = S.sbuf([128, 8, 8], BF16, 'wr_sb')
    S.dma('pool', wr_sb[:], wrt[:].rearrange('(k p) f -> p k f', p=128), writes=[wr_sb])
    lg = S.sbuf([128, 8], F32, 'lg')
    tp8 = S.sbuf([128, 8], F32, 'tp8')
    gg = S.sbuf([128, 4], F32, 'gg')
    m12 = S.sbuf([128, 16], F32, 'm12')
    gate = S.sbuf([128, NTB, 8], F32, 'gate')
    pg = S.sbuf([128, 8, DM], BF16, 'pleg_sb')
    pp = S.sbuf([128, 2, DM], BF16, 'plep_sb')
    S.dma('pool', pg[:], pleg[:].rearrange('(k p) f -> p k f', p=128), writes=[pg])
    S.dma('pool', pp[:], plep[:].rearrange('(k p) f -> p k f', p=128), writes=[pp])
    h1r = [S.sbuf([128, DM], F32, 'h1r%d' % i) for i in range(2)]
    y2 = S.sbuf([128, DM], F32, 'y2')
    h2 = [S.sbuf([128, DM], F32, 'h2_%d' % i) for i in range(2)]
    h2T = S.sbuf([128, 8, 128], BF16, 'h2T')
    prow = S.sbuf([128, 256], F32, 'prow')
    pT = S.sbuf([128, 2, 128], BF16, 'pT')
    sg = S.sbuf([128, 512], F32, 'sg')
    oo = [S.sbuf([128, DM], F32, 'oo%d' % i) for i in range(2)]
    wi = 0
    for grp in range(TO // TG):
        t0 = grp * TG
        S.dma('sp', h1T[:], H1T[:, :, t0:t0 + TG].rearrange('k p t -> p k t'), reads=[H1T], writes=[h1T])
        if moe:
            for tb in range(NTB):
                bank = PS[6 + tb % 2]
                for kc in range(8):
                    S.mm(bank, bank[:, 0:8], h1T, h1T[:, kc, tb * 128:(tb + 1) * 128], wr_sb, wr_sb[:, kc, :],
                         start=(kc == 0), stop=(kc == 7))
                S.cp('dve', lg, lg[:], bank, bank[:, 0:8])
                S.raw('dve', 'max', reads=[lg], writes=[tp8], out=tp8[:], in_=lg[:])
                S.tt('dve', gg, gg[:, 0:1], tp8, tp8[:, 1:2], tp8, tp8[:, 0:1], ALU.subtract)
                S.act(gg, gg[:, 1:2], gg, gg[:, 0:1], AF.Sigmoid)
                S.ts('dve', gg, gg[:, 2:3], gg, gg[:, 1:2], -1.0, 1.0, ALU.mult, ALU.add)
                S.ts('dve', m12, m12[:, 0:8], lg, lg[:], tp8[:, 0:1], gg[:, 2:3], ALU.is_equal, ALU.mult, reads=[tp8, gg])
                S.ts('dve', m12, m12[:, 8:16], lg, lg[:], tp8[:, 1:2], gg[:, 1:2], ALU.is_equal, ALU.mult, reads=[tp8, gg])
                S.tt('dve', gate, gate[:, tb, :], m12, m12[:, 0:8], m12, m12[:, 8:16], ALU.add)
        for e_ in range(ne):
            for sc in range(4):
                w2t = w2sc[(e_ * 4 + sc) % 2]
                S.dma('pool', w2t[:], fw2[e_, sc * 896:(sc + 1) * 896, :].rearrange('(j p) f -> p j f', p=128), writes=[w2t])
                for j in range(7):
                    ffc = sc * 7 + j
                    a_w, b_w = w1c[wi % 2], w3c[wi % 2]
                    wi += 1
                    S.dma('pool', a_w[:], fw1[e_, :, ffc * 128:(ffc + 1) * 128].rearrange('(k p) f -> p k f', p=128), writes=[a_w])
                    S.dma('pool', b_w[:], fw3[e_, :, ffc * 128:(ffc + 1) * 128].rearrange('(k p) f -> p k f', p=128), writes=[b_w])
                    for hf in range(TG // 512):
                        ba, bb = PS[(hf % 2) * 2], PS[(hf % 2) * 2 + 1]
                        for kc in range(8):
                            S.mm(ba, ba[:], a_w, a_w[:, kc, :], h1T, h1T[:, kc, hf * 512:(hf + 1) * 512], start=(kc == 0), stop=(kc == 7))
                        for kc in range(8):
                            S.mm(bb, bb[:], b_w, b_w[:, kc, :], h1T, h1T[:, kc, hf * 512:(hf + 1) * 512], start=(kc == 0), stop=(kc == 7))
                        s_ = sa[hf % 2]
                        S.act(s_, s_[:], ba, ba[:], AF.Silu)
                        S.tt('dve', gT, gT[:, j, hf * 512:(hf + 1) * 512], s_, s_[:], bb, bb[:], ALU.mult)
                for tb in range(NTB):
                    for hd in range(2):
                        bank = PS[4 + (tb * 2 + hd) % 4]
                        for j in range(7):
                            S.mm(bank, bank[:], gT, gT[:, j, tb * 128:(tb + 1) * 128], w2t, w2t[:, j, hd * 512:(hd + 1) * 512],
                                 start=(j == 0), stop=(j == 6))
                        fa = facc[:, tb, hd * 512:(hd + 1) * 512]
                        first = (e_ == 0 and sc == 0)
                        if moe:
                            gsc = gate[:, tb, e_:e_ + 1]
                            if first:
                                S.ts('dve', facc, fa, bank, bank[:], gsc, None, ALU.mult, reads=[gate])
                            else:
                                S.stt(facc, fa, bank, bank[:], gsc, facc, fa, ALU.mult, ALU.add, reads=[gate])
                        else:
                            if first:
                                S.cp('dve', facc, fa, bank, bank[:])
                            else:
                                S.tt('dve', facc, fa, facc, fa, bank, bank[:], ALU.add)
        for tb in range(NTB):
            r0 = t0 + tb * 128
            hr, h2_, o_ = h1r[tb % 2], h2[tb % 2], oo[tb % 2]
            S.dma('sp', hr[:], H1[r0:r0 + 128, :], reads=[H1], writes=[hr])
            st_, sap_ = p_rows(r0)
            S.dma('sp', prow[:], sap_, reads=[st_], writes=[prow])
            S.stt(y2, y2[:], hr, hr[:], ALPHA, facc, facc[:, tb, :], ALU.mult, ALU.add)
            layer_norm(y2, h2_, 2)
            transpose_rows(h2_, 8, h2T, lambda g0, n: h2T[:, g0:g0 + n, :])
            transpose_rows(prow, 2, pT, lambda g0, n: pT[:, g0:g0 + n, :])
            for hd in range(2):
                bg, bp = PS[hd * 2], PS[hd * 2 + 1]
                for kc in range(8):
                    S.mm(bg, bg[:], h2T, h2T[:, kc, :], pg, pg[:, kc, hd * 512:(hd + 1) * 512], start=(kc == 0), stop=(kc == 7))
                for k2 in range(2):
                    S.mm(bp, bp[:], pT, pT[:, k2, :], pp, pp[:, k2, hd * 512:(hd + 1) * 512], start=(k2 == 0), stop=(k2 == 1))
                S.act(sg, sg[:], bg, bg[:], AF.Sigmoid)
                S.tt('dve', sg, sg[:], sg, sg[:], bp, bp[:], ALU.mult)
                S.tt('pool', o_, o_[:, hd * 512:(hd + 1) * 512], sg, sg[:], h2_, h2_[:, hd * 512:(hd + 1) * 512], ALU.add)
            ot_, oap_ = out_rows(r0)
            S.dma('sp', oap_, o_[:], reads=[o_], writes=[ot_])
    S.pop_scope()
    S.pop_scope()
    return


WNAMES = ['wkf', 'wqf', 'wkt', 'wqt', 'wout', 'vecs', 'dlam', 'dgain', 'cpos', 'cw1', 'cw2', 'fw1', 'fw3', 'fw2',
          'wrt', 'plep', 'pleg']


def build_fused(cfg=None):
    nc = bass.Bass("TRN2", target_bir_lowering=False)
    S = Sched(nc)
    EI = 'ExternalInput'
    G = dict(nc=nc, S=S)
    hfull = S.dram('hfull', [SEQ, DM], F32, EI)
    posfull = S.dram('posfull', [SEQ], I32, EI)
    posown = S.dram('posown', [TO], I32, EI)
    pown1 = S.dram('pown1', [TO, 256], F32, EI)
    hout = S.dram('hout', [TO, DM], F32, 'ExternalOutput')

    def wset(sfx, moe):
        ne = NEXP if moe else 1
        shp = dict(wkf=[DM, NKF * 128], wqf=[DM, NQF * 128], wkt=[DM, 896], wqt=[DM, 12], wout=[DM, DM], vecs=[4, DM],
                   dlam=[128], dgain=[64], cpos=[2, 2048], cw1=[2, 2048, 256], cw2=[2, 256, 64], fw1=[ne, DM, DFF],
                   fw3=[ne, DM, DFF], fw2=[ne, DFF, DM], wrt=[DM, 8], plep=[256, DM], pleg=[DM, DM])
        return {k: S.dram(k + sfx, shp[k], F32, EI) for k in WNAMES}

    W0 = wset('_l0', False)
    W1 = wset('_l1', True)
    c_ident = S.dram('ident', [128, 128], F32, EI)
    c_ptab = S.dram('ptab', [128, 16], F32, EI)
    G['c_negtri'] = S.dram('negtri', [128, 128], F32, EI)
    G['c_negones'] = S.dram('negones', [128, 128], F32, EI)
    G['c_emoba'] = S.dram('emoba', [32, 64 * 128], F32, EI)
    G['c_esel'] = S.dram('esel', [128, 64 * 128], F32, EI)
    G['c_overlap'] = S.dram('overlap', [128, 512], F32, EI)
    CM = {k: S.dram('masks_' + k, [6, 128, 512], F32, EI) for k in ('own',)}
    CT = {k: S.dram('tq_' + k, [1, NSLOT * 128], F32, EI) for k in ('own',)}
    CS = {k: S.dram('selm_' + k, [128, NSLOT * 128], F32, EI) for k in ('own',)}
    KF0 = S.dram('KF0', [NKFD, 128, SEQ], BF16)
    VA0 = S.dram('VA0', [SEQ, NVA], BF16)
    KF1 = S.dram('KF1', [NKFD, 128, SEQ], BF16)
    VA1 = S.dram('VA1', [SEQ, NVA], BF16)

    ident = S.sbuf([128, 128], F32, 'ident')
    S.dma('sp', ident[:], c_ident[:], writes=[ident])
    ptab = S.sbuf([128, 16], F32, 'ptab')
    S.dma('sp', ptab[:], c_ptab[:], writes=[ptab])
    G['ident'], G['ptab'] = ident, ptab
    G['masks'] = S.sbuf([128, 6, 512], BF16, 'masks')
    G['kmean'] = S.sbuf([128, 2, 32], BF16, 'kmean')
    P2 = [S.psum([128, 1024], F32, 'bank2_%d' % i) for i in range(4)]
    G['P2'] = P2
    G['PS'] = [TileView(P2[i // 2][:, (i % 2) * 512:(i % 2 + 1) * 512], 'bank%d' % i) for i in range(8)]
    G['SB2'] = [TileView(P2[i][:, :], 'sb2_%d' % i) for i in range(2)]

    hown0 = S.dram('hown0', [TO, DM], F32, EI)
    pown0 = S.dram('pown0', [TO, 256], F32, EI)
    NCH = 16
    CR = TO // NCH
    HOWN = [S.dram('HOWN%d' % c, [CR, DM], F32) for c in range(NCH)]
    HG = [S.dram('HG%d' % c, [2 * CR, DM], F32) for c in range(NCH)]
    P = dict(W0)
    P.update(layer=0, moe=False, sfx='_0', KF=KF0, VA=VA0, do_k=True, c_masks=CM['own'], c_tq=CT['own'], c_selm=CS['own'])
    P['hk'] = lambda r0: (hfull, hfull[r0:r0 + 128, :])
    P['hq'] = lambda r0: (hown0, hown0[r0:r0 + 128, :])
    P['posk'] = lambda c0: (posfull, posfull[c0:c0 + 512].partition_broadcast(128), False)
    P['posq'] = lambda c0: (posown, posown[c0:c0 + 512].partition_broadcast(128), False)
    P['prow'] = lambda r0: (pown0, pown0[r0:r0 + 128, :])
    P['out'] = lambda r0: (HOWN[r0 // CR], HOWN[r0 // CR][r0 % CR:r0 % CR + 128, :])
    emit_pass(G, P, cfg)
    for c in range(NCH):
        S.collective('AllGather', [[0, 1], [2, 3], [4, 5], [6, 7]], HOWN[c], HOWN[c][:], HG[c], HG[c][:])
    P = dict(W1)
    P.update(layer=1, moe=True, sfx='_1', KF=KF1, VA=VA1, do_k=True, c_masks=CM['own'], c_tq=CT['own'], c_selm=CS['own'])

    def hk1(r0):
        blk = r0 // 128
        par, i0 = blk % 2, (blk // 2) * 128
        t_ = HG[i0 // CR]
        o_ = par * CR + i0 % CR
        return (t_, t_[o_:o_ + 128, :])

    P['hk'] = hk1
    P['hq'] = lambda r0: (HOWN[r0 // CR], HOWN[r0 // CR][r0 % CR:r0 % CR + 128, :])
    P['posk'] = lambda c0: (posfull, posfull[c0:c0 + 512].partition_broadcast(128), False)
    P['posq'] = lambda c0: (posown, posown[c0:c0 + 512].partition_broadcast(128), False)
    P['prow'] = lambda r0: (pown1, pown1[r0:r0 + 128, :])
    P['out'] = lambda r0: (hout, hout[r0:r0 + 128, :])
    emit_pass(G, P, cfg)
    return nc, S


def _own_rows(parity):
    blk = 2 * np.arange(NSLOT) + parity
    return (blk[:, None] * 128 + np.arange(128)[None, :]).reshape(-1)


def _layer_weights(layer, inp, sfx):
    f = np.float32
    w_in = np.asarray(inp['w_in'][layer], f)
    moe = (layer % 2 == 1)
    w = {
        'wkf': _gather_cols(w_in, KF_IDX), 'wqf': _gather_cols(w_in, QF_IDX),
        'wkt': _gather_cols(w_in, KT_IDX), 'wqt': _gather_cols(w_in, QT_IDX),
        'wout': np.ascontiguousarray(inp['w_out'][layer], f),
        'vecs': np.stack([inp['ln_mix_g'][layer], inp['ln_mix_b'][layer], inp['ln_ffn_g'][layer],
                          inp['ln_ffn_b'][layer]]).astype(f),
        'dlam': np.ascontiguousarray(inp['diff_lambda'][layer], f).reshape(128),
        'dgain': np.ascontiguousarray(inp['diff_gain'][layer], f),
        'cpos': np.ascontiguousarray(inp['nsa_cmp_pos'][layer], f).reshape(2, 2048),
        'cw1': np.ascontiguousarray(inp['nsa_cmp_w1'][layer], f),
        'cw2': np.ascontiguousarray(inp['nsa_cmp_w2'][layer], f),
        'plep': np.ascontiguousarray(inp['ple_proj'][layer], f),
        'pleg': np.ascontiguousarray(inp['ple_gate'][layer], f),
    }
    if moe:
        w['fw1'] = np.ascontiguousarray(inp['moe_w1'][layer // 2], f)
        w['fw3'] = np.ascontiguousarray(inp['moe_w3'][layer // 2], f)
        w['fw2'] = np.ascontiguousarray(inp['moe_w2'][layer // 2], f)
        w['wrt'] = np.ascontiguousarray(inp['moe_router'][layer // 2], f)
    else:
        w['fw1'] = np.ascontiguousarray(inp['ffn_w1'][layer // 2], f)[None]
        w['fw3'] = np.ascontiguousarray(inp['ffn_w3'][layer // 2], f)[None]
        w['fw2'] = np.ascontiguousarray(inp['ffn_w2'][layer // 2], f)[None]
        w['wrt'] = np.zeros((DM, 8), f)
    return {k + sfx: v for k, v in w.items()}


def fused_inputs(inp, cores):
    f = np.float32
    shared = {}
    shared.update(_layer_weights(0, inp, '_l0'))
    shared.update(_layer_weights(1, inp, '_l1'))
    consts = [_host_consts(0), _host_consts(1)]
    for k in ('ident', 'ptab', 'negtri', 'negones', 'emoba', 'esel', 'overlap'):
        shared[k] = consts[0][k]
    maps = []
    for core in cores:
        b, par = core // 2, core % 2
        rows = _own_rows(par)
        m = dict(shared)
        m['masks_own'] = consts[par]['masks']
        m['tq_own'] = consts[par]['tq']
        m['selm_own'] = consts[par]['selm']
        m['hfull'] = np.ascontiguousarray(inp['x'][b], f)
        m['posfull'] = np.ascontiguousarray(inp['positions'][b], np.int32)
        m['posown'] = np.ascontiguousarray(inp['positions'][b][rows], np.int32)
        m['hown0'] = np.ascontiguousarray(inp['x'][b][rows], f)
        m['pown0'] = np.ascontiguousarray(inp['p'][0, b][rows], f)
        m['pown1'] = np.ascontiguousarray(inp['p'][1, b][rows], f)
        maps.append(m)
    return maps


def kernel(**inputs):
    inp = {k: np.asarray(v) for k, v in inputs.items()}
    cores = list(range(8))
    nc, S = build_fused()
    S.finish()
    maps = fused_inputs(inp, cores)
    res = run_bass_kernel_spmd(nc, maps, core_ids=cores)
    out = np.zeros((4, SEQ, DM), np.float32)
    for core in cores:
        b, par = core // 2, core % 2
        out[b][_own_rows(par)] = res.results[core]['hout']
    return out
```
